# Optimizing a Trainium2 kernel written in Bass

```python
import jax, jax.numpy as jnp
from jax import lax
import numpy as np

D_MODEL = 1024
BATCH = 32
SEQ = 2048
DEPTH = 2

GRID_W = 64
N_HEADS = 8
N_KV_HEADS = 2
HEAD_DIM = 64
Q_GROUP = N_HEADS // N_KV_HEADS
ROPE_THETA = 10000.0
Q_BLOCK = 128
SGU_GROUPS = 4
SGU_HEAD = 64
SGU_W = SGU_GROUPS * SGU_HEAD
SGU_CHUNK = 128
POOL_WINDOWS = (2, 4, 8, 16)
POOL_GROUPS = len(POOL_WINDOWS)
POOL_HEAD = 64
POOL_W = POOL_GROUPS * POOL_HEAD
Q_W = N_HEADS * HEAD_DIM
KV_W = N_KV_HEADS * HEAD_DIM
N_BRANCHES = 3
GATE_W = N_BRANCHES * D_MODEL
SPLITS = (Q_W, KV_W, KV_W, SGU_W, SGU_W, POOL_W, GATE_W)
IN_W = sum(SPLITS)
N_EXPERTS = 16
CAPACITY_FACTOR = 2
D_FF_EXPERT = 1024
EPS = 1e-6

kernel_name = "hybrid_gated_attn_sgu_pool_ecmoe"


def rmsnorm(x, g):
    xf = x.astype(jnp.float32)
    y = xf * lax.rsqrt(jnp.mean(xf * xf, axis=-1, keepdims=True) + EPS)
    return (y * g.astype(jnp.float32)).astype(x.dtype)


def axial_rope_tables(seq_len):
    rows = seq_len // GRID_W
    row = jnp.broadcast_to(jnp.arange(rows, dtype=jnp.float32)[:, None], (rows, GRID_W)).reshape(-1)
    col = jnp.broadcast_to(jnp.arange(GRID_W, dtype=jnp.float32)[None, :], (rows, GRID_W)).reshape(-1)
    part = HEAD_DIM // 2
    freqs = ROPE_THETA ** (-jnp.arange(0, part, 2, dtype=jnp.float32) / part)
    ang = jnp.stack([row[:, None] * freqs, col[:, None] * freqs], axis=1)
    return jnp.cos(ang), jnp.sin(ang)


def apply_axial_rope(x, cos, sin):
    B, S, H, _ = x.shape
    nf = HEAD_DIM // 4
    xf = x.astype(jnp.float32).reshape(B, S, H, 2, 2, nf)
    x1, x2 = xf[..., 0, :], xf[..., 1, :]
    c = cos[None, :, None]
    s = sin[None, :, None]
    out = jnp.stack([x1 * c - x2 * s, x2 * c + x1 * s], axis=-2)
    return out.reshape(B, S, H, HEAD_DIM).astype(x.dtype)


def block_attention(q, k, v):
    B, S = q.shape[:2]
    nb = S // Q_BLOCK
    scale = HEAD_DIM ** -0.5
    qb = q.reshape(B, nb, Q_BLOCK, N_KV_HEADS, Q_GROUP, HEAD_DIM).transpose(1, 0, 2, 3, 4, 5)

    def one_block(qblk):
        s = jnp.einsum('bqkgd,bskd->bkgqs', qblk, k).astype(jnp.float32) * scale
        p = jax.nn.softmax(s, axis=-1).astype(v.dtype)
        return jnp.einsum('bkgqs,bskd->bqkgd', p, v)

    o = lax.map(one_block, qb)
    return o.transpose(1, 0, 2, 3, 4, 5).reshape(B, S, Q_W)


def spatial_gating(u, v, g_sgu, w_spatial, b_spatial):
    B, S, _ = v.shape
    nc = S // SGU_CHUNK
    vn = rmsnorm(v, g_sgu).reshape(B, nc, SGU_CHUNK, SGU_GROUPS, SGU_HEAD)
    z = jnp.einsum('gpq,bcqgd->bcpgd', w_spatial, vn) + b_spatial.T[None, None, :, :, None]
    return u * z.reshape(B, S, SGU_W)


def multiscale_pool(p, w_pool, pool_scale):
    B, S, _ = p.shape
    pf = p.astype(jnp.float32)
    cs = jnp.concatenate([jnp.zeros((B, 1, POOL_W), jnp.float32), jnp.cumsum(pf, axis=1)], axis=1)
    t = jnp.arange(S)
    means = []
    for g, w in enumerate(POOL_WINDOWS):
        lo = jnp.clip(t - w // 2, 0, S - 1)
        hi = jnp.clip(t + (w - 1 - w // 2), 0, S - 1)
        seg = cs[:, :, g * POOL_HEAD:(g + 1) * POOL_HEAD]
        cnt = (hi - lo + 1).astype(jnp.float32)
        means.append((jnp.take(seg, hi + 1, axis=1) - jnp.take(seg, lo, axis=1)) / cnt[None, :, None])
    pooled = jnp.concatenate(means, axis=-1)
    d = (pooled - pf).astype(p.dtype).reshape(B, S, POOL_GROUPS, POOL_HEAD)
    y = jnp.einsum('bsgc,gcd->bsgd', d, w_pool).reshape(B, S, POOL_W)
    return y * pool_scale


def expert_choice_moe(h, w_router, w_gate_e, w_up_e, w_down_e):
    B, S, D = h.shape
    cap = CAPACITY_FACTOR * S // N_EXPERTS
    aff = jax.nn.softmax(jnp.einsum('bsd,de->bse', h, w_router).astype(jnp.float32), axis=-1)
    vals, idx = lax.top_k(aff.transpose(0, 2, 1), cap)
    xs = jax.vmap(lambda hb, ib: hb[ib])(h, idx)
    a = jnp.einsum('becd,edf->becf', xs, w_gate_e)
    b = jnp.einsum('becd,edf->becf', xs, w_up_e)
    out = jnp.einsum('becf,efd->becd', jax.nn.silu(a) * b, w_down_e)
    out = out * vals[..., None].astype(out.dtype)
    return jax.vmap(lambda ib, ob: jnp.zeros((S, D), ob.dtype).at[ib.reshape(-1)].add(ob.reshape(-1, D)))(idx, out)


def setup_inputs(seed: int = 0) -> dict:
    key = jax.random.key(seed)
    ks = jax.random.split(key, 24)
    f32 = jnp.float32

    def nrm(k, shape, fan_in):
        return jax.random.normal(k, shape, f32) * (fan_in ** -0.5)

    def gain(k, shape, s=0.05):
        return 1.0 + s * jax.random.normal(k, shape, f32)

    L = DEPTH
    return {
        "x": jax.random.normal(ks[0], (BATCH, SEQ, D_MODEL), f32),
        "g_mix": gain(ks[1], (L, D_MODEL)),
        "w_in": nrm(ks[2], (L, D_MODEL, IN_W), D_MODEL),
        "g_q": gain(ks[3], (L, HEAD_DIM)),
        "g_k": gain(ks[4], (L, HEAD_DIM)),
        "g_sgu": gain(ks[5], (L, SGU_W)),
        "w_spatial": nrm(ks[6], (L, SGU_GROUPS, SGU_CHUNK, SGU_CHUNK), SGU_CHUNK),
        "b_spatial": gain(ks[7], (L, SGU_GROUPS, SGU_CHUNK)),
        "w_pool": nrm(ks[8], (L, POOL_GROUPS, POOL_HEAD, POOL_HEAD), POOL_HEAD),
        "pool_scale": gain(ks[9], (L, POOL_W), 0.1),
        "w_attn_o": nrm(ks[10], (L, Q_W, D_MODEL), Q_W),
        "w_sgu_o": nrm(ks[11], (L, SGU_W, D_MODEL), SGU_W),
        "w_pool_o": nrm(ks[12], (L, POOL_W, D_MODEL), POOL_W),
        "w_out": nrm(ks[13], (L, D_MODEL, D_MODEL), D_MODEL),
        "g_ffn": gain(ks[14], (L, D_MODEL)),
        "w_router": nrm(ks[15], (L, D_MODEL, N_EXPERTS), D_MODEL),
        "w_gate_e": nrm(ks[16], (L, N_EXPERTS, D_MODEL, D_FF_EXPERT), D_MODEL),
        "w_up_e": nrm(ks[17], (L, N_EXPERTS, D_MODEL, D_FF_EXPERT), D_MODEL),
        "w_down_e": nrm(ks[18], (L, N_EXPERTS, D_FF_EXPERT, D_MODEL), D_FF_EXPERT),
        "g_final": gain(ks[19], (D_MODEL,)),
    }


def reference(x, g_mix, w_in, g_q, g_k, g_sgu, w_spatial, b_spatial, w_pool, pool_scale,
              w_attn_o, w_sgu_o, w_pool_o, w_out, g_ffn, w_router, w_gate_e, w_up_e, w_down_e, g_final):
    B, S, D = x.shape
    cos, sin = axial_rope_tables(S)
    cuts = [int(c) for c in np.cumsum(SPLITS)[:-1]]
    for l in range(DEPTH):
        h = rmsnorm(x, g_mix[l])
        proj = jnp.einsum('bsd,dn->bsn', h, w_in[l])
        q, k, v, u, vs, p, gl = jnp.split(proj, cuts, axis=-1)
        q = apply_axial_rope(rmsnorm(q.reshape(B, S, N_HEADS, HEAD_DIM), g_q[l]), cos, sin)
        k = apply_axial_rope(rmsnorm(k.reshape(B, S, N_KV_HEADS, HEAD_DIM), g_k[l]), cos, sin)
        v = v.reshape(B, S, N_KV_HEADS, HEAD_DIM)
        br_a = jnp.einsum('bsc,cd->bsd', block_attention(q, k, v), w_attn_o[l])
        br_b = jnp.einsum('bsc,cd->bsd', spatial_gating(u, vs, g_sgu[l], w_spatial[l], b_spatial[l]), w_sgu_o[l])
        br_c = jnp.einsum('bsc,cd->bsd', multiscale_pool(p, w_pool[l], pool_scale[l]), w_pool_o[l])
        ga, gb, gc = jnp.split(jax.nn.sigmoid(gl), N_BRANCHES, axis=-1)
        merged = ga * br_a + gb * br_b + gc * br_c
        x = x + jnp.einsum('bsd,de->bse', merged, w_out[l])
        h2 = rmsnorm(x, g_ffn[l])
        x = x + expert_choice_moe(h2, w_router[l], w_gate_e[l], w_up_e[l], w_down_e[l])
    return rmsnorm(x, g_final)
```

```python
import numpy as np
import concourse.bass as bass
import concourse.mybir as mybir
from concourse.bass_utils import run_bass_kernel_spmd

F32 = mybir.dt.float32
BF16 = mybir.dt.bfloat16
I32 = mybir.dt.int32
U32 = mybir.dt.uint32
ALU = mybir.AluOpType
AF = mybir.ActivationFunctionType
AX = mybir.AxisListType

D = 1024
S = 2048
NT = 16
KD = 8
L = 2
NSEQ = 4
IN_W = 4608
NE = 16
CAP = 256
EPS = 1e-6
POOL_WINDOWS = (2, 4, 8, 16)
N_CORES = 8
SCHEDULE = True
GRP = 4
STRICT = True


class Op:
    __slots__ = ("eng", "fn", "deps", "signal", "val", "is_dma", "sem_slot", "idx", "prev_same_slot", "odeps", "cost")

    def __init__(self, eng, fn, is_dma):
        self.eng = eng
        self.fn = fn
        self.deps = []
        self.odeps = []
        self.cost = None
        self.signal = False
        self.val = 0
        self.is_dma = is_dma
        self.sem_slot = None
        self.prev_same_slot = None


class Prog:
    ENGS = ("pe", "act", "dve", "pool", "sp")
    NDMA_SEMS = {"sp": 12, "pool": 12, "act": 4}

    def __init__(self):
        self.ops = []
        self.last_w = {}
        self.readers = {}
        self.dma_count = {"sp": 0, "pool": 0, "act": 0}
        self.dma_last = {}

    def _add_dep(self, op, dep, kind):
        if dep is None or dep is op:
            return
        op.odeps.append(dep)
        if (not dep.is_dma) and dep.eng == op.eng and not op.is_dma:
            if op.eng == "pe" or (kind != "raw" and not STRICT):
                return
        op.deps.append(dep)
        dep.signal = True

    def op(self, eng, fn, R=(), W=(), dma=False, cost=None):
        o = Op(eng, fn, dma)
        o.cost = cost
        for t in R:
            self._add_dep(o, self.last_w.get(t), "raw")
        for t in W:
            self._add_dep(o, self.last_w.get(t), "waw")
            for r in self.readers.get(t, ()):
                self._add_dep(o, r, "war")
        for t in R:
            lst = self.readers.setdefault(t, [])
            if not dma:
                lst[:] = [r for r in lst if r.is_dma or r.eng != eng]
            lst.append(o)
        for t in W:
            self.last_w[t] = o
            self.readers[t] = []
        if dma:
            o.signal = True
            self.dma_count[eng] += 1
        self.ops.append(o)
        return o

    def dma(self, queue, out, in_, R=(), W=(), cost=None, **kw):
        return self.op(queue, lambda e: e.dma_start(out=out, in_=in_, **kw), R, W, dma=True, cost=cost)

    DEF_COST = {"pe": 160.0, "act": 450.0, "dve": 450.0, "pool": 800.0}

    def schedule(self, window=24):
        ops = self.ops
        for i, o in enumerate(ops):
            o.idx = i
        pend = {e: [o for o in ops if o.eng == e] for e in self.ENGS}
        head = {e: 0 for e in self.ENGS}
        fin = [None] * len(ops)
        etime = {e: 0.0 for e in self.ENGS}
        order = {e: [] for e in self.ENGS}
        done = [False] * len(ops)
        remaining = len(ops)
        SEM = 120.0

        def cand(e):
            lst = pend[e]
            h = head[e]
            while h < len(lst) and done[lst[h].idx]:
                h += 1
            head[e] = h
            best = None
            seen = 0
            i = h
            while i < len(lst) and seen < window:
                o = lst[i]
                i += 1
                if done[o.idx]:
                    continue
                seen += 1
                st = etime[e]
                ok = True
                for d in o.odeps:
                    f = fin[d.idx]
                    if f is None:
                        ok = False
                        break
                    if d.eng != e or d.is_dma:
                        f = f + SEM
                    elif not o.is_dma and not d.is_dma:
                        f = f if (e == "pe") else f + 60.0
                    if f > st:
                        st = f
                if not ok:
                    continue
                if best is None or st < best[0]:
                    best = (st, o)
                    if st <= etime[e]:
                        break
            return best
        cands = {e: cand(e) for e in self.ENGS}
        while remaining:
            be = None
            for e in self.ENGS:
                c = cands[e]
                if c is not None and (be is None or c[0] < cands[be][0] or (c[0] == cands[be][0] and c[1].idx < cands[be][1].idx)):
                    be = e
            assert be is not None, "scheduler deadlock"
            st, o = cands[be]
            if o.is_dma:
                etime[be] = st + 90.0
                fin[o.idx] = st + (o.cost if o.cost is not None else 3000.0)
            else:
                c = o.cost if o.cost is not None else self.DEF_COST[be]
                etime[be] = st + c
                fin[o.idx] = st + c + (150.0 if be == "pe" else 0.0)
            done[o.idx] = True
            order[be].append(o)
            remaining -= 1
            for e in self.ENGS:
                cands[e] = cand(e)
        self.order = order
        self.sim_time = max(etime.values())

    def emit(self, nc):
        if not hasattr(self, "order"):
            self.order = {e: [o for o in self.ops if o.eng == e] for e in self.ENGS}
        per_eng = self.order
        cnt = {e: 0 for e in self.ENGS}
        for e in self.ENGS:
            ndma = 0
            last_slot = {}
            for o in per_eng[e]:
                if o.is_dma:
                    k = self.NDMA_SEMS[e]
                    o.sem_slot = (e, ndma % k)
                    o.val = 16 * (ndma // k + 1)
                    o.prev_same_slot = last_slot.get(o.sem_slot)
                    last_slot[o.sem_slot] = o
                    ndma += 1
                elif o.signal:
                    cnt[e] += 1
                    o.val = cnt[e]
        self.sig_counts = dict(cnt)
        import contextlib
        with contextlib.ExitStack() as st:
            esem = {e: st.enter_context(nc.semaphore("s_" + e)) for e in self.ENGS}
            dsem = {}
            for q, k in self.NDMA_SEMS.items():
                for i in range(k):
                    dsem[(q, i)] = st.enter_context(nc.semaphore("d_%s%d" % (q, i)))
            block = st.enter_context(nc.Block())

            def sem_of(o):
                return dsem[o.sem_slot] if o.is_dma else esem[o.eng]

            def run(engname, eng):
                waited = {}
                tail = {}
                for o in per_eng[engname]:
                    deps = list(o.deps)
                    if o.is_dma and o.prev_same_slot is not None:
                        deps.append(o.prev_same_slot)
                    for d in deps:
                        s = sem_of(d)
                        key = id(s)
                        if waited.get(key, 0) < d.val:
                            eng.wait_ge(s, d.val)
                            waited[key] = d.val
                    ins = o.fn(eng)
                    if o.signal:
                        ins.then_inc(sem_of(o), 16 if o.is_dma else 1)
                    if o.is_dma:
                        tail[id(sem_of(o))] = (sem_of(o), o.val)
                for s, v in tail.values():
                    if waited.get(id(s), 0) < v:
                        eng.wait_ge(s, v)

            @block.tensor
            def _(e):
                run("pe", e)

            @block.scalar
            def _(e):
                run("act", e)

            @block.vector
            def _(e):
                run("dve", e)

            @block.gpsimd
            def _(e):
                run("pool", e)

            @block.sync
            def _(e):
                run("sp", e)


def _rope_table():
    rows = S // 64
    row = np.broadcast_to(np.arange(rows, dtype=np.float32)[:, None], (rows, 64)).reshape(-1)
    col = np.broadcast_to(np.arange(64, dtype=np.float32)[None, :], (rows, 64)).reshape(-1)
    freqs = (10000.0 ** (-np.arange(0, 32, 2, dtype=np.float32) / 32)).astype(np.float32)
    ang = np.stack([row[:, None] * freqs, col[:, None] * freqs], axis=1)
    cs = np.concatenate([np.cos(ang).reshape(S, 32), np.sin(ang).reshape(S, 32)], axis=1)
    return cs.astype(np.float32)


def _pool_bands():
    out = np.zeros((4, 5, 128, 128), np.float32)
    t = np.arange(S)
    for g, w in enumerate(POOL_WINDOWS):
        lo = np.clip(t - w // 2, 0, S - 1)
        hi = np.clip(t + (w - 1 - w // 2), 0, S - 1)
        cnt = (hi - lo + 1).astype(np.float32)

        def blk(ci, cj):
            m = np.zeros((128, 128), np.float32)
            for tt in range(ci * 128, ci * 128 + 128):
                for tp in range(max(lo[tt], cj * 128), min(hi[tt], cj * 128 + 127) + 1):
                    m[tp - cj * 128, tt - ci * 128] += 1.0 / cnt[tt]
                if ci == cj:
                    m[tt - cj * 128, tt - ci * 128] -= 1.0
            return m
        out[g, 0] = blk(5, 4)
        out[g, 1] = blk(5, 6)
        out[g, 2] = blk(5, 5)
        out[g, 3] = blk(0, 0)
        out[g, 4] = blk(15, 15)
    return out


def build_program(nseq=NSEQ, nlayers=L, debug=None, stop_after=None):
    nc = bass.Bass("TRN2", target_bir_lowering=False)
    P = Prog()
    dbg = {}

    def din(name, shape, dt=F32):
        return nc.dram_tensor(name, list(shape), dt, kind="ExternalInput").ap()

    x_in = din("x", [nseq, S, D])
    w_in = din("w_in", [L, D, IN_W])
    w_spatial = din("w_spatial", [L, 4, 128, 128])
    w_pool = din("w_pool", [L, 4, 64, 64])
    w_attn_o = din("w_attn_o", [L, 512, D])
    w_sgu_o = din("w_sgu_o", [L, 256, D])
    w_pool_o = din("w_pool_o", [L, 256, D])
    w_out = din("w_out", [L, D, D])
    w_router = din("w_router", [L, D, NE])
    w_gate_e = din("w_gate_e", [L, NE, D, D])
    w_up_e = din("w_up_e", [L, NE, D, D])
    w_down_e = din("w_down_e", [L, NE, D, D])
    gmix_b = din("gmix_b", [L, 128, D])
    gffn_b = din("gffn_b", [L, 128, D])
    gfin_b = din("gfin_b", [128, D])
    gq_b = din("gq_b", [L, 128, 64])
    gk_b = din("gk_b", [L, 128, 64])
    gsgu_b = din("gsgu_b", [L, 128, 256])
    bsp_t = din("bsp_t", [L, 128, 4])
    psc_t = din("psc_t", [L, 128, 2])
    ident_in = din("ident", [128, 128])
    cs_in = din("cs", [S, 64])
    band_in = din("band", [4, 5, 128, 128])

    y_out = nc.dram_tensor("y", [nseq, S, D], F32, kind="ExternalOutput").ap()
    xs_dram = [nc.dram_tensor("xs_scr%d" % i, [S, D], F32, kind="Internal").ap() for i in range(nseq)]
    h2_dram = [nc.dram_tensor("h2_scr%d" % i, [S, D], BF16, kind="Internal").ap() for i in range(GRP)]

    import contextlib
    st = contextlib.ExitStack()

    def sb(name, shape, dt):
        return st.enter_context(nc.sbuf_tensor(name, list(shape), dt))

    def ps(name, shape, dt):
        return st.enter_context(nc.psum_tensor(name, list(shape), dt))

    arena = sb("arena", [128, 32768], BF16)
    hT = arena[:, 0:16384].rearrange("p (k t) -> p k t", k=KD)
    qT = arena[:, 16384:24576].rearrange("p (j t) -> p j t", j=4)
    kT2 = arena[:, 24576:32768].rearrange("p (g v t) -> p g v t", g=2, v=2)
    xres = arena.bitcast(F32).rearrange("p (c d) -> p c d", c=NT)
    mT = sb("mT", [128, KD, S], BF16)
    wB = mT.ap().rearrange("p k t -> p (k t)")[:, 0:12288].rearrange("p (s k n) -> p s k n", s=3, k=KD)
    sy = sb("sy", [128, 8192], BF16)
    sgT = sy.ap()[:, 0:4096].rearrange("p (j t) -> p j t", j=2)
    yT = sy.ap()[:, 4096:8192].rearrange("p (j t) -> p j t", j=2)
    wD = sb("wD", [128, 2 * KD * 3 * 256], BF16)
    wDv = wD.ap().rearrange("p (b k x n) -> p b k x n", b=2, k=KD, x=3)
    wO = wD.ap()[:, 0:KD * D].rearrange("p (k n) -> p k n", k=KD)
    vaug = wD.ap()[:, 6144:12288].rearrange("p (c g e) -> p c g e", c=NT, g=2)
    NRING = 6
    wexp = [arena[:, i * 8192:(i + 1) * 8192].rearrange("p (k n) -> p k n", k=KD) for i in range(4)]
    wexp.append(sy.ap().rearrange("p (k n) -> p k n", k=KD))
    wexp.append(wD.ap()[:, 0:8192].rearrange("p (k n) -> p k n", k=KD))
    woA = sb("woA", [128, 2, 4, 256], BF16)
    woB = sb("woB", [128, 2, 2, 256], BF16)
    woC = sb("woC", [128, 2, 2, 256], BF16)
    xin = sb("xin", [128, 2, D], F32)
    hb = sb("hb", [128, 2, D], BF16)
    gnb = sb("gnb", [128, D], F32)
    ss = sb("ss", [128, NT], F32)
    sm = sb("sm", [128, 40], F32)
    epsb = sb("epsb", [128, 1], F32)
    ident = sb("identb", [128, 128], BF16)
    identf = sb("identf", [128, 128], F32)
    cs = sb("cs_sb", [128, NT, 64], F32)
    band = sb("band_sb", [128, 4, 5, 128], BF16)
    wsT = sb("wsT", [128, L, 4, 128], BF16)
    wpl2 = sb("wpl2", [128, L, 2, 128], BF16)
    wr = sb("wr", [128, L, KD, NE], BF16)
    gq = sb("gq", [128, L, 64], F32)
    gk = sb("gk", [128, L, 64], F32)
    gsg = sb("gsg", [128, L, 256], F32)
    bsp = sb("bsp", [128, L, 4], F32)
    psc = sb("psc", [128, L, 2], F32)
    vnb = sb("vnb", [128, 2, 256], BF16)
    sgtok = sb("sgtok", [128, 256], BF16)
    dtok = sb("dtok", [128, 256], BF16)
    ptok = sb("ptok", [128, NT, 256], BF16)
    dTc = sb("dTc", [128, 2, 128], BF16)
    aff = sb("aff", [128, GRP, NT, NE], F32)
    tvals = sb("tvals", [112, CAP], F32)
    tidx = sb("tidx", [112, CAP], U32)
    tidxf = sb("tidxf", [112, CAP], F32)
    idxT = sb("idxT", [128, 2, 112], I32)
    valsT = sb("valsT", [128, 2, 112], F32)
    sab = sb("sab", [128, 2, 512], F32)
    tmp = sb("tmp", [128, 4096], BF16)

    def tslot(i, n=1, dt=BF16):
        v = tmp.ap()[:, i * 512:(i + n) * 512]
        return v if dt == BF16 else v.bitcast(dt)

    def ttok(i, n=1):
        return [("tmp", j) for j in range(i, i + n)]
    wsp_raw = tslot(0, 1).rearrange("p (g q) -> p g q", g=4)
    qn = tslot(0, 2, F32); qr = tslot(2, 2, F32); sqf = tslot(4, 2, F32); qtok = tslot(6); utok = tslot(7, 1, F32)
    pT = [tslot(i) for i in range(3)]
    otok = tslot(3, 4).rearrange("p (c n) -> p c n", c=4)
    sga = [tslot(i) for i in range(3)]
    mprod = [tslot(3, 2, F32), tslot(5, 2, F32)]

    pbank = [ps("pb%d" % i, [128, 512], F32) for i in range(8)]

    def pb_bf(i):
        return pbank[i].ap().bitcast(BF16)

    P.dma("pool", ident[:], ident_in, W=["ident"])
    P.dma("sp", identf[:], ident_in, W=["identf"])
    P.dma("sp", cs[:], cs_in.rearrange("(c p) f -> p c f", p=128), W=["cs"])
    P.dma("pool", band[:], band_in.rearrange("g v a b -> a g v b"), W=["band"])
    P.op("dve", lambda e: e.memset(wpl2[:], 0.0), W=["wpl2"])
    P.op("dve", lambda e: e.memset(epsb[:], EPS), W=["epsb"])
    for l in range(L):
        for g in range(4):
            gg = g % 2
            P.dma("pool", wpl2[gg * 64:(gg + 1) * 64, l, g // 2, gg * 64:(gg + 1) * 64], w_pool[l, g], W=["wpl2"], R=[])
        P.dma("pool", wr[:, l], w_router[l].rearrange("(k p) e -> p k e", p=128), W=["wr"])
        P.dma("sp", gq[:, l], gq_b[l], W=["gq"])
        P.dma("sp", gk[:, l], gk_b[l], W=["gk"])
        P.dma("sp", gsg[:, l], gsgu_b[l], W=["gsg"])
        P.dma("sp", bsp[:, l], bsp_t[l], W=["bsp"])
        P.dma("sp", psc[:, l], psc_t[l], W=["psc"])
        P.dma("pool", wsp_raw, w_spatial[l].rearrange("g p q -> p g q"), W=["wsp_raw"] + ttok(0))
        tp = pb_bf(0)
        for g in range(4):
            P.op("pe", lambda e, g=g, tp=tp: e.transpose(tp[:, g * 128:(g + 1) * 128], wsp_raw[:, g, :], ident[:]),
                 R=["wsp_raw", "ident"] + ttok(0), W=[("pb", 0)])
        P.op("dve", lambda e, l=l, tp=tp: e.tensor_copy(out=wsT[:, l], in_=tp[:, 0:512].rearrange("p (g q) -> p g q", g=4)),
             R=[("pb", 0)], W=["wsT"])

    def rsqrt_eps(ap, toks):
        P.op("act", lambda e: e.activation(out=ap, in_=ap, func=AF.Sqrt, bias=epsb[0:ap.shape[0], 0:1], scale=1.0), R=toks + ["epsb"], W=toks)
        P.op("dve", lambda e: e.reciprocal(out=ap, in_=ap), R=toks, W=toks)

    def norm_phase(x_src_dram, g_dram, to_hT, from_xres=False, h2_dst=None, y_dst=None, hT_tok="hT", pbase=6, xtok=0, h2i=0):
        P.dma("sp", gnb[:], g_dram, W=["gnb"])
        if y_dst is not None:
            fnb = mT.ap().rearrange("p k t -> p (k t)").bitcast(F32).rearrange("p (i d) -> p i d", i=8)
        for c in range(NT):
            b = c % 2
            if y_dst is not None:
                bi_, bo_ = c % 4, 4 + c % 4
                P.dma("sp", fnb[:, bi_], x_src_dram[c * 128:(c + 1) * 128, :], R=[("xs_dram", xtok), ("xsd", xtok, c)], W=[("fn", bi_)])
                P.op("act", lambda e, c=c, bi_=bi_, bo_=bo_: e.activation(out=fnb[:, bo_], in_=fnb[:, bi_], func=AF.Square, scale=1.0 / 32.0, accum_out=ss[:, c:c + 1]),
                     R=[("fn", bi_)], W=[("fn", bo_), ("ss", c)])
                rsqrt_eps(ss[:, c:c + 1], [("ss", c)])
                P.op("dve", lambda e, c=c, bi_=bi_, bo_=bo_: e.scalar_tensor_tensor(out=fnb[:, bo_], in0=fnb[:, bi_], scalar=ss[:, c:c + 1],
                                                                                     in1=gnb[:], op0=ALU.mult, op1=ALU.mult),
                     R=[("fn", bi_), ("fn", bo_), ("ss", c), "gnb"], W=[("fn", bo_)])
                P.dma("sp", y_dst[c * 128:(c + 1) * 128, :], fnb[:, bo_], R=[("fn", bo_)], W=[("y_out", xtok, c)])
                continue
            if from_xres:
                src = xres[:, c, :]
                srcR = [("xres", c)]
            else:
                P.dma("sp", xin[:, b], x_src_dram[c * 128:(c + 1) * 128, :], R=[("xs_dram", xtok), ("xsd", xtok, c)], W=[("xin", b)])
                src = xin[:, b]
                srcR = [("xin", b)]
            P.op("act", lambda e, src=src, c=c, b=b: e.activation(out=hb[:, b], in_=src, func=AF.Square, scale=1.0 / 32.0, accum_out=ss[:, c:c + 1]),
                 R=srcR, W=[("hb", b), ("ss", c)])
            rsqrt_eps(ss[:, c:c + 1], [("ss", c)])
            P.op("dve", lambda e, src=src, c=c, b=b: e.scalar_tensor_tensor(out=hb[:, b], in0=src, scalar=ss[:, c:c + 1],
                                                                             in1=gnb[:], op0=ALU.mult, op1=ALU.mult),
                 R=srcR + [("ss", c), "gnb"], W=[("hb", b)])
            if h2_dst is not None:
                P.dma("sp", h2_dst[c * 128:(c + 1) * 128, :], hb[:, b], R=[("hb", b)], W=[("h2d", h2i, c)])
            bk = pbase + (c % 2)
            tp = pb_bf(bk)
            for k in range(KD):
                P.op("pe", lambda e, k=k, b=b, tp=tp: e.transpose(tp[:, k * 128:(k + 1) * 128], hb[:, b, k * 128:(k + 1) * 128], ident[:]),
                     R=[("hb", b), "ident"], W=[("pb", bk)])
            P.op("act", lambda e, c=c, tp=tp: e.activation(out=to_hT[:, :, c * 128:(c + 1) * 128],
                                                           in_=tp[:, 0:1024].rearrange("p (k t) -> p k t", k=KD), func=AF.Copy),
                 R=[("pb", bk)], W=[(hT_tok, c)])

    def headnorm_rope(src_ps, nh, gain, c, dst_bf, Rsrc, Wdst):
        w = nh * 64
        P.op("act", lambda e: e.activation(out=sqf[:, 0:w], in_=src_ps, func=AF.Square, scale=0.125), R=Rsrc, W=ttok(4, 2))
        P.op("dve", lambda e: e.tensor_reduce(out=sm[:, 0:nh], in_=sqf[:, 0:w].rearrange("p (h d) -> p h d", h=nh), axis=AX.X, op=ALU.add),
             R=ttok(4, 2), W=["sm"])
        rsqrt_eps(sm[:, 0:nh], ["sm"])
        P.op("dve", lambda e: e.tensor_tensor(out=qn[:, 0:w].rearrange("p (h d) -> p h d", h=nh),
                                              in0=src_ps.rearrange("p (h d) -> p h d", h=nh),
                                              in1=sm[:, 0:nh].unsqueeze(2).to_broadcast([128, nh, 64]), op=ALU.mult),
             R=Rsrc + ["sm"], W=ttok(0, 2))
        P.op("dve", lambda e: e.tensor_tensor(out=qn[:, 0:w].rearrange("p (h d) -> p h d", h=nh),
                                              in0=qn[:, 0:w].rearrange("p (h d) -> p h d", h=nh),
                                              in1=gain.unsqueeze(1).to_broadcast([128, nh, 64]), op=ALU.mult),
             R=ttok(0, 2) + ["gq", "gk"], W=ttok(0, 2))
        m = nh * 2
        qv = qn[:, 0:w].rearrange("p (m two f) -> p m two f", m=m, two=2)
        rv = qr[:, 0:w].rearrange("p (m two f) -> p m two f", m=m, two=2)
        dv = dst_bf.rearrange("p (m two f) -> p m two f", m=m, two=2)
        cosb = cs[:, c, 0:32].rearrange("p (a f) -> p a f", a=2)
        sinb = cs[:, c, 32:64].rearrange("p (a f) -> p a f", a=2)

        def bc(t):
            return t.unsqueeze(1).to_broadcast([128, nh, 2, 16])
        x1 = qn[:, 0:w].rearrange("p (h a two f) -> p h a two f", h=nh, a=2, two=2)
        r_ = qr[:, 0:w].rearrange("p (h a two f) -> p h a two f", h=nh, a=2, two=2)
        d_ = dst_bf.rearrange("p (h a two f) -> p h a two f", h=nh, a=2, two=2)
        P.op("dve", lambda e: e.tensor_tensor(out=r_[:, :, :, 0, :], in0=x1[:, :, :, 0, :], in1=bc(cosb), op=ALU.mult), R=ttok(0, 2) + ["cs"], W=ttok(2, 2))
        P.op("dve", lambda e: e.tensor_tensor(out=r_[:, :, :, 1, :], in0=x1[:, :, :, 1, :], in1=bc(sinb), op=ALU.mult), R=ttok(0, 2) + ["cs"], W=ttok(2, 2))
        P.op("dve", lambda e: e.tensor_tensor(out=d_[:, :, :, 0, :], in0=r_[:, :, :, 0, :], in1=r_[:, :, :, 1, :], op=ALU.subtract), R=ttok(2, 2), W=Wdst)
        P.op("dve", lambda e: e.tensor_tensor(out=r_[:, :, :, 0, :], in0=x1[:, :, :, 1, :], in1=bc(cosb), op=ALU.mult), R=ttok(0, 2) + ["cs"], W=ttok(2, 2))
        P.op("dve", lambda e: e.tensor_tensor(out=r_[:, :, :, 1, :], in0=x1[:, :, :, 0, :], in1=bc(sinb), op=ALU.mult), R=ttok(0, 2) + ["cs"], W=ttok(2, 2))
        P.op("dve", lambda e: e.tensor_tensor(out=d_[:, :, :, 1, :], in0=r_[:, :, :, 0, :], in1=r_[:, :, :, 1, :], op=ALU.add), R=ttok(2, 2), W=Wdst)

    def dbg_out(name, ap_sb, shape, dt=F32, R=()):
        t = nc.dram_tensor("dbg_" + name, list(shape), dt, kind="ExternalOutput").ap()
        if hasattr(ap_sb, "ap") and callable(getattr(ap_sb, "ap")):
            ap_sb = ap_sb.ap()
        P.dma("sp", t, ap_sb, R=list(R), W=["dbg_" + name])
        dbg[name] = t

    ALLHT = [("hT", c) for c in range(NT)]
    ALLQT = [("qT", c) for c in range(NT)]
    ALLMT = [("mT", c) for c in range(NT)]
    ALLX = [("xres", c) for c in range(NT)]
    ALLQT = [("qT", c, j, hp) for c in range(NT) for j in range(4) for hp in range(2)]
    VAUGT = [("vaug", c) for c in range(NT)] + ["vaug_ones"]
    MIXT = ALLHT + ALLQT + [("kT2", c) for c in range(NT)] + [("kT2b", c) for c in range(NT)] + ["kT2_zero"] + VAUGT
    WEXPT = [("wexp", i) for i in range(6)]
    SYT = [("sgT", c) for c in range(NT)] + [("yT", c) for c in range(NT)]
    WDT = [("wD", bb, x_) for bb in range(2) for x_ in range(3)] + ["wO_a", "wO_b"]
    WBT = [("wB", s) for s in range(3)]
    SUBB = [("pb", 3), ("pb", 4), ("pb", 5), ("pb", 3), ("pb", 3), ("pb", 4), ("pb", 4), ("pb", 5), ("pb", 5), "sq_", "qr_", ("utok_", 0), ("utok_", 1)]
    XSD = [[("xsd", b_, c) for c in range(NT)] for b_ in range(nseq)]
    H2D = [[("h2d", i_, c) for c in range(NT)] for i_ in range(GRP)]
    EXPACT = [("xg", i) for i in range(4)] + [("oe", i) for i in range(4)] + ["xsT", "gT"]
    done = False
    def layer_body(b, l, bi):
        if True:
            x_src = x_in[b] if l == 0 else xs_dram[b]
            P.op("dve", lambda e: e.memset(sm[:, 32:33], 0.0), W=MIXT + ALLX + WEXPT + ALLMT + WBT + EXPACT + SYT + WDT + SUBB + [("fn", i) for i in range(8)] + ["fence"])
            if stop_after == "A00" and l == nlayers - 1:
                dbg_out("xs", xs_dram[0], [S, D], F32, R=[("xs_dram", 0)] + XSD[0])
                return True
            P.op("pool", lambda e: e.memset(kT2[64:128, :, 0, :], 0.0), W=["kT2_zero"])
            P.op("pool", lambda e: e.memset(kT2[0:64, :, 1, :], 0.0), W=["kT2_zero"])
            P.op("dve", lambda e: e.memset(vaug[:, :, :, 0:64], 1.0), W=["vaug_ones"])
            P.op("dve", lambda e: e.memset(vaug[:, :, :, 128:192], 1.0), W=["vaug_ones"])
            for s in range(3):
                P.dma("pool", wB[:, s], w_in[l][:, s * 512:(s + 1) * 512].rearrange("(k p) n -> p k n", p=128),
                      W=ALLMT + [("wB", s)] if s == 0 else [("wB", s)], R=[])
            if stop_after == "A0" and l == nlayers - 1:
                dbg_out("xs", xs_dram[0], [S, D], F32, R=[("xs_dram", 0)] + XSD[0])
                return True
            norm_phase(x_src, gmix_b[l], hT, xtok=b)
            if stop_after == "A" and l == nlayers - 1:
                dbg_out("hT", hT, [128, KD, S], BF16, R=ALLHT)
                if l > 0:
                    dbg_out("xs", xs_dram[0], [S, D], F32, R=[("xs_dram", 0)] + XSD[0])
                return True
            mtail = mT.ap().rearrange("p k t -> p (k t)")[:, 12288:16384]
            qkb = [tslot(0, 3, F32)[:, 0:640], tslot(3, 3, F32)[:, 0:640]]
            qkt = [ttok(0, 3), ttok(3, 3)]
            qtok2 = tslot(6, 2)[:, 0:640]
            sq_ = mtail[:, 0:1280].bitcast(F32)
            qr_ = mtail[:, 1280:2560].bitcast(F32)
            utk = [mtail[:, 2560:3072].bitcast(F32), mtail[:, 3072:3584].bitcast(F32)]

            def pool_chunk(c):
                for g in range(4):
                    terms = []
                    if c > 0:
                        terms.append((c - 1, 0))
                    terms.append((c, 3 if c == 0 else (4 if c == NT - 1 else 2)))
                    if c < NT - 1:
                        terms.append((c + 1, 1))
                    for i, (cj, v) in enumerate(terms):
                        P.op("pe", lambda e, g=g, cj=cj, v=v, i=i, n=len(terms): e.matmul(
                            pbank[5][:, 256 + g * 64:256 + (g + 1) * 64], lhsT=band[:, g, v, :], rhs=ptok[:, cj, g * 64:(g + 1) * 64],
                            start=(i == 0), stop=(i == n - 1)), R=[("ptok", cj), "band"], W=[("pb", 5)])
                P.op("act", lambda e: e.activation(out=dtok[:, :], in_=pbank[5][:, 256:512], func=AF.Copy), R=[("pb", 5)], W=["dtok"])
                tpd = pb_bf(3)
                for j in range(2):
                    P.op("pe", lambda e, j=j: e.transpose(tpd[:, 512 + j * 128:512 + (j + 1) * 128], dtok[:, j * 128:(j + 1) * 128], ident[:]),
                         R=["dtok", "ident"], W=[("pb", 3)])
                P.op("act", lambda e: e.activation(out=dTc[:, :, :], in_=tpd[:, 512:768].rearrange("p (j t) -> p j t", j=2), func=AF.Copy),
                     R=[("pb", 3)], W=["dTc"])
                for j in range(2):
                    P.op("pe", lambda e, j=j: e.matmul(pbank[4][:, 256 + j * 128:256 + (j + 1) * 128], lhsT=wpl2[:, l, j, :], rhs=dTc[:, j, :],
                                                       start=True, stop=True), R=["dTc", "wpl2"], W=[("pb", 4)])
                P.op("dve", lambda e: e.tensor_tensor(out=yT[:, :, c * 128:(c + 1) * 128], in0=pbank[4][:, 256:512].rearrange("p (j t) -> p j t", j=2),
                                                      in1=psc[:, l, :].unsqueeze(2).to_broadcast([128, 2, 128]), op=ALU.mult),
                     R=[("pb", 4), "psc"], W=[("yT", c)])

            def projB1(c):
                cb = c % 2
                qk = qkb[cb]
                QK = qkt[cb]
                vn_ = vnb[:, cb, :]
                VN = [("vn", cb)]
                utok_ = utk[cb]
                UT = [("utok_", cb)]
                for s_ in range(3):
                    for k in range(KD):
                        P.op("pe", lambda e, s_=s_, k=k: e.matmul(pbank[s_][:, :], lhsT=hT[:, k, c * 128:(c + 1) * 128], rhs=wB[:, s_, k, :],
                                                                  start=(k == 0), stop=(k == KD - 1)),
                             R=[("hT", c), ("wB", s_)], W=[("pb", s_)])
                P.op("act", lambda e: e.activation(out=qk[:, 0:512], in_=pbank[0][:, :], func=AF.Copy), R=[("pb", 0)], W=QK)
                P.op("act", lambda e: e.activation(out=qk[:, 512:640], in_=pbank[1][:, 0:128], func=AF.Copy), R=[("pb", 1)], W=QK)
                P.op("act", lambda e: e.activation(out=sq_[:, :], in_=qk[:, :], func=AF.Square, scale=0.125), R=QK, W=["sq_"])
                P.op("act", lambda e: e.activation(out=vaug[:, c, :, 64:128], in_=pbank[1][:, 128:256].rearrange("p (g d) -> p g d", g=2), func=AF.Copy),
                     R=[("pb", 1)], W=[("vaug", c)])
                P.op("act", lambda e: e.activation(out=utok_[:, :], in_=pbank[1][:, 256:512], func=AF.Copy), R=[("pb", 1)], W=UT)
                P.op("dve", lambda e: e.tensor_reduce(out=sm[:, 0:10], in_=sq_[:, :].rearrange("p (h d) -> p h d", h=10), axis=AX.X, op=ALU.add),
                     R=["sq_"], W=["sm"])
                rsqrt_eps(sm[:, 0:10], ["sm"])
                P.op("dve", lambda e: e.tensor_tensor(out=qk[:, :].rearrange("p (h d) -> p h d", h=10),
                                                      in0=qk[:, :].rearrange("p (h d) -> p h d", h=10),
                                                      in1=sm[:, 0:10].unsqueeze(2).to_broadcast([128, 10, 64]), op=ALU.mult),
                     R=QK + ["sm"], W=QK)
                P.op("act", lambda e: e.activation(out=vn_, in_=pbank[2][:, 0:256], func=AF.Square, scale=1.0 / 16.0, accum_out=sm[:, 16:17]),
                     R=[("pb", 2)], W=VN + ["sm2"])
                rsqrt_eps(sm[:, 16:17], ["sm2"])
                P.op("dve", lambda e: e.scalar_tensor_tensor(out=vn_, in0=pbank[2][:, 0:256], scalar=sm[:, 16:17], in1=gsg[:, l, :],
                                                             op0=ALU.mult, op1=ALU.mult), R=[("pb", 2), "sm2", "gsg"], W=VN)
                P.op("act", lambda e: e.activation(out=ptok[:, c, :], in_=pbank[2][:, 256:512], func=AF.Copy), R=[("pb", 2)], W=[("ptok", c)])
            def projB2(c):
                cb = c % 2
                qk = qkb[cb]
                QK = qkt[cb]
                vn_ = vnb[:, cb, :]
                VN = [("vn", cb)]
                utok_ = utk[cb]
                UT = [("utok_", cb)]
                P.op("dve", lambda e: e.tensor_tensor(out=qk[:, 0:512].rearrange("p (h d) -> p h d", h=8),
                                                       in0=qk[:, 0:512].rearrange("p (h d) -> p h d", h=8),
                                                       in1=gq[:, l, :].unsqueeze(1).to_broadcast([128, 8, 64]), op=ALU.mult), R=QK + ["gq"], W=QK)
                P.op("dve", lambda e: e.tensor_tensor(out=qk[:, 512:640].rearrange("p (h d) -> p h d", h=2),
                                                       in0=qk[:, 512:640].rearrange("p (h d) -> p h d", h=2),
                                                       in1=gk[:, l, :].unsqueeze(1).to_broadcast([128, 2, 64]), op=ALU.mult), R=QK + ["gk"], W=QK)
                x1 = qk[:, :].rearrange("p (h a two f) -> p h a two f", h=10, a=2, two=2)
                r_ = qr_[:, :].rearrange("p (h a two f) -> p h a two f", h=10, a=2, two=2)
                d_ = qtok2.rearrange("p (h a two f) -> p h a two f", h=10, a=2, two=2)
                cosb = cs[:, c, 0:32].rearrange("p (a f) -> p a f", a=2).unsqueeze(1).to_broadcast([128, 10, 2, 16])
                sinb = cs[:, c, 32:64].rearrange("p (a f) -> p a f", a=2).unsqueeze(1).to_broadcast([128, 10, 2, 16])
                QT2 = ttok(6, 2)
                P.op("pool", lambda e: e.tensor_tensor(out=r_[:, :, :, 0, :], in0=x1[:, :, :, 0, :], in1=cosb, op=ALU.mult), R=QK + ["cs"], W=["qr_"])
                P.op("pool", lambda e: e.tensor_tensor(out=r_[:, :, :, 1, :], in0=x1[:, :, :, 1, :], in1=sinb, op=ALU.mult), R=QK + ["cs"], W=["qr_"])
                P.op("pool", lambda e: e.tensor_tensor(out=d_[:, :, :, 0, :], in0=r_[:, :, :, 0, :], in1=r_[:, :, :, 1, :], op=ALU.subtract), R=["qr_"], W=QT2)
                P.op("pool", lambda e: e.tensor_tensor(out=r_[:, :, :, 0, :], in0=x1[:, :, :, 1, :], in1=cosb, op=ALU.mult), R=QK + ["cs"], W=["qr_"])
                P.op("pool", lambda e: e.tensor_tensor(out=r_[:, :, :, 1, :], in0=x1[:, :, :, 0, :], in1=sinb, op=ALU.mult), R=QK + ["cs"], W=["qr_"])
                P.op("pool", lambda e: e.tensor_tensor(out=d_[:, :, :, 1, :], in0=r_[:, :, :, 0, :], in1=r_[:, :, :, 1, :], op=ALU.add), R=["qr_"], W=QT2)
                tp = pb_bf(3)
                for j in range(4):
                    P.op("pe", lambda e, j=j: e.transpose(tp[:, j * 128:(j + 1) * 128], qtok2[:, j * 128:(j + 1) * 128], ident[:]),
                         R=QT2 + ["ident"], W=[("pb", 3)])
                P.op("act", lambda e: e.activation(out=qT[:, :, c * 128:(c + 1) * 128],
                                                   in_=tp[:, 0:512].rearrange("p (j t) -> p j t", j=4), func=AF.Copy),
                     R=[("pb", 3)], W=[("qT", c, j_, hp_) for j_ in range(4) for hp_ in range(2)])
                tpk = pb_bf(4)
                for g in range(2):
                    for hh in range(2):
                        P.op("pe", lambda e, g=g, hh=hh: e.transpose(
                            tpk[hh * 64:(hh + 1) * 64, g * 128:(g + 1) * 128], qtok2[:, 512 + g * 64:512 + (g + 1) * 64], ident[:]),
                            R=QT2 + ["ident"], W=[("pb", 4)])
                P.op("act", lambda e: e.activation(out=kT2[0:64, :, 0, c * 128:(c + 1) * 128],
                                                   in_=tpk[0:64, 0:256].rearrange("p (g t) -> p g t", g=2), func=AF.Copy),
                     R=[("pb", 4)], W=[("kT2", c)])
                P.op("dve", lambda e: e.tensor_copy(out=kT2[64:128, :, 1, c * 128:(c + 1) * 128],
                                                    in_=tpk[64:128, 0:256].rearrange("p (g t) -> p g t", g=2)),
                     R=[("pb", 4)], W=[("kT2b", c)])
                for g in range(4):
                    P.op("pe", lambda e, g=g: e.matmul(pbank[5][:, g * 64:(g + 1) * 64], lhsT=wsT[:, l, g, :], rhs=vn_[:, g * 64:(g + 1) * 64],
                                                       start=True, stop=True), R=VN + ["wsT"], W=[("pb", 5)])
                P.op("dve", lambda e: e.tensor_tensor(out=sgtok[:, :].rearrange("p (g d) -> p g d", g=4),
                                                      in0=pbank[5][:, 0:256].rearrange("p (g d) -> p g d", g=4),
                                                      in1=bsp[:, l, :].unsqueeze(2).to_broadcast([128, 4, 64]), op=ALU.add),
                     R=[("pb", 5), "bsp"], W=["sgtok"])
                P.op("pool", lambda e: e.tensor_tensor(out=sgtok[:, :], in0=sgtok[:, :], in1=utok_[:, :], op=ALU.mult), R=["sgtok"] + UT, W=["sgtok"])
                tps = pb_bf(4)
                for j in range(2):
                    P.op("pe", lambda e, j=j: e.transpose(tps[:, 256 + j * 128:256 + (j + 1) * 128], sgtok[:, j * 128:(j + 1) * 128], ident[:]),
                         R=["sgtok", "ident"], W=[("pb", 4)])
                P.op("act", lambda e: e.activation(out=sgT[:, :, c * 128:(c + 1) * 128],
                                                   in_=tps[:, 256:512].rearrange("p (j t) -> p j t", j=2), func=AF.Copy),
                     R=[("pb", 4)], W=[("sgT", c)])

            projB1(0)
            for c in range(NT):
                if c + 1 < NT:
                    projB1(c + 1)
                projB2(c)
                if c >= 1:
                    pool_chunk(c - 1)
            pool_chunk(NT - 1)
            P.op("dve", lambda e: e.memset(sm[:, 34:35], 0.0),
                 W=[("pb", 3), ("pb", 4), ("pb", 5), ("pb", 3), ("pb", 3), ("pb", 4), ("pb", 4), ("pb", 5), ("pb", 5), "sq_", "qr_", ("utok_", 0), ("utok_", 1), "fenceB"])
            if stop_after == "B" and l == nlayers - 1:
                dbg_out("qT", qT, [128, 4, S], BF16, R=ALLQT)
                dbg_out("kT2", kT2, [128, 2, 2, S], BF16, R=[("kT2", c) for c in range(NT)] + [("kT2b", c) for c in range(NT)] + ["kT2_zero"])
                dbg_out("vaug", vaug, [128, NT, 2, 192], BF16, R=[("vaug", c) for c in range(NT)] + ["vaug_ones"])
                dbg_out("sgT", sgT, [128, 2, S], BF16, R=[("sgT", c) for c in range(NT)])
                dbg_out("yT", yT, [128, 2, S], BF16, R=[("yT", c) for c in range(NT)])
                return True
            items = [(qb, h, s_) for qb in range(4) for h in range(8) for s_ in range(NT)]
            recb = tslot(3, 2, F32)
            RECT = ttok(3, 2)

            def qk_exp(i):
                qb, h, s_ = items[i]
                g = h // 4
                hp = h % 2
                ho = hp * 64
                j = h // 2
                sbk = i % 3
                P.op("pe", lambda e: e.matmul(
                    pbank[sbk][:, :], lhsT=kT2[:, g, hp, s_ * 128:(s_ + 1) * 128], rhs=qT[:, j, qb * 512:(qb + 1) * 512],
                    start=True, stop=True),
                    R=[("kT2", s_), ("kT2b", s_), "kT2_zero"] + [("qT", qb * 4 + i_, j, hp_) for i_ in range(4) for hp_ in range(2)],
                    W=[("pb", sbk)], cost=230.0)
                P.op("act", lambda e: e.activation(out=pT[sbk], in_=pbank[sbk][:, :], func=AF.Exp, scale=0.125),
                     R=[("pb", sbk)], W=ttok(sbk), cost=560.0)

            def pv(i):
                qb, h, s_ = items[i]
                g = h // 4
                hp = h % 2
                j = h // 2
                sbk = i % 3
                pob = 3 + (h % 2)
                win = slice(64, 192) if hp == 0 else slice(0, 128)
                P.op("pe", lambda e: e.matmul(pbank[pob][:, :], lhsT=vaug[:, s_, g, win], rhs=pT[sbk],
                                              start=(s_ == 0), stop=(s_ == NT - 1)),
                     R=ttok(sbk) + [("vaug", s_), "vaug_ones"], W=[("pb", pob)], cost=230.0)
                if s_ == NT - 1:
                    op_ = slice(0, 64) if hp == 0 else slice(64, 128)
                    dp_ = slice(64, 128) if hp == 0 else slice(0, 64)
                    P.op("dve", lambda e: e.reciprocal(out=recb[dp_, :], in_=pbank[pob][dp_, :]), R=[("pb", pob)], W=RECT, cost=600.0)
                    P.op("dve", lambda e: e.tensor_tensor(out=qT[op_, j, qb * 512:(qb + 1) * 512], in0=pbank[pob][op_, :], in1=recb[dp_, :], op=ALU.mult),
                         R=[("pb", pob)] + RECT, W=[("qT", qb * 4 + i_, j, hp) for i_ in range(4)], cost=600.0)
            LOOK = 2
            for i in range(min(LOOK, len(items))):
                qk_exp(i)
            for i in range(len(items)):
                if i + LOOK < len(items):
                    qk_exp(i + LOOK)
                pv(i)
            if stop_after == "C" and l == nlayers - 1:
                dbg_out("oT", qT, [128, 4, S], BF16, R=ALLQT)
                return True
            P.op("dve", lambda e: e.memset(sm[:, 32:33], 0.0), W=ALLMT + WBT + ["sq_", "qr_", ("utok_", 0), ("utok_", 1), "fence"])

            def load_D(ns):
                buf = ns % 2
                for x_ in range(3):
                    c0 = 1536 + x_ * 1024 + ns * 256
                    P.dma("pool", wDv[:, buf, :, x_, :], w_in[l][:, c0:c0 + 256].rearrange("(k p) n -> p k n", p=128),
                          W=[("wD", buf, x_)] + (VAUGT if buf == 1 else []))
                P.dma("pool", woA[:, buf], w_attn_o[l][:, ns * 256:(ns + 1) * 256].rearrange("(j p) n -> p j n", p=128), W=[("woA", buf)])
                P.dma("pool", woB[:, buf], w_sgu_o[l][:, ns * 256:(ns + 1) * 256].rearrange("(j p) n -> p j n", p=128), W=[("woB", buf)])
                P.dma("pool", woC[:, buf], w_pool_o[l][:, ns * 256:(ns + 1) * 256].rearrange("(j p) n -> p j n", p=128), W=[("woC", buf)])
            load_D(0)
            for ns in range(4):
                buf = ns % 2
                if ns + 1 < 4:
                    load_D(ns + 1)
                for tb in range(4):
                    tsl = slice(tb * 512, (tb + 1) * 512)
                    tt = [tb * 4 + i for i in range(4)]
                    for nn in range(2):
                        n = ns * 2 + nn
                        nsl = slice(nn * 128, (nn + 1) * 128)
                        for x_ in range(3):
                            for k in range(KD):
                                P.op("pe", lambda e, x_=x_, k=k, buf=buf, nsl=nsl, tsl=tsl: e.matmul(
                                    pbank[x_][:, :], lhsT=wDv[:, buf, k, x_, nsl], rhs=hT[:, k, tsl], start=(k == 0), stop=(k == KD - 1)),
                                    R=[("wD", buf, x_)] + [("hT", t) for t in tt], W=[("pb", x_)])
                        for j in range(4):
                            P.op("pe", lambda e, j=j, buf=buf, nsl=nsl, tsl=tsl: e.matmul(
                                pbank[3][:, :], lhsT=woA[:, buf, j, nsl], rhs=qT[:, j, tsl], start=(j == 0), stop=(j == 3)),
                                R=[("woA", buf)] + [("qT", t, j, hp_) for t in tt for hp_ in range(2)], W=[("pb", 3)])
                        for j in range(2):
                            P.op("pe", lambda e, j=j, buf=buf, nsl=nsl, tsl=tsl: e.matmul(
                                pbank[4][:, :], lhsT=woB[:, buf, j, nsl], rhs=sgT[:, j, tsl], start=(j == 0), stop=(j == 1)),
                                R=[("woB", buf)] + [("sgT", t) for t in tt], W=[("pb", 4)])
                        for j in range(2):
                            P.op("pe", lambda e, j=j, buf=buf, nsl=nsl, tsl=tsl: e.matmul(
                                pbank[5][:, :], lhsT=woC[:, buf, j, nsl], rhs=yT[:, j, tsl], start=(j == 0), stop=(j == 1)),
                                R=[("woC", buf)] + [("yT", t) for t in tt], W=[("pb", 5)])
                        for x_ in range(3):
                            P.op("act", lambda e, x_=x_: e.activation(out=sga[x_], in_=pbank[x_][:, :], func=AF.Sigmoid),
                                 R=[("pb", x_)], W=ttok(x_))
                        P.op("dve", lambda e: e.tensor_tensor(out=mprod[0], in0=pbank[3][:, :], in1=sga[0], op=ALU.mult), R=[("pb", 3)] + ttok(0), W=ttok(3, 2))
                        P.op("dve", lambda e: e.tensor_tensor(out=mprod[1], in0=pbank[4][:, :], in1=sga[1], op=ALU.mult), R=[("pb", 4)] + ttok(1), W=ttok(5, 2))
                        P.op("pool", lambda e: e.tensor_tensor(out=mprod[0], in0=mprod[0], in1=mprod[1], op=ALU.add), R=ttok(3, 4), W=ttok(3, 2))
                        P.op("dve", lambda e: e.tensor_tensor(out=mprod[1], in0=pbank[5][:, :], in1=sga[2], op=ALU.mult), R=[("pb", 5)] + ttok(2), W=ttok(5, 2))
                        P.op("pool", lambda e, n=n, tsl=tsl: e.tensor_tensor(out=mT[:, n, tsl], in0=mprod[0], in1=mprod[1], op=ALU.add),
                             R=ttok(3, 4), W=[("mT", t) for t in tt])
            if stop_after == "D" and l == nlayers - 1:
                dbg_out("mT", mT, [128, KD, S], BF16, R=ALLMT)
                return True
            w_out_v = w_out[l].rearrange("(k p) n -> p k n", p=128)
            P.dma("pool", wO[:, 0:6, :], w_out_v[:, 0:6, :], W=[("wD", 0, x_) for x_ in range(3)] + ["wO_a"])
            P.dma("pool", wO[:, 6:8, :], w_out_v[:, 6:8, :], W=[("wD", 1, x_) for x_ in range(3)] + VAUGT + ["wO_b"])
            P.op("dve", lambda e: e.memset(sm[:, 32:33], 0.0), W=MIXT + ALLX + ["fence"])
            for c in range(NT):
                b_ = c % 2
                P.dma("sp", xin[:, b_], x_src[c * 128:(c + 1) * 128, :], R=[("xs_dram", b), ("xsd", b, c)], W=[("xin", b_)])
                for half in range(2):
                    bk = (c * 2 + half) % 4
                    for n in range(KD):
                        P.op("pe", lambda e, n=n, c=c, half=half, bk=bk: e.matmul(
                            pbank[bk][:, :], lhsT=mT[:, n, c * 128:(c + 1) * 128], rhs=wO[:, n, half * 512:(half + 1) * 512],
                            start=(n == 0), stop=(n == KD - 1)), R=[("mT", c), "wO_a" if n < 6 else "wO_b"], W=[("pb", bk)], cost=230.0)
                    P.op("dve", lambda e, c=c, half=half, bk=bk, b_=b_: e.tensor_tensor(
                        out=xres[:, c, half * 512:(half + 1) * 512], in0=pbank[bk][:, :], in1=xin[:, b_, half * 512:(half + 1) * 512], op=ALU.add),
                        R=[("pb", bk), ("xin", b_)], W=[("xres", c)])
            if stop_after == "E" and l == nlayers - 1:
                dbg_out("xres", xres, [128, NT, D], F32, R=ALLX)
                return True
            for c in range(NT):
                P.dma("sp", xs_dram[b][c * 128:(c + 1) * 128, :], xres[:, c, :], R=[("xres", c), ("xs_dram", b)], W=[("xsd", b, c)])
            norm_phase(None, gffn_b[l], mT.ap(), from_xres=True, h2_dst=h2_dram[bi], hT_tok="mT", h2i=bi)
            if stop_after == "F" and l == nlayers - 1:
                dbg_out("h2T", mT, [128, KD, S], BF16, R=ALLMT)
                return True
            lg = pbank[0].ap()[:, 0:256].rearrange("p (c e) -> p c e", c=NT)
            for c in range(NT):
                for k in range(KD):
                    P.op("pe", lambda e, c=c, k=k: e.matmul(pbank[0][:, c * 16:(c + 1) * 16], lhsT=mT[:, k, c * 128:(c + 1) * 128], rhs=wr[:, l, k, :],
                                                            start=(k == 0), stop=(k == KD - 1)), R=[("mT", c), "wr"], W=[("pb", 0)])
            SS = [("ss", c) for c in range(NT)]
            AFT = [("aff", bi)]
            av = aff[:, bi]
            P.op("dve", lambda e: e.tensor_reduce(out=ss[:, :], in_=lg, axis=AX.X, op=ALU.max), R=[("pb", 0)], W=SS)
            P.op("dve", lambda e: e.tensor_tensor(out=av, in0=lg, in1=ss[:, :].unsqueeze(2).to_broadcast([128, NT, NE]), op=ALU.subtract),
                 R=[("pb", 0)] + SS, W=AFT)
            P.op("act", lambda e: e.activation(out=av, in_=av, func=AF.Exp), R=AFT, W=AFT)
            P.op("dve", lambda e: e.tensor_reduce(out=ss[:, :], in_=av, axis=AX.X, op=ALU.add), R=AFT, W=SS)
            P.op("dve", lambda e: e.reciprocal(out=ss[:, :], in_=ss[:, :]), R=SS, W=SS)
            P.op("dve", lambda e: e.tensor_tensor(out=av, in0=av, in1=ss[:, :].unsqueeze(2).to_broadcast([128, NT, NE]), op=ALU.mult),
                 R=AFT + SS, W=AFT)
            if stop_after == "G0" and l == nlayers - 1:
                dbg_out("aff", av, [128, NT, NE], F32, R=AFT)
                return True
        return False

    def moe_body(pair, l):
        NP = len(pair)
        TP = 32 * NP - 16
        NC_ = NP * 256
        if True:
            P.op("dve", lambda e: e.memset(sm[:, 32:33], 0.0), W=ALLX + MIXT + WEXPT + SYT + WDT + ["fence"])
            wsrc = [w_gate_e, w_up_e, w_down_e]
            nload = [0]

            def load_w(i):
                e_, m_ = divmod(i, 3)
                P.dma("pool", wexp[i % NRING], wsrc[m_][l, e_].rearrange("(k p) n -> p k n", p=128), W=[("wexp", i % NRING)], cost=13000.0)
            while nload[0] < NRING:
                load_w(nload[0])
                nload[0] += 1
            affT = tmp.ap().bitcast(F32)[0:TP, :]
            AFALL = [("aff", bi) for bi in range(NP)]
            if NP >= 2:
                P.op("dve", lambda e: e.memset(affT[:, :], 0.0), W=ttok(0, 8))
            for bi in range(NP):
                for c in range(NT):
                    bk = 1 + c // 4
                    P.op("pe", lambda e, c=c, bk=bk, bi=bi: e.transpose(pbank[bk][0:16, (c % 4) * 128:(c % 4 + 1) * 128], aff[:, bi, c, :], identf[:]),
                         R=[("aff", bi), "identf"], W=[("pb", bk)])
                for q4 in range(4):
                    P.op("dve", lambda e, q4=q4, bi=bi: e.tensor_copy(out=affT[bi * 32:bi * 32 + 16, q4 * 512:(q4 + 1) * 512], in_=pbank[1 + q4][0:16, :]),
                         R=[("pb", 1 + q4)], W=ttok(0, 8))
            for r in range(CAP // 8):
                rs = slice(r * 8, (r + 1) * 8)
                P.op("dve", lambda e, rs=rs: e.max(out=tvals[0:TP, rs], in_=affT), R=ttok(0, 8), W=["tvals"], cost=2300.0)
                P.op("dve", lambda e, rs=rs: e.max_index(out=tidx[0:TP, rs], in_max=tvals[0:TP, rs], in_values=affT), R=ttok(0, 8) + ["tvals"], W=["tidx"], cost=2300.0)
                P.op("dve", lambda e, rs=rs: e.match_replace(out=affT, in_to_replace=tvals[0:TP, rs], in_values=affT, imm_value=-1.0),
                     R=ttok(0, 8) + ["tvals"], W=ttok(0, 8), cost=2300.0)
            P.op("dve", lambda e: e.tensor_copy(out=tidxf[0:TP, :], in_=tidx[0:TP, :]), R=["tidx"], W=["tidxf"])
            for half in range(2):
                P.op("pe", lambda e, half=half: e.transpose(pbank[5][:, half * 112:half * 112 + TP], tidxf[0:TP, half * 128:(half + 1) * 128], identf[0:TP, 0:TP]),
                     R=["tidxf", "identf"], W=[("pb", 5)])
                P.op("pe", lambda e, half=half: e.transpose(pbank[5][:, 224 + half * 112:224 + half * 112 + TP], tvals[0:TP, half * 128:(half + 1) * 128], identf[0:TP, 0:TP]),
                     R=["tvals", "identf"], W=[("pb", 5)])
            P.op("dve", lambda e: e.tensor_copy(out=idxT[:, :, 0:TP], in_=pbank[5][:, 0:224].rearrange("p (h e) -> p h e", h=2)[:, :, 0:TP]), R=[("pb", 5)], W=["idxT"])
            P.op("dve", lambda e: e.tensor_copy(out=valsT[:, :, 0:TP], in_=pbank[5][:, 224:448].rearrange("p (h e) -> p h e", h=2)[:, :, 0:TP]), R=[("pb", 5)], W=["valsT"])
            if stop_after == "G1" and l == nlayers - 1:
                dbg_out("idxT", idxT[:, :, 0:NE], [128, 2, NE], I32, R=["idxT"])
                dbg_out("valsT", valsT[:, :, 0:NE], [128, 2, NE], F32, R=["valsT"])
                return True
            mflat = mT.ap().rearrange("p k t -> p (k t)")
            oe = mflat[:, 0:8192].bitcast(F32).rearrange("p (s d) -> p s d", s=4)
            xg = mflat[:, 8192:12288].rearrange("p (s d) -> p s d", s=4)
            xsT = mflat[:, 12288:16384].rearrange("p (k c) -> p k c", k=KD)
            gT = ptok.ap().rearrange("p c n -> p (c n)").rearrange("p (k c) -> p k c", k=KD)
            PTK = [("ptok", c) for c in range(NT)]
            XG = [("xg", i) for i in range(4)]
            OE = [("oe", i) for i in range(4)]
            P.op("dve", lambda e: e.memset(sm[:, 33:34], 0.0), W=ALLMT + XG + OE + PTK + ["xsT", "gT", "fence2"])

            NSP = (NP + 1) // 2
            steps = [(e_, sp) for e_ in range(NE) for sp in range(NSP)]

            def gather(step):
                e_, sp = steps[step]
                for bl in range(min(2, NP - 2 * sp)):
                    bi = 2 * sp + bl
                    for half in range(2):
                        sc = bl * 2 + half
                        P.op("pool", lambda g_, half=half, bi=bi, sc=sc: g_.indirect_dma_start(
                            out=xg[:, sc, :], out_offset=None, in_=h2_dram[bi][:, :],
                            in_offset=bass.IndirectOffsetOnAxis(ap=idxT[:, half, bi * 32 + e_:bi * 32 + e_ + 1], axis=0)),
                            R=["idxT"] + H2D[bi], W=[("xg", sc)], dma=True, cost=4000.0)
            gather(0)

            def ex_trans(step):
                e_, sp = steps[step]
                NL = min(2, NP - 2 * sp)
                NCL = NL * 256
                for sc in range(2 * NL):
                    tpx = pb_bf(6 + sc % 2)
                    for k in range(KD):
                        P.op("pe", lambda e, k=k, sc=sc, tpx=tpx: e.transpose(tpx[:, k * 128:(k + 1) * 128], xg[:, sc, k * 128:(k + 1) * 128], ident[:]),
                             R=[("xg", sc), "ident"], W=[("pb", 6 + sc % 2)], cost=100.0)
                    P.op("act", lambda e, sc=sc, tpx=tpx: e.activation(out=xsT[:, :, sc * 128:(sc + 1) * 128],
                                                                       in_=tpx[:, 0:1024].rearrange("p (k t) -> p k t", k=KD), func=AF.Copy),
                         R=[("pb", 6 + sc % 2)], W=["xsT"], cost=800.0)
                if step + 1 < len(steps):
                    gather(step + 1)

            def ex_gateup(step):
                e_, sp = steps[step]
                NL = min(2, NP - 2 * sp)
                NCL = NL * 256
                if sp == 0:
                    while nload[0] <= min(3 * e_ + 5, 3 * NE - 1):
                        load_w(nload[0])
                        nload[0] += 1
                sg_, su_, sd_ = (3 * e_) % NRING, (3 * e_ + 1) % NRING, (3 * e_ + 2) % NRING
                mmc = 125.0 * NL
                for f in range(KD):
                    bk = f % 2
                    fs = slice(f * 128, (f + 1) * 128)
                    for k in range(KD):
                        P.op("pe", lambda e, k=k, fs=fs, bk=bk: e.matmul(pbank[bk][:, 0:NCL], lhsT=wexp[sg_][:, k, fs], rhs=xsT[:, k, 0:NCL],
                                                                         start=(k == 0), stop=(k == KD - 1)), R=[("wexp", sg_), "xsT"], W=[("pb", bk)], cost=mmc)
                    for k in range(KD):
                        P.op("pe", lambda e, k=k, fs=fs, bk=bk: e.matmul(pbank[2 + bk][:, 0:NCL], lhsT=wexp[su_][:, k, fs], rhs=xsT[:, k, 0:NCL],
                                                                         start=(k == 0), stop=(k == KD - 1)), R=[("wexp", su_), "xsT"], W=[("pb", 2 + bk)], cost=mmc)
                    P.op("act", lambda e, bk=bk: e.activation(out=sab[:, bk, 0:NCL], in_=pbank[bk][:, 0:NCL], func=AF.Silu), R=[("pb", bk)], W=[("sab", bk)], cost=300.0 * NL)
                    P.op("dve", lambda e, bk=bk, f=f: e.tensor_tensor(out=gT[:, f, 0:NCL], in0=pbank[2 + bk][:, 0:NCL], in1=sab[:, bk, 0:NCL], op=ALU.mult),
                         R=[("pb", 2 + bk), ("sab", bk)], W=["gT"], cost=300.0 * NL)

            def ex_down(step):
                e_, sp = steps[step]
                NL = min(2, NP - 2 * sp)
                NCL = NL * 256
                sg_, su_, sd_ = (3 * e_) % NRING, (3 * e_ + 1) % NRING, (3 * e_ + 2) % NRING
                for sc in range(2 * NL):
                    bl, half = divmod(sc, 2)
                    bi = 2 * sp + bl
                    for dh in range(2):
                        bk = 4 + dh
                        for f in range(KD):
                            P.op("pe", lambda e, f=f, sc=sc, dh=dh, bk=bk: e.matmul(
                                pbank[bk][:, :], lhsT=gT[:, f, sc * 128:(sc + 1) * 128], rhs=wexp[sd_][:, f, dh * 512:(dh + 1) * 512],
                                start=(f == 0), stop=(f == KD - 1)), R=[("wexp", sd_), "gT"], W=[("pb", bk)], cost=230.0)
                        P.op("act", lambda e, sc=sc, dh=dh, bk=bk, bi=bi, half=half: e.activation(
                            out=oe[:, sc, dh * 512:(dh + 1) * 512], in_=pbank[bk][:, :], func=AF.Copy, scale=valsT[:, half, bi * 32 + e_:bi * 32 + e_ + 1]),
                            R=[("pb", bk), "valsT"], W=[("oe", sc)], cost=600.0)
                    P.op("pool", lambda g_, sc=sc, bi=bi, half=half: g_.indirect_dma_start(
                        out=xs_dram[pair[bi]][:, :], out_offset=bass.IndirectOffsetOnAxis(ap=idxT[:, half, bi * 32 + e_:bi * 32 + e_ + 1], axis=0),
                        in_=oe[:, sc, :], in_offset=None, compute_op=ALU.add),
                        R=[("oe", sc), "idxT"] + XSD[pair[bi]], W=[("xs_dram", pair[bi])], dma=True, cost=5000.0)
            ex_trans(0)
            for step in range(len(steps)):
                ex_gateup(step)
                if step + 1 < len(steps):
                    ex_trans(step + 1)
                ex_down(step)
            if stop_after == "G" and l == nlayers - 1:
                dbg_out("xs", xs_dram[0], [S, D], F32, R=[("xs_dram", 0)] + XSD[0])
                return True
        return False

    for p0 in range(0, nseq, GRP):
        pair = list(range(p0, min(p0 + GRP, nseq)))
        for l in range(nlayers):
            for bi, b in enumerate(pair):
                if layer_body(b, l, bi):
                    done = True
                    break
            if done:
                break
            if moe_body(pair, l):
                done = True
                break
        if done:
            break
        P.op("dve", lambda e: e.memset(sm[:, 35:36], 0.0), W=ALLMT + EXPACT + [("fn", i) for i in range(8)] + ["fence3"])
        for b in pair:
            norm_phase(xs_dram[b], gfin_b, None, y_dst=y_out[b], xtok=b)

    if SCHEDULE:
        P.schedule()
    P.emit(nc)
    st.close()
    return nc, dbg


def _host_consts(inp):
    f = lambda a: np.ascontiguousarray(np.asarray(a, dtype=np.float32))
    rep = lambda a, n=128: np.ascontiguousarray(np.broadcast_to(np.asarray(a, np.float32)[:, None, :], (a.shape[0], n, a.shape[1])))
    c = {}
    for k in ("w_in", "w_spatial", "w_pool", "w_attn_o", "w_sgu_o", "w_pool_o", "w_out", "w_router",
              "w_gate_e", "w_up_e", "w_down_e"):
        c[k] = f(inp[k])
    c["gmix_b"] = rep(inp["g_mix"])
    c["gffn_b"] = rep(inp["g_ffn"])
    c["gfin_b"] = np.ascontiguousarray(np.broadcast_to(np.asarray(inp["g_final"], np.float32)[None, :], (128, D)))
    c["gq_b"] = rep(inp["g_q"])
    c["gk_b"] = rep(inp["g_k"])
    c["gsgu_b"] = rep(inp["g_sgu"])
    c["bsp_t"] = np.ascontiguousarray(np.asarray(inp["b_spatial"], np.float32).transpose(0, 2, 1))
    c["psc_t"] = np.ascontiguousarray(np.asarray(inp["pool_scale"], np.float32).reshape(L, 2, 128).transpose(0, 2, 1))
    c["ident"] = np.eye(128, dtype=np.float32)
    c["cs"] = _rope_table()
    c["band"] = _pool_bands()
    return c


def kernel(**inputs):
    x = np.asarray(inputs["x"], dtype=np.float32)
    consts = _host_consts(inputs)
    nc, _ = build_program()
    in_maps = []
    for i in range(N_CORES):
        m = dict(consts)
        m["x"] = np.ascontiguousarray(x[i * NSEQ:(i + 1) * NSEQ])
        in_maps.append(m)
    res = run_bass_kernel_spmd(nc, in_maps, core_ids=list(range(N_CORES)))
    out = np.concatenate([np.asarray(r["y"]) for r in res.results], axis=0)
    return out.astype(np.float32)
```

```python
import numpy as np
import concourse.bass as bass
import concourse.mybir as mybir
from concourse.bass_utils import run_bass_kernel_spmd

F32 = mybir.dt.float32
BF16 = mybir.dt.bfloat16
I32 = mybir.dt.int32
U32 = mybir.dt.uint32
ALU = mybir.AluOpType
AF = mybir.ActivationFunctionType
AX = mybir.AxisListType

D = 1024
S = 2048
NT = 16
KD = 8
L = 2
NSEQ = 4
IN_W = 4608
NE = 16
CAP = 256
EPS = 1e-6
POOL_WINDOWS = (2, 4, 8, 16)
N_CORES = 8
SCHEDULE = True
GRP = 4
STRICT = True


class Op:
    __slots__ = ("eng", "fn", "deps", "signal", "val", "is_dma", "sem_slot", "idx", "prev_same_slot", "odeps", "cost")

    def __init__(self, eng, fn, is_dma):
        self.eng = eng
        self.fn = fn
        self.deps = []
        self.odeps = []
        self.cost = None
        self.signal = False
        self.val = 0
        self.is_dma = is_dma
        self.sem_slot = None
        self.prev_same_slot = None


class Prog:
    ENGS = ("pe", "act", "dve", "pool", "sp")
    NDMA_SEMS = {"sp": 12, "pool": 12, "act": 4}

    def __init__(self):
        self.ops = []
        self.last_w = {}
        self.readers = {}
        self.dma_count = {"sp": 0, "pool": 0, "act": 0}
        self.dma_last = {}

    def _add_dep(self, op, dep, kind):
        if dep is None or dep is op:
            return
        op.odeps.append(dep)
        if (not dep.is_dma) and dep.eng == op.eng and not op.is_dma:
            if op.eng == "pe" or (kind != "raw" and not STRICT):
                return
        op.deps.append(dep)
        dep.signal = True

    def op(self, eng, fn, R=(), W=(), dma=False, cost=None):
        o = Op(eng, fn, dma)
        o.cost = cost
        for t in R:
            self._add_dep(o, self.last_w.get(t), "raw")
        for t in W:
            self._add_dep(o, self.last_w.get(t), "waw")
            for r in self.readers.get(t, ()):
                self._add_dep(o, r, "war")
        for t in R:
            lst = self.readers.setdefault(t, [])
            if not dma:
                lst[:] = [r for r in lst if r.is_dma or r.eng != eng]
            lst.append(o)
        for t in W:
            self.last_w[t] = o
            self.readers[t] = []
        if dma:
            o.signal = True
            self.dma_count[eng] += 1
        self.ops.append(o)
        return o

    def dma(self, queue, out, in_, R=(), W=(), cost=None, **kw):
        return self.op(queue, lambda e: e.dma_start(out=out, in_=in_, **kw), R, W, dma=True, cost=cost)

    DEF_COST = {"pe": 160.0, "act": 450.0, "dve": 450.0, "pool": 800.0}

    def schedule(self, window=24):
        ops = self.ops
        for i, o in enumerate(ops):
            o.idx = i
        pend = {e: [o for o in ops if o.eng == e] for e in self.ENGS}
        head = {e: 0 for e in self.ENGS}
        fin = [None] * len(ops)
        etime = {e: 0.0 for e in self.ENGS}
        order = {e: [] for e in self.ENGS}
        done = [False] * len(ops)
        remaining = len(ops)
        SEM = 120.0

        def cand(e):
            lst = pend[e]
            h = head[e]
            while h < len(lst) and done[lst[h].idx]:
                h += 1
            head[e] = h
            best = None
            seen = 0
            i = h
            while i < len(lst) and seen < window:
                o = lst[i]
                i += 1
                if done[o.idx]:
                    continue
                seen += 1
                st = etime[e]
                ok = True
                for d in o.odeps:
                    f = fin[d.idx]
                    if f is None:
                        ok = False
                        break
                    if d.eng != e or d.is_dma:
                        f = f + SEM
                    elif not o.is_dma and not d.is_dma:
                        f = f if (e == "pe") else f + 60.0
                    if f > st:
                        st = f
                if not ok:
                    continue
                if best is None or st < best[0]:
                    best = (st, o)
                    if st <= etime[e]:
                        break
            return best
        cands = {e: cand(e) for e in self.ENGS}
        while remaining:
            be = None
            for e in self.ENGS:
                c = cands[e]
                if c is not None and (be is None or c[0] < cands[be][0] or (c[0] == cands[be][0] and c[1].idx < cands[be][1].idx)):
                    be = e
            assert be is not None, "scheduler deadlock"
            st, o = cands[be]
            if o.is_dma:
                etime[be] = st + 90.0
                fin[o.idx] = st + (o.cost if o.cost is not None else 3000.0)
            else:
                c = o.cost if o.cost is not None else self.DEF_COST[be]
                etime[be] = st + c
                fin[o.idx] = st + c + (150.0 if be == "pe" else 0.0)
            done[o.idx] = True
            order[be].append(o)
            remaining -= 1
            for e in self.ENGS:
                cands[e] = cand(e)
        self.order = order
        self.sim_time = max(etime.values())

    def emit(self, nc):
        if not hasattr(self, "order"):
            self.order = {e: [o for o in self.ops if o.eng == e] for e in self.ENGS}
        per_eng = self.order
        cnt = {e: 0 for e in self.ENGS}
        for e in self.ENGS:
            ndma = 0
            last_slot = {}
            for o in per_eng[e]:
                if o.is_dma:
                    k = self.NDMA_SEMS[e]
                    o.sem_slot = (e, ndma % k)
                    o.val = 16 * (ndma // k + 1)
                    o.prev_same_slot = last_slot.get(o.sem_slot)
                    last_slot[o.sem_slot] = o
                    ndma += 1
                elif o.signal:
                    cnt[e] += 1
                    o.val = cnt[e]
        self.sig_counts = dict(cnt)
        import contextlib
        with contextlib.ExitStack() as st:
            esem = {e: st.enter_context(nc.semaphore("s_" + e)) for e in self.ENGS}
            dsem = {}
            for q, k in self.NDMA_SEMS.items():
                for i in range(k):
                    dsem[(q, i)] = st.enter_context(nc.semaphore("d_%s%d" % (q, i)))
            block = st.enter_context(nc.Block())

            def sem_of(o):
                return dsem[o.sem_slot] if o.is_dma else esem[o.eng]

            def run(engname, eng):
                waited = {}
                tail = {}
                for o in per_eng[engname]:
                    deps = list(o.deps)
                    if o.is_dma and o.prev_same_slot is not None:
                        deps.append(o.prev_same_slot)
                    for d in deps:
                        s = sem_of(d)
                        key = id(s)
                        if waited.get(key, 0) < d.val:
                            eng.wait_ge(s, d.val)
                            waited[key] = d.val
                    ins = o.fn(eng)
                    if o.signal:
                        ins.then_inc(sem_of(o), 16 if o.is_dma else 1)
                    if o.is_dma:
                        tail[id(sem_of(o))] = (sem_of(o), o.val)
                for s, v in tail.values():
                    if waited.get(id(s), 0) < v:
                        eng.wait_ge(s, v)

            @block.tensor
            def _(e):
                run("pe", e)

            @block.scalar
            def _(e):
                run("act", e)

            @block.vector
            def _(e):
                run("dve", e)

            @block.gpsimd
            def _(e):
                run("pool", e)

            @block.sync
            def _(e):
                run("sp", e)


def _rope_table():
    rows = S // 64
    row = np.broadcast_to(np.arange(rows, dtype=np.float32)[:, None], (rows, 64)).reshape(-1)
    col = np.broadcast_to(np.arange(64, dtype=np.float32)[None, :], (rows, 64)).reshape(-1)
    freqs = (10000.0 ** (-np.arange(0, 32, 2, dtype=np.float32) / 32)).astype(np.float32)
    ang = np.stack([row[:, None] * freqs, col[:, None] * freqs], axis=1)
    cs = np.concatenate([np.cos(ang).reshape(S, 32), np.sin(ang).reshape(S, 32)], axis=1)
    return cs.astype(np.float32)


def _pool_bands():
    out = np.zeros((4, 5, 128, 128), np.float32)
    t = np.arange(S)
    for g, w in enumerate(POOL_WINDOWS):
        lo = np.clip(t - w // 2, 0, S - 1)
        hi = np.clip(t + (w - 1 - w // 2), 0, S - 1)
        cnt = (hi - lo + 1).astype(np.float32)

        def blk(ci, cj):
            m = np.zeros((128, 128), np.float32)
            for tt in range(ci * 128, ci * 128 + 128):
                for tp in range(max(lo[tt], cj * 128), min(hi[tt], cj * 128 + 127) + 1):
                    m[tp - cj * 128, tt - ci * 128] += 1.0 / cnt[tt]
                if ci == cj:
                    m[tt - cj * 128, tt - ci * 128] -= 1.0
            return m
        out[g, 0] = blk(5, 4)
        out[g, 1] = blk(5, 6)
        out[g, 2] = blk(5, 5)
        out[g, 3] = blk(0, 0)
        out[g, 4] = blk(15, 15)
    return out


def build_program(nseq=NSEQ, nlayers=L, debug=None, stop_after=None):
    nc = bass.Bass("TRN2", target_bir_lowering=False)
    P = Prog()
    dbg = {}

    def din(name, shape, dt=F32):
        return nc.dram_tensor(name, list(shape), dt, kind="ExternalInput").ap()

    x_in = din("x", [nseq, S, D])
    w_in = din("w_in", [L, D, IN_W])
    w_spatial = din("w_spatial", [L, 4, 128, 128])
    w_pool = din("w_pool", [L, 4, 64, 64])
    w_attn_o = din("w_attn_o", [L, 512, D])
    w_sgu_o = din("w_sgu_o", [L, 256, D])
    w_pool_o = din("w_pool_o", [L, 256, D])
    w_out = din("w_out", [L, D, D])
    w_router = din("w_router", [L, D, NE])
    w_gate_e = din("w_gate_e", [L, NE, D, D])
    w_up_e = din("w_up_e", [L, NE, D, D])
    w_down_e = din("w_down_e", [L, NE, D, D])
    gmix_b = din("gmix_b", [L, 128, D])
    gffn_b = din("gffn_b", [L, 128, D])
    gfin_b = din("gfin_b", [128, D])
    gq_b = din("gq_b", [L, 128, 64])
    gk_b = din("gk_b", [L, 128, 64])
    gsgu_b = din("gsgu_b", [L, 128, 256])
    bsp_t = din("bsp_t", [L, 128, 4])
    psc_t = din("psc_t", [L, 128, 2])
    ident_in = din("ident", [128, 128])
    cs_in = din("cs", [S, 64])
    band_in = din("band", [4, 5, 128, 128])

    y_out = nc.dram_tensor("y", [nseq, S, D], F32, kind="ExternalOutput").ap()
    xs_dram = [nc.dram_tensor("xs_scr%d" % i, [S, D], F32, kind="Internal").ap() for i in range(nseq)]
    h2_dram = [nc.dram_tensor("h2_scr%d" % i, [S, D], BF16, kind="Internal").ap() for i in range(GRP)]

    import contextlib
    st = contextlib.ExitStack()

    def sb(name, shape, dt):
        return st.enter_context(nc.sbuf_tensor(name, list(shape), dt))

    def ps(name, shape, dt):
        return st.enter_context(nc.psum_tensor(name, list(shape), dt))

    arena = sb("arena", [128, 32768], BF16)
    hT = arena[:, 0:16384].rearrange("p (k t) -> p k t", k=KD)
    qT = arena[:, 16384:24576].rearrange("p (j t) -> p j t", j=4)
    kT2 = arena[:, 24576:32768].rearrange("p (g v t) -> p g v t", g=2, v=2)
    xres = arena.bitcast(F32).rearrange("p (c d) -> p c d", c=NT)
    mT = sb("mT", [128, KD, S], BF16)
    wB = mT.ap().rearrange("p k t -> p (k t)")[:, 0:12288].rearrange("p (s k n) -> p s k n", s=3, k=KD)
    sy = sb("sy", [128, 8192], BF16)
    sgT = sy.ap()[:, 0:4096].rearrange("p (j t) -> p j t", j=2)
    yT = sy.ap()[:, 4096:8192].rearrange("p (j t) -> p j t", j=2)
    wD = sb("wD", [128, 2 * KD * 3 * 256], BF16)
    wDv = wD.ap().rearrange("p (b k x n) -> p b k x n", b=2, k=KD, x=3)
    wO = wD.ap()[:, 0:KD * D].rearrange("p (k n) -> p k n", k=KD)
    vaug = wD.ap()[:, 6144:12288].rearrange("p (c g e) -> p c g e", c=NT, g=2)
    NRING = 6
    wexp = [arena[:, i * 8192:(i + 1) * 8192].rearrange("p (k n) -> p k n", k=KD) for i in range(4)]
    wexp.append(sy.ap().rearrange("p (k n) -> p k n", k=KD))
    wexp.append(wD.ap()[:, 0:8192].rearrange("p (k n) -> p k n", k=KD))
    woA = sb("woA", [128, 2, 4, 256], BF16)
    woB = sb("woB", [128, 2, 2, 256], BF16)
    woC = sb("woC", [128, 2, 2, 256], BF16)
    xin = sb("xin", [128, 2, D], F32)
    hb = sb("hb", [128, 2, D], BF16)
    gnb = sb("gnb", [128, D], F32)
    ss = sb("ss", [128, NT], F32)
    sm = sb("sm", [128, 40], F32)
    epsb = sb("epsb", [128, 1], F32)
    ident = sb("identb", [128, 128], BF16)
    identf = sb("identf", [128, 128], F32)
    cs = sb("cs_sb", [128, NT, 64], F32)
    band = sb("band_sb", [128, 4, 5, 128], BF16)
    wsT = sb("wsT", [128, L, 4, 128], BF16)
    wpl2 = sb("wpl2", [128, L, 2, 128], BF16)
    wr = sb("wr", [128, L, KD, NE], BF16)
    gq = sb("gq", [128, L, 64], F32)
    gk = sb("gk", [128, L, 64], F32)
    gsg = sb("gsg", [128, L, 256], F32)
    bsp = sb("bsp", [128, L, 4], F32)
    psc = sb("psc", [128, L, 2], F32)
    vnb = sb("vnb", [128, 2, 256], BF16)
    sgtok = sb("sgtok", [128, 256], BF16)
    dtok = sb("dtok", [128, 256], BF16)
    ptok = sb("ptok", [128, NT, 256], BF16)
    dTc = sb("dTc", [128, 2, 128], BF16)
    aff = sb("aff", [128, (GRP + 1) // 2, NT, 2 * NE], F32)
    tvals = sb("tvals", [112, CAP], F32)
    tidx = sb("tidx", [112, CAP], U32)
    tidxf = sb("tidxf", [112, CAP], F32)
    idxT = sb("idxT", [128, 2, 112], I32)
    valsT = sb("valsT", [128, 2, 112], F32)
    sab = sb("sab", [128, 2, 512], F32)
    tmp = sb("tmp", [128, 4096], BF16)

    def tslot(i, n=1, dt=BF16):
        v = tmp.ap()[:, i * 512:(i + n) * 512]
        return v if dt == BF16 else v.bitcast(dt)

    def ttok(i, n=1):
        return [("tmp", j) for j in range(i, i + n)]
    wsp_raw = tslot(0, 1).rearrange("p (g q) -> p g q", g=4)
    qn = tslot(0, 2, F32); qr = tslot(2, 2, F32); sqf = tslot(4, 2, F32); qtok = tslot(6); utok = tslot(7, 1, F32)
    pT = [tslot(i) for i in range(3)]
    otok = tslot(3, 4).rearrange("p (c n) -> p c n", c=4)
    sga = [tslot(i) for i in range(3)]
    mprod = [tslot(3, 2, F32), tslot(5, 2, F32)]

    pbank = [ps("pb%d" % i, [128, 512], F32) for i in range(8)]

    def pb_bf(i):
        return pbank[i].ap().bitcast(BF16)

    P.dma("pool", ident[:], ident_in, W=["ident"])
    P.dma("sp", identf[:], ident_in, W=["identf"])
    P.dma("sp", cs[:], cs_in.rearrange("(c p) f -> p c f", p=128), W=["cs"])
    P.dma("pool", band[:], band_in.rearrange("g v a b -> a g v b"), W=["band"])
    P.op("dve", lambda e: e.memset(wpl2[:], 0.0), W=["wpl2"])
    P.op("dve", lambda e: e.memset(epsb[:], EPS), W=["epsb"])
    for l in range(L):
        for g in range(4):
            gg = g % 2
            P.dma("pool", wpl2[gg * 64:(gg + 1) * 64, l, g // 2, gg * 64:(gg + 1) * 64], w_pool[l, g], W=["wpl2"], R=[])
        P.dma("pool", wr[:, l], w_router[l].rearrange("(k p) e -> p k e", p=128), W=["wr"])
        P.dma("sp", gq[:, l], gq_b[l], W=["gq"])
        P.dma("sp", gk[:, l], gk_b[l], W=["gk"])
        P.dma("sp", gsg[:, l], gsgu_b[l], W=["gsg"])
        P.dma("sp", bsp[:, l], bsp_t[l], W=["bsp"])
        P.dma("sp", psc[:, l], psc_t[l], W=["psc"])
        P.dma("pool", wsp_raw, w_spatial[l].rearrange("g p q -> p g q"), W=["wsp_raw"] + ttok(0))
        tp = pb_bf(0)
        for g in range(4):
            P.op("pe", lambda e, g=g, tp=tp: e.transpose(tp[:, g * 128:(g + 1) * 128], wsp_raw[:, g, :], ident[:]),
                 R=["wsp_raw", "ident"] + ttok(0), W=[("pb", 0)])
        P.op("dve", lambda e, l=l, tp=tp: e.tensor_copy(out=wsT[:, l], in_=tp[:, 0:512].rearrange("p (g q) -> p g q", g=4)),
             R=[("pb", 0)], W=["wsT"])

    def rsqrt_eps(ap, toks):
        P.op("act", lambda e: e.activation(out=ap, in_=ap, func=AF.Sqrt, bias=epsb[0:ap.shape[0], 0:1], scale=1.0), R=toks + ["epsb"], W=toks)
        P.op("dve", lambda e: e.reciprocal(out=ap, in_=ap), R=toks, W=toks)

    def norm_phase(x_src_dram, g_dram, to_hT, from_xres=False, h2_dst=None, y_dst=None, hT_tok="hT", pbase=6, xtok=0, h2i=0):
        P.dma("sp", gnb[:], g_dram, W=["gnb"])
        if y_dst is not None:
            fnb = mT.ap().rearrange("p k t -> p (k t)").bitcast(F32).rearrange("p (i d) -> p i d", i=8)
        for c in range(NT):
            b = c % 2
            if y_dst is not None:
                bi_, bo_ = c % 4, 4 + c % 4
                P.dma("sp", fnb[:, bi_], x_src_dram[c * 128:(c + 1) * 128, :], R=[("xs_dram", xtok), ("xsd", xtok, c)], W=[("fn", bi_)])
                P.op("act", lambda e, c=c, bi_=bi_, bo_=bo_: e.activation(out=fnb[:, bo_], in_=fnb[:, bi_], func=AF.Square, scale=1.0 / 32.0, accum_out=ss[:, c:c + 1]),
                     R=[("fn", bi_)], W=[("fn", bo_), ("ss", c)])
                rsqrt_eps(ss[:, c:c + 1], [("ss", c)])
                P.op("dve", lambda e, c=c, bi_=bi_, bo_=bo_: e.scalar_tensor_tensor(out=fnb[:, bo_], in0=fnb[:, bi_], scalar=ss[:, c:c + 1],
                                                                                     in1=gnb[:], op0=ALU.mult, op1=ALU.mult),
                     R=[("fn", bi_), ("fn", bo_), ("ss", c), "gnb"], W=[("fn", bo_)])
                P.dma("sp", y_dst[c * 128:(c + 1) * 128, :], fnb[:, bo_], R=[("fn", bo_)], W=[("y_out", xtok, c)])
                continue
            if from_xres:
                src = xres[:, c, :]
                srcR = [("xres", c)]
            else:
                P.dma("sp", xin[:, b], x_src_dram[c * 128:(c + 1) * 128, :], R=[("xs_dram", xtok), ("xsd", xtok, c)], W=[("xin", b)])
                src = xin[:, b]
                srcR = [("xin", b)]
            P.op("act", lambda e, src=src, c=c, b=b: e.activation(out=hb[:, b], in_=src, func=AF.Square, scale=1.0 / 32.0, accum_out=ss[:, c:c + 1]),
                 R=srcR, W=[("hb", b), ("ss", c)])
            rsqrt_eps(ss[:, c:c + 1], [("ss", c)])
            P.op("dve", lambda e, src=src, c=c, b=b: e.scalar_tensor_tensor(out=hb[:, b], in0=src, scalar=ss[:, c:c + 1],
                                                                             in1=gnb[:], op0=ALU.mult, op1=ALU.mult),
                 R=srcR + [("ss", c), "gnb"], W=[("hb", b)])
            if h2_dst is not None:
                P.dma("sp", h2_dst[c * 128:(c + 1) * 128, :], hb[:, b], R=[("hb", b)], W=[("h2d", h2i, c)])
            bk = pbase + (c % 2)
            tp = pb_bf(bk)
            for k in range(KD):
                P.op("pe", lambda e, k=k, b=b, tp=tp: e.transpose(tp[:, k * 128:(k + 1) * 128], hb[:, b, k * 128:(k + 1) * 128], ident[:]),
                     R=[("hb", b), "ident"], W=[("pb", bk)])
            P.op("act", lambda e, c=c, tp=tp: e.activation(out=to_hT[:, :, c * 128:(c + 1) * 128],
                                                           in_=tp[:, 0:1024].rearrange("p (k t) -> p k t", k=KD), func=AF.Copy),
                 R=[("pb", bk)], W=[(hT_tok, c)])

    def headnorm_rope(src_ps, nh, gain, c, dst_bf, Rsrc, Wdst):
        w = nh * 64
        P.op("act", lambda e: e.activation(out=sqf[:, 0:w], in_=src_ps, func=AF.Square, scale=0.125), R=Rsrc, W=ttok(4, 2))
        P.op("dve", lambda e: e.tensor_reduce(out=sm[:, 0:nh], in_=sqf[:, 0:w].rearrange("p (h d) -> p h d", h=nh), axis=AX.X, op=ALU.add),
             R=ttok(4, 2), W=["sm"])
        rsqrt_eps(sm[:, 0:nh], ["sm"])
        P.op("dve", lambda e: e.tensor_tensor(out=qn[:, 0:w].rearrange("p (h d) -> p h d", h=nh),
                                              in0=src_ps.rearrange("p (h d) -> p h d", h=nh),
                                              in1=sm[:, 0:nh].unsqueeze(2).to_broadcast([128, nh, 64]), op=ALU.mult),
             R=Rsrc + ["sm"], W=ttok(0, 2))
        P.op("dve", lambda e: e.tensor_tensor(out=qn[:, 0:w].rearrange("p (h d) -> p h d", h=nh),
                                              in0=qn[:, 0:w].rearrange("p (h d) -> p h d", h=nh),
                                              in1=gain.unsqueeze(1).to_broadcast([128, nh, 64]), op=ALU.mult),
             R=ttok(0, 2) + ["gq", "gk"], W=ttok(0, 2))
        m = nh * 2
        qv = qn[:, 0:w].rearrange("p (m two f) -> p m two f", m=m, two=2)
        rv = qr[:, 0:w].rearrange("p (m two f) -> p m two f", m=m, two=2)
        dv = dst_bf.rearrange("p (m two f) -> p m two f", m=m, two=2)
        cosb = cs[:, c, 0:32].rearrange("p (a f) -> p a f", a=2)
        sinb = cs[:, c, 32:64].rearrange("p (a f) -> p a f", a=2)

        def bc(t):
            return t.unsqueeze(1).to_broadcast([128, nh, 2, 16])
        x1 = qn[:, 0:w].rearrange("p (h a two f) -> p h a two f", h=nh, a=2, two=2)
        r_ = qr[:, 0:w].rearrange("p (h a two f) -> p h a two f", h=nh, a=2, two=2)
        d_ = dst_bf.rearrange("p (h a two f) -> p h a two f", h=nh, a=2, two=2)
        P.op("dve", lambda e: e.tensor_tensor(out=r_[:, :, :, 0, :], in0=x1[:, :, :, 0, :], in1=bc(cosb), op=ALU.mult), R=ttok(0, 2) + ["cs"], W=ttok(2, 2))
        P.op("dve", lambda e: e.tensor_tensor(out=r_[:, :, :, 1, :], in0=x1[:, :, :, 1, :], in1=bc(sinb), op=ALU.mult), R=ttok(0, 2) + ["cs"], W=ttok(2, 2))
        P.op("dve", lambda e: e.tensor_tensor(out=d_[:, :, :, 0, :], in0=r_[:, :, :, 0, :], in1=r_[:, :, :, 1, :], op=ALU.subtract), R=ttok(2, 2), W=Wdst)
        P.op("dve", lambda e: e.tensor_tensor(out=r_[:, :, :, 0, :], in0=x1[:, :, :, 1, :], in1=bc(cosb), op=ALU.mult), R=ttok(0, 2) + ["cs"], W=ttok(2, 2))
        P.op("dve", lambda e: e.tensor_tensor(out=r_[:, :, :, 1, :], in0=x1[:, :, :, 0, :], in1=bc(sinb), op=ALU.mult), R=ttok(0, 2) + ["cs"], W=ttok(2, 2))
        P.op("dve", lambda e: e.tensor_tensor(out=d_[:, :, :, 1, :], in0=r_[:, :, :, 0, :], in1=r_[:, :, :, 1, :], op=ALU.add), R=ttok(2, 2), W=Wdst)

    def dbg_out(name, ap_sb, shape, dt=F32, R=()):
        t = nc.dram_tensor("dbg_" + name, list(shape), dt, kind="ExternalOutput").ap()
        if hasattr(ap_sb, "ap") and callable(getattr(ap_sb, "ap")):
            ap_sb = ap_sb.ap()
        P.dma("sp", t, ap_sb, R=list(R), W=["dbg_" + name])
        dbg[name] = t

    ALLHT = [("hT", c) for c in range(NT)]
    ALLQT = [("qT", c) for c in range(NT)]
    ALLMT = [("mT", c) for c in range(NT)]
    ALLX = [("xres", c) for c in range(NT)]
    ALLQT = [("qT", c, j, hp) for c in range(NT) for j in range(4) for hp in range(2)]
    VAUGT = [("vaug", c) for c in range(NT)] + ["vaug_ones"]
    MIXT = ALLHT + ALLQT + [("kT2", c) for c in range(NT)] + [("kT2b", c) for c in range(NT)] + ["kT2_zero"] + VAUGT
    WEXPT = [("wexp", i) for i in range(6)]
    SYT = [("sgT", c) for c in range(NT)] + [("yT", c) for c in range(NT)]
    WDT = [("wD", bb, x_) for bb in range(2) for x_ in range(3)] + ["wO_a", "wO_b"]
    WBT = [("wB", s) for s in range(3)]
    SUBB = [("pb", 3), ("pb", 4), ("pb", 5), ("pb", 3), ("pb", 3), ("pb", 4), ("pb", 4), ("pb", 5), ("pb", 5), "sq_", "qr_", ("utok_", 0), ("utok_", 1)]
    XSD = [[("xsd", b_, c) for c in range(NT)] for b_ in range(nseq)]
    H2D = [[("h2d", i_, c) for c in range(NT)] for i_ in range(GRP)]
    EXPACT = [("xg", i) for i in range(4)] + [("oe", i) for i in range(4)] + ["xsT", "gT"]
    done = False
    def layer_body(b, l, bi):
        if True:
            x_src = x_in[b] if l == 0 else xs_dram[b]
            P.op("dve", lambda e: e.memset(sm[:, 32:33], 0.0), W=MIXT + ALLX + WEXPT + ALLMT + WBT + EXPACT + SYT + WDT + SUBB + [("fn", i) for i in range(8)] + ["fence"])
            if stop_after == "A00" and l == nlayers - 1:
                dbg_out("xs", xs_dram[0], [S, D], F32, R=[("xs_dram", 0)] + XSD[0])
                return True
            P.op("pool", lambda e: e.memset(kT2[64:128, :, 0, :], 0.0), W=["kT2_zero"])
            P.op("pool", lambda e: e.memset(kT2[0:64, :, 1, :], 0.0), W=["kT2_zero"])
            P.op("dve", lambda e: e.memset(vaug[:, :, :, 0:64], 1.0), W=["vaug_ones"])
            P.op("dve", lambda e: e.memset(vaug[:, :, :, 128:192], 1.0), W=["vaug_ones"])
            for s in range(3):
                P.dma("pool", wB[:, s], w_in[l][:, s * 512:(s + 1) * 512].rearrange("(k p) n -> p k n", p=128),
                      W=ALLMT + [("wB", s)] if s == 0 else [("wB", s)], R=[])
            if stop_after == "A0" and l == nlayers - 1:
                dbg_out("xs", xs_dram[0], [S, D], F32, R=[("xs_dram", 0)] + XSD[0])
                return True
            norm_phase(x_src, gmix_b[l], hT, xtok=b)
            if stop_after == "A" and l == nlayers - 1:
                dbg_out("hT", hT, [128, KD, S], BF16, R=ALLHT)
                if l > 0:
                    dbg_out("xs", xs_dram[0], [S, D], F32, R=[("xs_dram", 0)] + XSD[0])
                return True
            mtail = mT.ap().rearrange("p k t -> p (k t)")[:, 12288:16384]
            qkb = [tslot(0, 3, F32)[:, 0:640], tslot(3, 3, F32)[:, 0:640]]
            qkt = [ttok(0, 3), ttok(3, 3)]
            qtok2 = tslot(6, 2)[:, 0:640]
            sq_ = mtail[:, 0:1280].bitcast(F32)
            qr_ = mtail[:, 1280:2560].bitcast(F32)
            utk = [mtail[:, 2560:3072].bitcast(F32), mtail[:, 3072:3584].bitcast(F32)]

            def pool_chunk(c):
                for g in range(4):
                    terms = []
                    if c > 0:
                        terms.append((c - 1, 0))
                    terms.append((c, 3 if c == 0 else (4 if c == NT - 1 else 2)))
                    if c < NT - 1:
                        terms.append((c + 1, 1))
                    for i, (cj, v) in enumerate(terms):
                        P.op("pe", lambda e, g=g, cj=cj, v=v, i=i, n=len(terms): e.matmul(
                            pbank[5][:, 256 + g * 64:256 + (g + 1) * 64], lhsT=band[:, g, v, :], rhs=ptok[:, cj, g * 64:(g + 1) * 64],
                            start=(i == 0), stop=(i == n - 1)), R=[("ptok", cj), "band"], W=[("pb", 5)])
                P.op("act", lambda e: e.activation(out=dtok[:, :], in_=pbank[5][:, 256:512], func=AF.Copy), R=[("pb", 5)], W=["dtok"])
                tpd = pb_bf(3)
                for j in range(2):
                    P.op("pe", lambda e, j=j: e.transpose(tpd[:, 512 + j * 128:512 + (j + 1) * 128], dtok[:, j * 128:(j + 1) * 128], ident[:]),
                         R=["dtok", "ident"], W=[("pb", 3)])
                P.op("act", lambda e: e.activation(out=dTc[:, :, :], in_=tpd[:, 512:768].rearrange("p (j t) -> p j t", j=2), func=AF.Copy),
                     R=[("pb", 3)], W=["dTc"])
                for j in range(2):
                    P.op("pe", lambda e, j=j: e.matmul(pbank[4][:, 256 + j * 128:256 + (j + 1) * 128], lhsT=wpl2[:, l, j, :], rhs=dTc[:, j, :],
                                                       start=True, stop=True), R=["dTc", "wpl2"], W=[("pb", 4)])
                P.op("dve", lambda e: e.tensor_tensor(out=yT[:, :, c * 128:(c + 1) * 128], in0=pbank[4][:, 256:512].rearrange("p (j t) -> p j t", j=2),
                                                      in1=psc[:, l, :].unsqueeze(2).to_broadcast([128, 2, 128]), op=ALU.mult),
                     R=[("pb", 4), "psc"], W=[("yT", c)])

            def projB1(c):
                cb = c % 2
                qk = qkb[cb]
                QK = qkt[cb]
                vn_ = vnb[:, cb, :]
                VN = [("vn", cb)]
                utok_ = utk[cb]
                UT = [("utok_", cb)]
                for s_ in range(3):
                    for k in range(KD):
                        P.op("pe", lambda e, s_=s_, k=k: e.matmul(pbank[s_][:, :], lhsT=hT[:, k, c * 128:(c + 1) * 128], rhs=wB[:, s_, k, :],
                                                                  start=(k == 0), stop=(k == KD - 1)),
                             R=[("hT", c), ("wB", s_)], W=[("pb", s_)])
                P.op("act", lambda e: e.activation(out=qk[:, 0:512], in_=pbank[0][:, :], func=AF.Copy), R=[("pb", 0)], W=QK)
                P.op("act", lambda e: e.activation(out=qk[:, 512:640], in_=pbank[1][:, 0:128], func=AF.Copy), R=[("pb", 1)], W=QK)
                P.op("act", lambda e: e.activation(out=sq_[:, :], in_=qk[:, :], func=AF.Square, scale=0.125), R=QK, W=["sq_"])
                P.op("act", lambda e: e.activation(out=vaug[:, c, :, 64:128], in_=pbank[1][:, 128:256].rearrange("p (g d) -> p g d", g=2), func=AF.Copy),
                     R=[("pb", 1)], W=[("vaug", c)])
                P.op("act", lambda e: e.activation(out=utok_[:, :], in_=pbank[1][:, 256:512], func=AF.Copy), R=[("pb", 1)], W=UT)
                P.op("dve", lambda e: e.tensor_reduce(out=sm[:, 0:10], in_=sq_[:, :].rearrange("p (h d) -> p h d", h=10), axis=AX.X, op=ALU.add),
                     R=["sq_"], W=["sm"])
                rsqrt_eps(sm[:, 0:10], ["sm"])
                P.op("dve", lambda e: e.tensor_tensor(out=qk[:, :].rearrange("p (h d) -> p h d", h=10),
                                                      in0=qk[:, :].rearrange("p (h d) -> p h d", h=10),
                                                      in1=sm[:, 0:10].unsqueeze(2).to_broadcast([128, 10, 64]), op=ALU.mult),
                     R=QK + ["sm"], W=QK)
                P.op("act", lambda e: e.activation(out=vn_, in_=pbank[2][:, 0:256], func=AF.Square, scale=1.0 / 16.0, accum_out=sm[:, 16:17]),
                     R=[("pb", 2)], W=VN + ["sm2"])
                rsqrt_eps(sm[:, 16:17], ["sm2"])
                P.op("dve", lambda e: e.scalar_tensor_tensor(out=vn_, in0=pbank[2][:, 0:256], scalar=sm[:, 16:17], in1=gsg[:, l, :],
                                                             op0=ALU.mult, op1=ALU.mult), R=[("pb", 2), "sm2", "gsg"], W=VN)
                P.op("act", lambda e: e.activation(out=ptok[:, c, :], in_=pbank[2][:, 256:512], func=AF.Copy), R=[("pb", 2)], W=[("ptok", c)])
            def projB2(c):
                cb = c % 2
                qk = qkb[cb]
                QK = qkt[cb]
                vn_ = vnb[:, cb, :]
                VN = [("vn", cb)]
                utok_ = utk[cb]
                UT = [("utok_", cb)]
                P.op("dve", lambda e: e.tensor_tensor(out=qk[:, 0:512].rearrange("p (h d) -> p h d", h=8),
                                                       in0=qk[:, 0:512].rearrange("p (h d) -> p h d", h=8),
                                                       in1=gq[:, l, :].unsqueeze(1).to_broadcast([128, 8, 64]), op=ALU.mult), R=QK + ["gq"], W=QK)
                P.op("dve", lambda e: e.tensor_tensor(out=qk[:, 512:640].rearrange("p (h d) -> p h d", h=2),
                                                       in0=qk[:, 512:640].rearrange("p (h d) -> p h d", h=2),
                                                       in1=gk[:, l, :].unsqueeze(1).to_broadcast([128, 2, 64]), op=ALU.mult), R=QK + ["gk"], W=QK)
                x1 = qk[:, :].rearrange("p (h a two f) -> p h a two f", h=10, a=2, two=2)
                r_ = qr_[:, :].rearrange("p (h a two f) -> p h a two f", h=10, a=2, two=2)
                d_ = qtok2.rearrange("p (h a two f) -> p h a two f", h=10, a=2, two=2)
                cosb = cs[:, c, 0:32].rearrange("p (a f) -> p a f", a=2).unsqueeze(1).to_broadcast([128, 10, 2, 16])
                sinb = cs[:, c, 32:64].rearrange("p (a f) -> p a f", a=2).unsqueeze(1).to_broadcast([128, 10, 2, 16])
                QT2 = ttok(6, 2)
                P.op("pool", lambda e: e.tensor_tensor(out=r_[:, :, :, 0, :], in0=x1[:, :, :, 0, :], in1=cosb, op=ALU.mult), R=QK + ["cs"], W=["qr_"])
                P.op("pool", lambda e: e.tensor_tensor(out=r_[:, :, :, 1, :], in0=x1[:, :, :, 1, :], in1=sinb, op=ALU.mult), R=QK + ["cs"], W=["qr_"])
                P.op("pool", lambda e: e.tensor_tensor(out=d_[:, :, :, 0, :], in0=r_[:, :, :, 0, :], in1=r_[:, :, :, 1, :], op=ALU.subtract), R=["qr_"], W=QT2)
                P.op("pool", lambda e: e.tensor_tensor(out=r_[:, :, :, 0, :], in0=x1[:, :, :, 1, :], in1=cosb, op=ALU.mult), R=QK + ["cs"], W=["qr_"])
                P.op("pool", lambda e: e.tensor_tensor(out=r_[:, :, :, 1, :], in0=x1[:, :, :, 0, :], in1=sinb, op=ALU.mult), R=QK + ["cs"], W=["qr_"])
                P.op("pool", lambda e: e.tensor_tensor(out=d_[:, :, :, 1, :], in0=r_[:, :, :, 0, :], in1=r_[:, :, :, 1, :], op=ALU.add), R=["qr_"], W=QT2)
                tp = pb_bf(3)
                for j in range(4):
                    P.op("pe", lambda e, j=j: e.transpose(tp[:, j * 128:(j + 1) * 128], qtok2[:, j * 128:(j + 1) * 128], ident[:]),
                         R=QT2 + ["ident"], W=[("pb", 3)])
                P.op("act", lambda e: e.activation(out=qT[:, :, c * 128:(c + 1) * 128],
                                                   in_=tp[:, 0:512].rearrange("p (j t) -> p j t", j=4), func=AF.Copy),
                     R=[("pb", 3)], W=[("qT", c, j_, hp_) for j_ in range(4) for hp_ in range(2)])
                tpk = pb_bf(4)
                for g in range(2):
                    for hh in range(2):
                        P.op("pe", lambda e, g=g, hh=hh: e.transpose(
                            tpk[hh * 64:(hh + 1) * 64, g * 128:(g + 1) * 128], qtok2[:, 512 + g * 64:512 + (g + 1) * 64], ident[:]),
                            R=QT2 + ["ident"], W=[("pb", 4)])
                P.op("act", lambda e: e.activation(out=kT2[0:64, :, 0, c * 128:(c + 1) * 128],
                                                   in_=tpk[0:64, 0:256].rearrange("p (g t) -> p g t", g=2), func=AF.Copy),
                     R=[("pb", 4)], W=[("kT2", c)])
                P.op("dve", lambda e: e.tensor_copy(out=kT2[64:128, :, 1, c * 128:(c + 1) * 128],
                                                    in_=tpk[64:128, 0:256].rearrange("p (g t) -> p g t", g=2)),
                     R=[("pb", 4)], W=[("kT2b", c)])
                for g in range(4):
                    P.op("pe", lambda e, g=g: e.matmul(pbank[5][:, g * 64:(g + 1) * 64], lhsT=wsT[:, l, g, :], rhs=vn_[:, g * 64:(g + 1) * 64],
                                                       start=True, stop=True), R=VN + ["wsT"], W=[("pb", 5)])
                P.op("dve", lambda e: e.tensor_tensor(out=sgtok[:, :].rearrange("p (g d) -> p g d", g=4),
                                                      in0=pbank[5][:, 0:256].rearrange("p (g d) -> p g d", g=4),
                                                      in1=bsp[:, l, :].unsqueeze(2).to_broadcast([128, 4, 64]), op=ALU.add),
                     R=[("pb", 5), "bsp"], W=["sgtok"])
                P.op("pool", lambda e: e.tensor_tensor(out=sgtok[:, :], in0=sgtok[:, :], in1=utok_[:, :], op=ALU.mult), R=["sgtok"] + UT, W=["sgtok"])
                tps = pb_bf(4)
                for j in range(2):
                    P.op("pe", lambda e, j=j: e.transpose(tps[:, 256 + j * 128:256 + (j + 1) * 128], sgtok[:, j * 128:(j + 1) * 128], ident[:]),
                         R=["sgtok", "ident"], W=[("pb", 4)])
                P.op("act", lambda e: e.activation(out=sgT[:, :, c * 128:(c + 1) * 128],
                                                   in_=tps[:, 256:512].rearrange("p (j t) -> p j t", j=2), func=AF.Copy),
                     R=[("pb", 4)], W=[("sgT", c)])

            projB1(0)
            for c in range(NT):
                if c + 1 < NT:
                    projB1(c + 1)
                projB2(c)
                if c >= 1:
                    pool_chunk(c - 1)
            pool_chunk(NT - 1)
            P.op("dve", lambda e: e.memset(sm[:, 34:35], 0.0),
                 W=[("pb", 3), ("pb", 4), ("pb", 5), ("pb", 3), ("pb", 3), ("pb", 4), ("pb", 4), ("pb", 5), ("pb", 5), "sq_", "qr_", ("utok_", 0), ("utok_", 1), "fenceB"])
            if stop_after == "B" and l == nlayers - 1:
                dbg_out("qT", qT, [128, 4, S], BF16, R=ALLQT)
                dbg_out("kT2", kT2, [128, 2, 2, S], BF16, R=[("kT2", c) for c in range(NT)] + [("kT2b", c) for c in range(NT)] + ["kT2_zero"])
                dbg_out("vaug", vaug, [128, NT, 2, 192], BF16, R=[("vaug", c) for c in range(NT)] + ["vaug_ones"])
                dbg_out("sgT", sgT, [128, 2, S], BF16, R=[("sgT", c) for c in range(NT)])
                dbg_out("yT", yT, [128, 2, S], BF16, R=[("yT", c) for c in range(NT)])
                return True
            items = [(qb, h, s_) for qb in range(4) for h in range(8) for s_ in range(NT)]
            recb = tslot(3, 2, F32)
            RECT = ttok(3, 2)

            def qk_exp(i):
                qb, h, s_ = items[i]
                g = h // 4
                hp = h % 2
                ho = hp * 64
                j = h // 2
                sbk = i % 3
                P.op("pe", lambda e: e.matmul(
                    pbank[sbk][:, :], lhsT=kT2[:, g, hp, s_ * 128:(s_ + 1) * 128], rhs=qT[:, j, qb * 512:(qb + 1) * 512],
                    start=True, stop=True),
                    R=[("kT2", s_), ("kT2b", s_), "kT2_zero"] + [("qT", qb * 4 + i_, j, hp_) for i_ in range(4) for hp_ in range(2)],
                    W=[("pb", sbk)], cost=230.0)
                P.op("act", lambda e: e.activation(out=pT[sbk], in_=pbank[sbk][:, :], func=AF.Exp, scale=0.125),
                     R=[("pb", sbk)], W=ttok(sbk), cost=560.0)

            def pv(i):
                qb, h, s_ = items[i]
                g = h // 4
                hp = h % 2
                j = h // 2
                sbk = i % 3
                pob = 3 + (h % 2)
                win = slice(64, 192) if hp == 0 else slice(0, 128)
                P.op("pe", lambda e: e.matmul(pbank[pob][:, :], lhsT=vaug[:, s_, g, win], rhs=pT[sbk],
                                              start=(s_ == 0), stop=(s_ == NT - 1)),
                     R=ttok(sbk) + [("vaug", s_), "vaug_ones"], W=[("pb", pob)], cost=230.0)
                if s_ == NT - 1:
                    op_ = slice(0, 64) if hp == 0 else slice(64, 128)
                    dp_ = slice(64, 128) if hp == 0 else slice(0, 64)
                    P.op("dve", lambda e: e.reciprocal(out=recb[dp_, :], in_=pbank[pob][dp_, :]), R=[("pb", pob)], W=RECT, cost=600.0)
                    P.op("dve", lambda e: e.tensor_tensor(out=qT[op_, j, qb * 512:(qb + 1) * 512], in0=pbank[pob][op_, :], in1=recb[dp_, :], op=ALU.mult),
                         R=[("pb", pob)] + RECT, W=[("qT", qb * 4 + i_, j, hp) for i_ in range(4)], cost=600.0)
            LOOK = 2
            for i in range(min(LOOK, len(items))):
                qk_exp(i)
            for i in range(len(items)):
                if i + LOOK < len(items):
                    qk_exp(i + LOOK)
                pv(i)
            if stop_after == "C" and l == nlayers - 1:
                dbg_out("oT", qT, [128, 4, S], BF16, R=ALLQT)
                return True
            P.op("dve", lambda e: e.memset(sm[:, 32:33], 0.0), W=ALLMT + WBT + ["sq_", "qr_", ("utok_", 0), ("utok_", 1), "fence"])

            def load_D(ns):
                buf = ns % 2
                for x_ in range(3):
                    c0 = 1536 + x_ * 1024 + ns * 256
                    P.dma("pool", wDv[:, buf, :, x_, :], w_in[l][:, c0:c0 + 256].rearrange("(k p) n -> p k n", p=128),
                          W=[("wD", buf, x_)] + (VAUGT if buf == 1 else []))
                P.dma("pool", woA[:, buf], w_attn_o[l][:, ns * 256:(ns + 1) * 256].rearrange("(j p) n -> p j n", p=128), W=[("woA", buf)])
                P.dma("pool", woB[:, buf], w_sgu_o[l][:, ns * 256:(ns + 1) * 256].rearrange("(j p) n -> p j n", p=128), W=[("woB", buf)])
                P.dma("pool", woC[:, buf], w_pool_o[l][:, ns * 256:(ns + 1) * 256].rearrange("(j p) n -> p j n", p=128), W=[("woC", buf)])
            load_D(0)
            for ns in range(4):
                buf = ns % 2
                if ns + 1 < 4:
                    load_D(ns + 1)
                for tb in range(4):
                    tsl = slice(tb * 512, (tb + 1) * 512)
                    tt = [tb * 4 + i for i in range(4)]
                    for nn in range(2):
                        n = ns * 2 + nn
                        nsl = slice(nn * 128, (nn + 1) * 128)
                        for x_ in range(3):
                            for k in range(KD):
                                P.op("pe", lambda e, x_=x_, k=k, buf=buf, nsl=nsl, tsl=tsl: e.matmul(
                                    pbank[x_][:, :], lhsT=wDv[:, buf, k, x_, nsl], rhs=hT[:, k, tsl], start=(k == 0), stop=(k == KD - 1)),
                                    R=[("wD", buf, x_)] + [("hT", t) for t in tt], W=[("pb", x_)])
                        for j in range(4):
                            P.op("pe", lambda e, j=j, buf=buf, nsl=nsl, tsl=tsl: e.matmul(
                                pbank[3][:, :], lhsT=woA[:, buf, j, nsl], rhs=qT[:, j, tsl], start=(j == 0), stop=(j == 3)),
                                R=[("woA", buf)] + [("qT", t, j, hp_) for t in tt for hp_ in range(2)], W=[("pb", 3)])
                        for j in range(2):
                            P.op("pe", lambda e, j=j, buf=buf, nsl=nsl, tsl=tsl: e.matmul(
                                pbank[4][:, :], lhsT=woB[:, buf, j, nsl], rhs=sgT[:, j, tsl], start=(j == 0), stop=(j == 1)),
                                R=[("woB", buf)] + [("sgT", t) for t in tt], W=[("pb", 4)])
                        for j in range(2):
                            P.op("pe", lambda e, j=j, buf=buf, nsl=nsl, tsl=tsl: e.matmul(
                                pbank[5][:, :], lhsT=woC[:, buf, j, nsl], rhs=yT[:, j, tsl], start=(j == 0), stop=(j == 1)),
                                R=[("woC", buf)] + [("yT", t) for t in tt], W=[("pb", 5)])
                        for x_ in range(3):
                            P.op("act", lambda e, x_=x_: e.activation(out=sga[x_], in_=pbank[x_][:, :], func=AF.Sigmoid),
                                 R=[("pb", x_)], W=ttok(x_))
                        P.op("dve", lambda e: e.tensor_tensor(out=mprod[0], in0=pbank[3][:, :], in1=sga[0], op=ALU.mult), R=[("pb", 3)] + ttok(0), W=ttok(3, 2))
                        P.op("dve", lambda e: e.tensor_tensor(out=mprod[1], in0=pbank[4][:, :], in1=sga[1], op=ALU.mult), R=[("pb", 4)] + ttok(1), W=ttok(5, 2))
                        P.op("pool", lambda e: e.tensor_tensor(out=mprod[0], in0=mprod[0], in1=mprod[1], op=ALU.add), R=ttok(3, 4), W=ttok(3, 2))
                        P.op("dve", lambda e: e.tensor_tensor(out=mprod[1], in0=pbank[5][:, :], in1=sga[2], op=ALU.mult), R=[("pb", 5)] + ttok(2), W=ttok(5, 2))
                        P.op("pool", lambda e, n=n, tsl=tsl: e.tensor_tensor(out=mT[:, n, tsl], in0=mprod[0], in1=mprod[1], op=ALU.add),
                             R=ttok(3, 4), W=[("mT", t) for t in tt])
            if stop_after == "D" and l == nlayers - 1:
                dbg_out("mT", mT, [128, KD, S], BF16, R=ALLMT)
                return True
            w_out_v = w_out[l].rearrange("(k p) n -> p k n", p=128)
            P.dma("pool", wO[:, 0:6, :], w_out_v[:, 0:6, :], W=[("wD", 0, x_) for x_ in range(3)] + ["wO_a"])
            P.dma("pool", wO[:, 6:8, :], w_out_v[:, 6:8, :], W=[("wD", 1, x_) for x_ in range(3)] + VAUGT + ["wO_b"])
            P.op("dve", lambda e: e.memset(sm[:, 32:33], 0.0), W=MIXT + ALLX + ["fence"])
            for c in range(NT):
                b_ = c % 2
                P.dma("sp", xin[:, b_], x_src[c * 128:(c + 1) * 128, :], R=[("xs_dram", b), ("xsd", b, c)], W=[("xin", b_)])
                for half in range(2):
                    bk = (c * 2 + half) % 4
                    for n in range(KD):
                        P.op("pe", lambda e, n=n, c=c, half=half, bk=bk: e.matmul(
                            pbank[bk][:, :], lhsT=mT[:, n, c * 128:(c + 1) * 128], rhs=wO[:, n, half * 512:(half + 1) * 512],
                            start=(n == 0), stop=(n == KD - 1)), R=[("mT", c), "wO_a" if n < 6 else "wO_b"], W=[("pb", bk)], cost=230.0)
                    P.op("dve", lambda e, c=c, half=half, bk=bk, b_=b_: e.tensor_tensor(
                        out=xres[:, c, half * 512:(half + 1) * 512], in0=pbank[bk][:, :], in1=xin[:, b_, half * 512:(half + 1) * 512], op=ALU.add),
                        R=[("pb", bk), ("xin", b_)], W=[("xres", c)])
            if stop_after == "E" and l == nlayers - 1:
                dbg_out("xres", xres, [128, NT, D], F32, R=ALLX)
                return True
            for c in range(NT):
                P.dma("sp", xs_dram[b][c * 128:(c + 1) * 128, :], xres[:, c, :], R=[("xres", c), ("xs_dram", b)], W=[("xsd", b, c)])
            norm_phase(None, gffn_b[l], mT.ap(), from_xres=True, h2_dst=h2_dram[bi], hT_tok="mT", h2i=bi)
            if stop_after == "F" and l == nlayers - 1:
                dbg_out("h2T", mT, [128, KD, S], BF16, R=ALLMT)
                return True
            lg = pbank[0].ap()[:, 0:256].rearrange("p (c e) -> p c e", c=NT)
            for c in range(NT):
                for k in range(KD):
                    P.op("pe", lambda e, c=c, k=k: e.matmul(pbank[0][:, c * 16:(c + 1) * 16], lhsT=mT[:, k, c * 128:(c + 1) * 128], rhs=wr[:, l, k, :],
                                                            start=(k == 0), stop=(k == KD - 1)), R=[("mT", c), "wr"], W=[("pb", 0)])
            SS = [("ss", c) for c in range(NT)]
            AFT = [("aff", bi)]
            av = aff[:, bi // 2, :, (bi % 2) * NE:(bi % 2 + 1) * NE]
            P.op("dve", lambda e: e.tensor_reduce(out=ss[:, :], in_=lg, axis=AX.X, op=ALU.max), R=[("pb", 0)], W=SS)
            P.op("dve", lambda e: e.tensor_tensor(out=av, in0=lg, in1=ss[:, :].unsqueeze(2).to_broadcast([128, NT, NE]), op=ALU.subtract),
                 R=[("pb", 0)] + SS, W=AFT)
            P.op("act", lambda e: e.activation(out=av, in_=av, func=AF.Exp), R=AFT, W=AFT)
            P.op("dve", lambda e: e.tensor_reduce(out=ss[:, :], in_=av, axis=AX.X, op=ALU.add), R=AFT, W=SS)
            P.op("dve", lambda e: e.reciprocal(out=ss[:, :], in_=ss[:, :]), R=SS, W=SS)
            P.op("dve", lambda e: e.tensor_tensor(out=av, in0=av, in1=ss[:, :].unsqueeze(2).to_broadcast([128, NT, NE]), op=ALU.mult),
                 R=AFT + SS, W=AFT)
            if stop_after == "G0" and l == nlayers - 1:
                dbg_out("aff", av, [128, NT, NE], F32, R=AFT)
                return True
        return False

    def moe_body(pair, l):
        NP = len(pair)
        TP = 16 * NP if NP > 1 else 16
        NC_ = NP * 256
        if True:
            P.op("dve", lambda e: e.memset(sm[:, 32:33], 0.0), W=ALLX + MIXT + WEXPT + SYT + WDT + ["fence"])
            wsrc = [w_gate_e, w_up_e, w_down_e]
            nload = [0]

            def load_w(i):
                e_, m_ = divmod(i, 3)
                P.dma("pool", wexp[i % NRING], wsrc[m_][l, e_].rearrange("(k p) n -> p k n", p=128), W=[("wexp", i % NRING)], cost=13000.0)
            while nload[0] < NRING:
                load_w(nload[0])
                nload[0] += 1
            affT = tmp.ap().bitcast(F32)[0:TP, :]
            AFALL = [("aff", bi) for bi in range(NP)]
            for pr in range((NP + 1) // 2):
                nb_ = min(2, NP - 2 * pr) * 16
                for c in range(NT):
                    bk = 1 + c // 4
                    P.op("pe", lambda e, c=c, bk=bk, pr=pr, nb_=nb_: e.transpose(pbank[bk][0:nb_, (c % 4) * 128:(c % 4 + 1) * 128], aff[:, pr, c, 0:nb_], identf[:]),
                         R=[("aff", 2 * pr), ("aff", 2 * pr + 1), "identf"], W=[("pb", bk)])
                for q4 in range(4):
                    P.op("dve", lambda e, q4=q4, pr=pr, nb_=nb_: e.tensor_copy(out=affT[pr * 32:pr * 32 + nb_, q4 * 512:(q4 + 1) * 512], in_=pbank[1 + q4][0:nb_, :]),
                         R=[("pb", 1 + q4)], W=ttok(0, 8))
            for r in range(CAP // 8):
                rs = slice(r * 8, (r + 1) * 8)
                P.op("dve", lambda e, rs=rs: e.max(out=tvals[0:TP, rs], in_=affT), R=ttok(0, 8), W=["tvals"], cost=2300.0)
                P.op("dve", lambda e, rs=rs: e.max_index(out=tidx[0:TP, rs], in_max=tvals[0:TP, rs], in_values=affT), R=ttok(0, 8) + ["tvals"], W=["tidx"], cost=2300.0)
                P.op("dve", lambda e, rs=rs: e.match_replace(out=affT, in_to_replace=tvals[0:TP, rs], in_values=affT, imm_value=-1.0),
                     R=ttok(0, 8) + ["tvals"], W=ttok(0, 8), cost=2300.0)
            P.op("dve", lambda e: e.tensor_copy(out=tidxf[0:TP, :], in_=tidx[0:TP, :]), R=["tidx"], W=["tidxf"])
            for half in range(2):
                P.op("pe", lambda e, half=half: e.transpose(pbank[5][:, half * 112:half * 112 + TP], tidxf[0:TP, half * 128:(half + 1) * 128], identf[0:TP, 0:TP]),
                     R=["tidxf", "identf"], W=[("pb", 5)])
                P.op("pe", lambda e, half=half: e.transpose(pbank[5][:, 224 + half * 112:224 + half * 112 + TP], tvals[0:TP, half * 128:(half + 1) * 128], identf[0:TP, 0:TP]),
                     R=["tvals", "identf"], W=[("pb", 5)])
            P.op("dve", lambda e: e.tensor_copy(out=idxT[:, :, 0:TP], in_=pbank[5][:, 0:224].rearrange("p (h e) -> p h e", h=2)[:, :, 0:TP]), R=[("pb", 5)], W=["idxT"])
            P.op("dve", lambda e: e.tensor_copy(out=valsT[:, :, 0:TP], in_=pbank[5][:, 224:448].rearrange("p (h e) -> p h e", h=2)[:, :, 0:TP]), R=[("pb", 5)], W=["valsT"])
            if stop_after == "G1" and l == nlayers - 1:
                dbg_out("idxT", idxT[:, :, 0:NE], [128, 2, NE], I32, R=["idxT"])
                dbg_out("valsT", valsT[:, :, 0:NE], [128, 2, NE], F32, R=["valsT"])
                return True
            mflat = mT.ap().rearrange("p k t -> p (k t)")
            oe = mflat[:, 0:8192].bitcast(F32).rearrange("p (s d) -> p s d", s=4)
            xg = mflat[:, 8192:12288].rearrange("p (s d) -> p s d", s=4)
            xsT = mflat[:, 12288:16384].rearrange("p (k c) -> p k c", k=KD)
            gT = ptok.ap().rearrange("p c n -> p (c n)").rearrange("p (k c) -> p k c", k=KD)
            PTK = [("ptok", c) for c in range(NT)]
            XG = [("xg", i) for i in range(4)]
            OE = [("oe", i) for i in range(4)]
            P.op("dve", lambda e: e.memset(sm[:, 33:34], 0.0), W=ALLMT + XG + OE + PTK + ["xsT", "gT", "fence2"])

            NSP = (NP + 1) // 2
            steps = [(e_, sp) for e_ in range(NE) for sp in range(NSP)]

            def gather(step):
                e_, sp = steps[step]
                for bl in range(min(2, NP - 2 * sp)):
                    bi = 2 * sp + bl
                    for half in range(2):
                        sc = bl * 2 + half
                        P.op("pool", lambda g_, half=half, bi=bi, sc=sc: g_.indirect_dma_start(
                            out=xg[:, sc, :], out_offset=None, in_=h2_dram[bi][:, :],
                            in_offset=bass.IndirectOffsetOnAxis(ap=idxT[:, half, bi * 16 + e_:bi * 16 + e_ + 1], axis=0)),
                            R=["idxT"] + H2D[bi], W=[("xg", sc)], dma=True, cost=4000.0)
            gather(0)

            def ex_trans(step):
                e_, sp = steps[step]
                NL = min(2, NP - 2 * sp)
                NCL = NL * 256
                for sc in range(2 * NL):
                    tpx = pb_bf(6 + sc % 2)
                    for k in range(KD):
                        P.op("pe", lambda e, k=k, sc=sc, tpx=tpx: e.transpose(tpx[:, k * 128:(k + 1) * 128], xg[:, sc, k * 128:(k + 1) * 128], ident[:]),
                             R=[("xg", sc), "ident"], W=[("pb", 6 + sc % 2)], cost=100.0)
                    P.op("act", lambda e, sc=sc, tpx=tpx: e.activation(out=xsT[:, :, sc * 128:(sc + 1) * 128],
                                                                       in_=tpx[:, 0:1024].rearrange("p (k t) -> p k t", k=KD), func=AF.Copy),
                         R=[("pb", 6 + sc % 2)], W=["xsT"], cost=800.0)
                if step + 1 < len(steps):
                    gather(step + 1)

            def ex_gateup(step):
                e_, sp = steps[step]
                NL = min(2, NP - 2 * sp)
                NCL = NL * 256
                if sp == 0:
                    while nload[0] <= min(3 * e_ + 5, 3 * NE - 1):
                        load_w(nload[0])
                        nload[0] += 1
                sg_, su_, sd_ = (3 * e_) % NRING, (3 * e_ + 1) % NRING, (3 * e_ + 2) % NRING
                mmc = 125.0 * NL
                for f in range(KD):
                    bk = f % 2
                    fs = slice(f * 128, (f + 1) * 128)
                    for k in range(KD):
                        P.op("pe", lambda e, k=k, fs=fs, bk=bk: e.matmul(pbank[bk][:, 0:NCL], lhsT=wexp[sg_][:, k, fs], rhs=xsT[:, k, 0:NCL],
                                                                         start=(k == 0), stop=(k == KD - 1)), R=[("wexp", sg_), "xsT"], W=[("pb", bk)], cost=mmc)
                    for k in range(KD):
                        P.op("pe", lambda e, k=k, fs=fs, bk=bk: e.matmul(pbank[2 + bk][:, 0:NCL], lhsT=wexp[su_][:, k, fs], rhs=xsT[:, k, 0:NCL],
                                                                         start=(k == 0), stop=(k == KD - 1)), R=[("wexp", su_), "xsT"], W=[("pb", 2 + bk)], cost=mmc)
                    P.op("act", lambda e, bk=bk: e.activation(out=sab[:, bk, 0:NCL], in_=pbank[bk][:, 0:NCL], func=AF.Silu), R=[("pb", bk)], W=[("sab", bk)], cost=300.0 * NL)
                    P.op("dve", lambda e, bk=bk, f=f: e.tensor_tensor(out=gT[:, f, 0:NCL], in0=pbank[2 + bk][:, 0:NCL], in1=sab[:, bk, 0:NCL], op=ALU.mult),
                         R=[("pb", 2 + bk), ("sab", bk)], W=["gT"], cost=300.0 * NL)

            def ex_down(step):
                e_, sp = steps[step]
                NL = min(2, NP - 2 * sp)
                NCL = NL * 256
                sg_, su_, sd_ = (3 * e_) % NRING, (3 * e_ + 1) % NRING, (3 * e_ + 2) % NRING
                for sc in range(2 * NL):
                    bl, half = divmod(sc, 2)
                    bi = 2 * sp + bl
                    for dh in range(2):
                        bk = 4 + dh
                        for f in range(KD):
                            P.op("pe", lambda e, f=f, sc=sc, dh=dh, bk=bk: e.matmul(
                                pbank[bk][:, :], lhsT=gT[:, f, sc * 128:(sc + 1) * 128], rhs=wexp[sd_][:, f, dh * 512:(dh + 1) * 512],
                                start=(f == 0), stop=(f == KD - 1)), R=[("wexp", sd_), "gT"], W=[("pb", bk)], cost=230.0)
                        P.op("act", lambda e, sc=sc, dh=dh, bk=bk, bi=bi, half=half: e.activation(
                            out=oe[:, sc, dh * 512:(dh + 1) * 512], in_=pbank[bk][:, :], func=AF.Copy, scale=valsT[:, half, bi * 16 + e_:bi * 16 + e_ + 1]),
                            R=[("pb", bk), "valsT"], W=[("oe", sc)], cost=600.0)
                    P.op("pool", lambda g_, sc=sc, bi=bi, half=half: g_.indirect_dma_start(
                        out=xs_dram[pair[bi]][:, :], out_offset=bass.IndirectOffsetOnAxis(ap=idxT[:, half, bi * 16 + e_:bi * 16 + e_ + 1], axis=0),
                        in_=oe[:, sc, :], in_offset=None, compute_op=ALU.add),
                        R=[("oe", sc), "idxT"] + XSD[pair[bi]], W=[("xs_dram", pair[bi])], dma=True, cost=5000.0)
            ex_trans(0)
            for step in range(len(steps)):
                ex_gateup(step)
                if step + 1 < len(steps):
                    ex_trans(step + 1)
                ex_down(step)
            if stop_after == "G" and l == nlayers - 1:
                dbg_out("xs", xs_dram[0], [S, D], F32, R=[("xs_dram", 0)] + XSD[0])
                return True
        return False

    for p0 in range(0, nseq, GRP):
        pair = list(range(p0, min(p0 + GRP, nseq)))
        for l in range(nlayers):
            for bi, b in enumerate(pair):
                if layer_body(b, l, bi):
                    done = True
                    break
            if done:
                break
            if moe_body(pair, l):
                done = True
                break
        if done:
            break
        P.op("dve", lambda e: e.memset(sm[:, 35:36], 0.0), W=ALLMT + EXPACT + [("fn", i) for i in range(8)] + ["fence3"])
        for b in pair:
            norm_phase(xs_dram[b], gfin_b, None, y_dst=y_out[b], xtok=b)

    if SCHEDULE:
        P.schedule()
    P.emit(nc)
    st.close()
    return nc, dbg


def _host_consts(inp):
    f = lambda a: np.ascontiguousarray(np.asarray(a, dtype=np.float32))
    rep = lambda a, n=128: np.ascontiguousarray(np.broadcast_to(np.asarray(a, np.float32)[:, None, :], (a.shape[0], n, a.shape[1])))
    c = {}
    for k in ("w_in", "w_spatial", "w_pool", "w_attn_o", "w_sgu_o", "w_pool_o", "w_out", "w_router",
              "w_gate_e", "w_up_e", "w_down_e"):
        c[k] = f(inp[k])
    c["gmix_b"] = rep(inp["g_mix"])
    c["gffn_b"] = rep(inp["g_ffn"])
    c["gfin_b"] = np.ascontiguousarray(np.broadcast_to(np.asarray(inp["g_final"], np.float32)[None, :], (128, D)))
    c["gq_b"] = rep(inp["g_q"])
    c["gk_b"] = rep(inp["g_k"])
    c["gsgu_b"] = rep(inp["g_sgu"])
    c["bsp_t"] = np.ascontiguousarray(np.asarray(inp["b_spatial"], np.float32).transpose(0, 2, 1))
    c["psc_t"] = np.ascontiguousarray(np.asarray(inp["pool_scale"], np.float32).reshape(L, 2, 128).transpose(0, 2, 1))
    c["ident"] = np.eye(128, dtype=np.float32)
    c["cs"] = _rope_table()
    c["band"] = _pool_bands()
    return c


def kernel(**inputs):
    x = np.asarray(inputs["x"], dtype=np.float32)
    consts = _host_consts(inputs)
    nc, _ = build_program()
    in_maps = []
    for i in range(N_CORES):
        m = dict(consts)
        m["x"] = np.ascontiguousarray(x[i * NSEQ:(i + 1) * NSEQ])
        in_maps.append(m)
    res = run_bass_kernel_spmd(nc, in_maps, core_ids=list(range(N_CORES)))
    out = np.concatenate([np.asarray(r["y"]) for r in res.results], axis=0)
    return out.astype(np.float32)
```

```python
import numpy as np
import concourse.bass as bass
import concourse.mybir as mybir
from concourse.bass_utils import run_bass_kernel_spmd

F32 = mybir.dt.float32
BF16 = mybir.dt.bfloat16
I32 = mybir.dt.int32
U32 = mybir.dt.uint32
ALU = mybir.AluOpType
AF = mybir.ActivationFunctionType
AX = mybir.AxisListType

D = 1024
S = 2048
NT = 16
KD = 8
L = 2
NSEQ = 4
IN_W = 4608
NE = 16
CAP = 256
EPS = 1e-6
POOL_WINDOWS = (2, 4, 8, 16)
N_CORES = 8
SCHEDULE = True
GRP = 4
STRICT = True


class Op:
    __slots__ = ("eng", "fn", "deps", "signal", "val", "is_dma", "sem_slot", "idx", "prev_same_slot", "odeps", "cost")

    def __init__(self, eng, fn, is_dma):
        self.eng = eng
        self.fn = fn
        self.deps = []
        self.odeps = []
        self.cost = None
        self.signal = False
        self.val = 0
        self.is_dma = is_dma
        self.sem_slot = None
        self.prev_same_slot = None


class Prog:
    ENGS = ("pe", "act", "dve", "pool", "sp")
    NDMA_SEMS = {"sp": 12, "pool": 12, "act": 4}

    def __init__(self):
        self.ops = []
        self.last_w = {}
        self.readers = {}
        self.dma_count = {"sp": 0, "pool": 0, "act": 0}
        self.dma_last = {}

    def _add_dep(self, op, dep, kind):
        if dep is None or dep is op:
            return
        op.odeps.append(dep)
        if (not dep.is_dma) and dep.eng == op.eng and not op.is_dma:
            if op.eng == "pe" or (kind != "raw" and not STRICT):
                return
        op.deps.append(dep)
        dep.signal = True

    def op(self, eng, fn, R=(), W=(), dma=False, cost=None):
        o = Op(eng, fn, dma)
        o.cost = cost
        for t in R:
            self._add_dep(o, self.last_w.get(t), "raw")
        for t in W:
            self._add_dep(o, self.last_w.get(t), "waw")
            for r in self.readers.get(t, ()):
                self._add_dep(o, r, "war")
        for t in R:
            lst = self.readers.setdefault(t, [])
            if not dma:
                lst[:] = [r for r in lst if r.is_dma or r.eng != eng]
            lst.append(o)
        for t in W:
            self.last_w[t] = o
            self.readers[t] = []
        if dma:
            o.signal = True
            self.dma_count[eng] += 1
        self.ops.append(o)
        return o

    def dma(self, queue, out, in_, R=(), W=(), cost=None, **kw):
        return self.op(queue, lambda e: e.dma_start(out=out, in_=in_, **kw), R, W, dma=True, cost=cost)

    DEF_COST = {"pe": 160.0, "act": 450.0, "dve": 450.0, "pool": 800.0}

    def schedule(self, window=24):
        ops = self.ops
        for i, o in enumerate(ops):
            o.idx = i
        pend = {e: [o for o in ops if o.eng == e] for e in self.ENGS}
        head = {e: 0 for e in self.ENGS}
        fin = [None] * len(ops)
        etime = {e: 0.0 for e in self.ENGS}
        order = {e: [] for e in self.ENGS}
        done = [False] * len(ops)
        remaining = len(ops)
        SEM = 120.0

        def cand(e):
            lst = pend[e]
            h = head[e]
            while h < len(lst) and done[lst[h].idx]:
                h += 1
            head[e] = h
            best = None
            seen = 0
            i = h
            while i < len(lst) and seen < window:
                o = lst[i]
                i += 1
                if done[o.idx]:
                    continue
                seen += 1
                st = etime[e]
                ok = True
                for d in o.odeps:
                    f = fin[d.idx]
                    if f is None:
                        ok = False
                        break
                    if d.eng != e or d.is_dma:
                        f = f + SEM
                    elif not o.is_dma and not d.is_dma:
                        f = f if (e == "pe") else f + 60.0
                    if f > st:
                        st = f
                if not ok:
                    continue
                if best is None or st < best[0]:
                    best = (st, o)
                    if st <= etime[e]:
                        break
            return best
        cands = {e: cand(e) for e in self.ENGS}
        while remaining:
            be = None
            for e in self.ENGS:
                c = cands[e]
                if c is not None and (be is None or c[0] < cands[be][0] or (c[0] == cands[be][0] and c[1].idx < cands[be][1].idx)):
                    be = e
            assert be is not None, "scheduler deadlock"
            st, o = cands[be]
            if o.is_dma:
                etime[be] = st + 90.0
                fin[o.idx] = st + (o.cost if o.cost is not None else 3000.0)
            else:
                c = o.cost if o.cost is not None else self.DEF_COST[be]
                etime[be] = st + c
                fin[o.idx] = st + c + (150.0 if be == "pe" else 0.0)
            done[o.idx] = True
            order[be].append(o)
            remaining -= 1
            for e in self.ENGS:
                cands[e] = cand(e)
        self.order = order
        self.sim_time = max(etime.values())

    def emit(self, nc):
        if not hasattr(self, "order"):
            self.order = {e: [o for o in self.ops if o.eng == e] for e in self.ENGS}
        per_eng = self.order
        cnt = {e: 0 for e in self.ENGS}
        for e in self.ENGS:
            ndma = 0
            last_slot = {}
            for o in per_eng[e]:
                if o.is_dma:
                    k = self.NDMA_SEMS[e]
                    o.sem_slot = (e, ndma % k)
                    o.val = 16 * (ndma // k + 1)
                    o.prev_same_slot = last_slot.get(o.sem_slot)
                    last_slot[o.sem_slot] = o
                    ndma += 1
                elif o.signal:
                    cnt[e] += 1
                    o.val = cnt[e]
        self.sig_counts = dict(cnt)
        import contextlib
        with contextlib.ExitStack() as st:
            esem = {e: st.enter_context(nc.semaphore("s_" + e)) for e in self.ENGS}
            dsem = {}
            for q, k in self.NDMA_SEMS.items():
                for i in range(k):
                    dsem[(q, i)] = st.enter_context(nc.semaphore("d_%s%d" % (q, i)))
            block = st.enter_context(nc.Block())

            def sem_of(o):
                return dsem[o.sem_slot] if o.is_dma else esem[o.eng]

            def run(engname, eng):
                waited = {}
                tail = {}
                for o in per_eng[engname]:
                    deps = list(o.deps)
                    if o.is_dma and o.prev_same_slot is not None:
                        deps.append(o.prev_same_slot)
                    for d in deps:
                        s = sem_of(d)
                        key = id(s)
                        if waited.get(key, 0) < d.val:
                            eng.wait_ge(s, d.val)
                            waited[key] = d.val
                    ins = o.fn(eng)
                    if o.signal:
                        ins.then_inc(sem_of(o), 16 if o.is_dma else 1)
                    if o.is_dma:
                        tail[id(sem_of(o))] = (sem_of(o), o.val)
                for s, v in tail.values():
                    if waited.get(id(s), 0) < v:
                        eng.wait_ge(s, v)

            @block.tensor
            def _(e):
                run("pe", e)

            @block.scalar
            def _(e):
                run("act", e)

            @block.vector
            def _(e):
                run("dve", e)

            @block.gpsimd
            def _(e):
                run("pool", e)

            @block.sync
            def _(e):
                run("sp", e)


def _rope_table():
    rows = S // 64
    row = np.broadcast_to(np.arange(rows, dtype=np.float32)[:, None], (rows, 64)).reshape(-1)
    col = np.broadcast_to(np.arange(64, dtype=np.float32)[None, :], (rows, 64)).reshape(-1)
    freqs = (10000.0 ** (-np.arange(0, 32, 2, dtype=np.float32) / 32)).astype(np.float32)
    ang = np.stack([row[:, None] * freqs, col[:, None] * freqs], axis=1)
    cs = np.concatenate([np.cos(ang).reshape(S, 32), np.sin(ang).reshape(S, 32)], axis=1)
    return cs.astype(np.float32)


def _pool_bands():
    out = np.zeros((4, 5, 128, 128), np.float32)
    t = np.arange(S)
    for g, w in enumerate(POOL_WINDOWS):
        lo = np.clip(t - w // 2, 0, S - 1)
        hi = np.clip(t + (w - 1 - w // 2), 0, S - 1)
        cnt = (hi - lo + 1).astype(np.float32)

        def blk(ci, cj):
            m = np.zeros((128, 128), np.float32)
            for tt in range(ci * 128, ci * 128 + 128):
                for tp in range(max(lo[tt], cj * 128), min(hi[tt], cj * 128 + 127) + 1):
                    m[tp - cj * 128, tt - ci * 128] += 1.0 / cnt[tt]
                if ci == cj:
                    m[tt - cj * 128, tt - ci * 128] -= 1.0
            return m
        out[g, 0] = blk(5, 4)
        out[g, 1] = blk(5, 6)
        out[g, 2] = blk(5, 5)
        out[g, 3] = blk(0, 0)
        out[g, 4] = blk(15, 15)
    return out


def build_program(nseq=NSEQ, nlayers=L, debug=None, stop_after=None):
    nc = bass.Bass("TRN2", target_bir_lowering=False)
    P = Prog()
    dbg = {}

    def din(name, shape, dt=F32):
        return nc.dram_tensor(name, list(shape), dt, kind="ExternalInput").ap()

    x_in = din("x", [nseq, S, D])
    w_in = din("w_in", [L, D, IN_W])
    w_spatial = din("w_spatial", [L, 4, 128, 128])
    w_pool = din("w_pool", [L, 4, 64, 64])
    w_attn_o = din("w_attn_o", [L, 512, D])
    w_sgu_o = din("w_sgu_o", [L, 256, D])
    w_pool_o = din("w_pool_o", [L, 256, D])
    w_out = din("w_out", [L, D, D])
    w_router = din("w_router", [L, D, NE])
    w_gate_e = din("w_gate_e", [L, NE, D, D])
    w_up_e = din("w_up_e", [L, NE, D, D])
    w_down_e = din("w_down_e", [L, NE, D, D])
    gmix_b = din("gmix_b", [L, 128, D])
    gffn_b = din("gffn_b", [L, 128, D])
    gfin_b = din("gfin_b", [128, D])
    gq_b = din("gq_b", [L, 128, 64])
    gk_b = din("gk_b", [L, 128, 64])
    gsgu_b = din("gsgu_b", [L, 128, 256])
    bsp_t = din("bsp_t", [L, 128, 4])
    psc_t = din("psc_t", [L, 128, 2])
    ident_in = din("ident", [128, 128])
    cs_in = din("cs", [S, 64])
    band_in = din("band", [4, 5, 128, 128])

    y_out = nc.dram_tensor("y", [nseq, S, D], F32, kind="ExternalOutput").ap()
    xs_dram = [nc.dram_tensor("xs_scr%d" % i, [S, D], F32, kind="Internal").ap() for i in range(nseq)]
    h2_dram = [nc.dram_tensor("h2_scr%d" % i, [S, D], BF16, kind="Internal").ap() for i in range(GRP)]

    import contextlib
    st = contextlib.ExitStack()

    def sb(name, shape, dt):
        return st.enter_context(nc.sbuf_tensor(name, list(shape), dt))

    def ps(name, shape, dt):
        return st.enter_context(nc.psum_tensor(name, list(shape), dt))

    arena = sb("arena", [128, 32768], BF16)
    hT = arena[:, 0:16384].rearrange("p (k t) -> p k t", k=KD)
    qT = arena[:, 16384:24576].rearrange("p (j t) -> p j t", j=4)
    kT2 = arena[:, 24576:32768].rearrange("p (g v t) -> p g v t", g=2, v=2)
    xres = arena.bitcast(F32).rearrange("p (c d) -> p c d", c=NT)
    mT = sb("mT", [128, KD, S], BF16)
    wB = mT.ap().rearrange("p k t -> p (k t)")[:, 0:12288].rearrange("p (s k n) -> p s k n", s=3, k=KD)
    sy = sb("sy", [128, 8192], BF16)
    sgT = sy.ap()[:, 0:4096].rearrange("p (j t) -> p j t", j=2)
    yT = sy.ap()[:, 4096:8192].rearrange("p (j t) -> p j t", j=2)
    wD = sb("wD", [128, 2 * KD * 3 * 256], BF16)
    wDv = wD.ap().rearrange("p (b k x n) -> p b k x n", b=2, k=KD, x=3)
    wO = wD.ap()[:, 0:KD * D].rearrange("p (k n) -> p k n", k=KD)
    vaug = wD.ap()[:, 6144:12288].rearrange("p (c g e) -> p c g e", c=NT, g=2)
    NRING = 6
    wexp = [arena[:, i * 8192:(i + 1) * 8192].rearrange("p (k n) -> p k n", k=KD) for i in range(4)]
    wexp.append(sy.ap().rearrange("p (k n) -> p k n", k=KD))
    wexp.append(wD.ap()[:, 0:8192].rearrange("p (k n) -> p k n", k=KD))
    woA = sb("woA", [128, 2, 4, 256], BF16)
    woB = sb("woB", [128, 2, 2, 256], BF16)
    woC = sb("woC", [128, 2, 2, 256], BF16)
    xin = sb("xin", [128, 2, D], F32)
    hb = sb("hb", [128, 2, D], BF16)
    gnb = sb("gnb", [128, D], F32)
    ss = sb("ss", [128, NT], F32)
    sm = sb("sm", [128, 40], F32)
    epsb = sb("epsb", [128, 1], F32)
    ident = sb("identb", [128, 128], BF16)
    identf = sb("identf", [128, 128], F32)
    cs = sb("cs_sb", [128, NT, 64], F32)
    band = sb("band_sb", [128, 4, 5, 128], BF16)
    wsT = sb("wsT", [128, L, 4, 128], BF16)
    wpl2 = sb("wpl2", [128, L, 2, 128], BF16)
    wr = sb("wr", [128, L, KD, NE], BF16)
    gq = sb("gq", [128, L, 64], F32)
    gk = sb("gk", [128, L, 64], F32)
    gsg = sb("gsg", [128, L, 256], F32)
    bsp = sb("bsp", [128, L, 4], F32)
    psc = sb("psc", [128, L, 2], F32)
    vnb = sb("vnb", [128, 2, 256], BF16)
    sgtok = sb("sgtok", [128, 256], BF16)
    dtok = sb("dtok", [128, 256], BF16)
    ptok = sb("ptok", [128, NT, 256], BF16)
    dTc = sb("dTc", [128, 2, 128], BF16)
    aff = sb("aff", [128, (GRP + 1) // 2, NT, 2 * NE], F32)
    tvals = sb("tvals", [112, CAP], F32)
    tidx = sb("tidx", [112, CAP], U32)
    tidxf = sb("tidxf", [112, CAP], F32)
    idxT = sb("idxT", [128, 2, 112], I32)
    valsT = sb("valsT", [128, 2, 112], F32)
    sab = sb("sab", [128, 2, 512], F32)
    tmp = sb("tmp", [128, 4096], BF16)

    def tslot(i, n=1, dt=BF16):
        v = tmp.ap()[:, i * 512:(i + n) * 512]
        return v if dt == BF16 else v.bitcast(dt)

    def ttok(i, n=1):
        return [("tmp", j) for j in range(i, i + n)]
    wsp_raw = tslot(0, 1).rearrange("p (g q) -> p g q", g=4)
    qn = tslot(0, 2, F32); qr = tslot(2, 2, F32); sqf = tslot(4, 2, F32); qtok = tslot(6); utok = tslot(7, 1, F32)
    pT = [tslot(i) for i in range(3)]
    otok = tslot(3, 4).rearrange("p (c n) -> p c n", c=4)
    sga = [tslot(i) for i in range(3)]
    mprod = [tslot(3, 2, F32), tslot(5, 2, F32)]

    pbank = [ps("pb%d" % i, [128, 512], F32) for i in range(8)]

    def pb_bf(i):
        return pbank[i].ap().bitcast(BF16)

    P.dma("pool", ident[:], ident_in, W=["ident"])
    P.dma("sp", identf[:], ident_in, W=["identf"])
    P.dma("sp", cs[:], cs_in.rearrange("(c p) f -> p c f", p=128), W=["cs"])
    P.dma("pool", band[:], band_in.rearrange("g v a b -> a g v b"), W=["band"])
    P.op("dve", lambda e: e.memset(wpl2[:], 0.0), W=["wpl2"])
    P.op("dve", lambda e: e.memset(epsb[:], EPS), W=["epsb"])
    for l in range(L):
        for g in range(4):
            gg = g % 2
            P.dma("pool", wpl2[gg * 64:(gg + 1) * 64, l, g // 2, gg * 64:(gg + 1) * 64], w_pool[l, g], W=["wpl2"], R=[])
        P.dma("pool", wr[:, l], w_router[l].rearrange("(k p) e -> p k e", p=128), W=["wr"])
        P.dma("sp", gq[:, l], gq_b[l], W=["gq"])
        P.dma("sp", gk[:, l], gk_b[l], W=["gk"])
        P.dma("sp", gsg[:, l], gsgu_b[l], W=["gsg"])
        P.dma("sp", bsp[:, l], bsp_t[l], W=["bsp"])
        P.dma("sp", psc[:, l], psc_t[l], W=["psc"])
        P.dma("pool", wsp_raw, w_spatial[l].rearrange("g p q -> p g q"), W=["wsp_raw"] + ttok(0))
        tp = pb_bf(0)
        for g in range(4):
            P.op("pe", lambda e, g=g, tp=tp: e.transpose(tp[:, g * 128:(g + 1) * 128], wsp_raw[:, g, :], ident[:]),
                 R=["wsp_raw", "ident"] + ttok(0), W=[("pb", 0)])
        P.op("dve", lambda e, l=l, tp=tp: e.tensor_copy(out=wsT[:, l], in_=tp[:, 0:512].rearrange("p (g q) -> p g q", g=4)),
             R=[("pb", 0)], W=["wsT"])

    def rsqrt_eps(ap, toks):
        P.op("act", lambda e: e.activation(out=ap, in_=ap, func=AF.Sqrt, bias=epsb[0:ap.shape[0], 0:1], scale=1.0), R=toks + ["epsb"], W=toks)
        P.op("dve", lambda e: e.reciprocal(out=ap, in_=ap), R=toks, W=toks)

    def norm_phase(x_src_dram, g_dram, to_hT, from_xres=False, h2_dst=None, y_dst=None, hT_tok="hT", pbase=6, xtok=0, h2i=0,
                   pre_chunk=None, post_chunk=None):
        P.dma("sp", gnb[:], g_dram, W=["gnb"])
        if y_dst is not None:
            fnb = mT.ap().rearrange("p k t -> p (k t)").bitcast(F32).rearrange("p (i d) -> p i d", i=8)
        for c in range(NT):
            b = c % 2
            if pre_chunk is not None:
                pre_chunk(c)
            if y_dst is not None:
                bi_, bo_ = c % 4, 4 + c % 4
                P.dma("sp", fnb[:, bi_], x_src_dram[c * 128:(c + 1) * 128, :], R=[("xs_dram", xtok), ("xsd", xtok, c)], W=[("fn", bi_)])
                P.op("act", lambda e, c=c, bi_=bi_, bo_=bo_: e.activation(out=fnb[:, bo_], in_=fnb[:, bi_], func=AF.Square, scale=1.0 / 32.0, accum_out=ss[:, c:c + 1]),
                     R=[("fn", bi_)], W=[("fn", bo_), ("ss", c)])
                rsqrt_eps(ss[:, c:c + 1], [("ss", c)])
                P.op("dve", lambda e, c=c, bi_=bi_, bo_=bo_: e.scalar_tensor_tensor(out=fnb[:, bo_], in0=fnb[:, bi_], scalar=ss[:, c:c + 1],
                                                                                     in1=gnb[:], op0=ALU.mult, op1=ALU.mult),
                     R=[("fn", bi_), ("fn", bo_), ("ss", c), "gnb"], W=[("fn", bo_)])
                P.dma("sp", y_dst[c * 128:(c + 1) * 128, :], fnb[:, bo_], R=[("fn", bo_)], W=[("y_out", xtok, c)])
                continue
            if from_xres:
                src = xres[:, c, :]
                srcR = [("xres", c)]
            else:
                P.dma("sp", xin[:, b], x_src_dram[c * 128:(c + 1) * 128, :], R=[("xs_dram", xtok), ("xsd", xtok, c)], W=[("xin", b)])
                src = xin[:, b]
                srcR = [("xin", b)]
            P.op("act", lambda e, src=src, c=c, b=b: e.activation(out=hb[:, b], in_=src, func=AF.Square, scale=1.0 / 32.0, accum_out=ss[:, c:c + 1]),
                 R=srcR, W=[("hb", b), ("ss", c)])
            rsqrt_eps(ss[:, c:c + 1], [("ss", c)])
            P.op("dve", lambda e, src=src, c=c, b=b: e.scalar_tensor_tensor(out=hb[:, b], in0=src, scalar=ss[:, c:c + 1],
                                                                             in1=gnb[:], op0=ALU.mult, op1=ALU.mult),
                 R=srcR + [("ss", c), "gnb"], W=[("hb", b)])
            if h2_dst is not None:
                P.dma("sp", h2_dst[c * 128:(c + 1) * 128, :], hb[:, b], R=[("hb", b)], W=[("h2d", h2i, c)])
            bk = pbase + (c % 2)
            tp = pb_bf(bk)
            for k in range(KD):
                P.op("pe", lambda e, k=k, b=b, tp=tp: e.transpose(tp[:, k * 128:(k + 1) * 128], hb[:, b, k * 128:(k + 1) * 128], ident[:]),
                     R=[("hb", b), "ident"], W=[("pb", bk)])
            P.op("act", lambda e, c=c, tp=tp: e.activation(out=to_hT[:, :, c * 128:(c + 1) * 128],
                                                           in_=tp[:, 0:1024].rearrange("p (k t) -> p k t", k=KD), func=AF.Copy),
                 R=[("pb", bk)], W=[(hT_tok, c)])
            if post_chunk is not None:
                post_chunk(c)

    def headnorm_rope(src_ps, nh, gain, c, dst_bf, Rsrc, Wdst):
        w = nh * 64
        P.op("act", lambda e: e.activation(out=sqf[:, 0:w], in_=src_ps, func=AF.Square, scale=0.125), R=Rsrc, W=ttok(4, 2))
        P.op("dve", lambda e: e.tensor_reduce(out=sm[:, 0:nh], in_=sqf[:, 0:w].rearrange("p (h d) -> p h d", h=nh), axis=AX.X, op=ALU.add),
             R=ttok(4, 2), W=["sm"])
        rsqrt_eps(sm[:, 0:nh], ["sm"])
        P.op("dve", lambda e: e.tensor_tensor(out=qn[:, 0:w].rearrange("p (h d) -> p h d", h=nh),
                                              in0=src_ps.rearrange("p (h d) -> p h d", h=nh),
                                              in1=sm[:, 0:nh].unsqueeze(2).to_broadcast([128, nh, 64]), op=ALU.mult),
             R=Rsrc + ["sm"], W=ttok(0, 2))
        P.op("dve", lambda e: e.tensor_tensor(out=qn[:, 0:w].rearrange("p (h d) -> p h d", h=nh),
                                              in0=qn[:, 0:w].rearrange("p (h d) -> p h d", h=nh),
                                              in1=gain.unsqueeze(1).to_broadcast([128, nh, 64]), op=ALU.mult),
             R=ttok(0, 2) + ["gq", "gk"], W=ttok(0, 2))
        m = nh * 2
        qv = qn[:, 0:w].rearrange("p (m two f) -> p m two f", m=m, two=2)
        rv = qr[:, 0:w].rearrange("p (m two f) -> p m two f", m=m, two=2)
        dv = dst_bf.rearrange("p (m two f) -> p m two f", m=m, two=2)
        cosb = cs[:, c, 0:32].rearrange("p (a f) -> p a f", a=2)
        sinb = cs[:, c, 32:64].rearrange("p (a f) -> p a f", a=2)

        def bc(t):
            return t.unsqueeze(1).to_broadcast([128, nh, 2, 16])
        x1 = qn[:, 0:w].rearrange("p (h a two f) -> p h a two f", h=nh, a=2, two=2)
        r_ = qr[:, 0:w].rearrange("p (h a two f) -> p h a two f", h=nh, a=2, two=2)
        d_ = dst_bf.rearrange("p (h a two f) -> p h a two f", h=nh, a=2, two=2)
        P.op("dve", lambda e: e.tensor_tensor(out=r_[:, :, :, 0, :], in0=x1[:, :, :, 0, :], in1=bc(cosb), op=ALU.mult), R=ttok(0, 2) + ["cs"], W=ttok(2, 2))
        P.op("dve", lambda e: e.tensor_tensor(out=r_[:, :, :, 1, :], in0=x1[:, :, :, 1, :], in1=bc(sinb), op=ALU.mult), R=ttok(0, 2) + ["cs"], W=ttok(2, 2))
        P.op("dve", lambda e: e.tensor_tensor(out=d_[:, :, :, 0, :], in0=r_[:, :, :, 0, :], in1=r_[:, :, :, 1, :], op=ALU.subtract), R=ttok(2, 2), W=Wdst)
        P.op("dve", lambda e: e.tensor_tensor(out=r_[:, :, :, 0, :], in0=x1[:, :, :, 1, :], in1=bc(cosb), op=ALU.mult), R=ttok(0, 2) + ["cs"], W=ttok(2, 2))
        P.op("dve", lambda e: e.tensor_tensor(out=r_[:, :, :, 1, :], in0=x1[:, :, :, 0, :], in1=bc(sinb), op=ALU.mult), R=ttok(0, 2) + ["cs"], W=ttok(2, 2))
        P.op("dve", lambda e: e.tensor_tensor(out=d_[:, :, :, 1, :], in0=r_[:, :, :, 0, :], in1=r_[:, :, :, 1, :], op=ALU.add), R=ttok(2, 2), W=Wdst)

    def dbg_out(name, ap_sb, shape, dt=F32, R=()):
        t = nc.dram_tensor("dbg_" + name, list(shape), dt, kind="ExternalOutput").ap()
        if hasattr(ap_sb, "ap") and callable(getattr(ap_sb, "ap")):
            ap_sb = ap_sb.ap()
        P.dma("sp", t, ap_sb, R=list(R), W=["dbg_" + name])
        dbg[name] = t

    ALLHT = [("hT", c) for c in range(NT)]
    ALLQT = [("qT", c) for c in range(NT)]
    ALLMT = [("mT", c) for c in range(NT)]
    ALLX = [("xres", c) for c in range(NT)]
    ALLQT = [("qT", c, j, hp) for c in range(NT) for j in range(4) for hp in range(2)]
    VAUGT = [("vaug", c) for c in range(NT)] + ["vaug_ones"]
    MIXT = ALLHT + ALLQT + [("kT2", c) for c in range(NT)] + [("kT2b", c) for c in range(NT)] + ["kT2_zero"] + VAUGT
    WEXPT = [("wexp", i) for i in range(6)]
    SYT = [("sgT", c) for c in range(NT)] + [("yT", c) for c in range(NT)]
    WDT = [("wD", bb, x_) for bb in range(2) for x_ in range(3)] + ["wO_a", "wO_b"]
    WBT = [("wB", s) for s in range(3)]
    SUBB = [("pb", 3), ("pb", 4), ("pb", 5), ("pb", 3), ("pb", 3), ("pb", 4), ("pb", 4), ("pb", 5), ("pb", 5), "sq_", "qr_", ("utok_", 0), ("utok_", 1)]
    XSD = [[("xsd", b_, c) for c in range(NT)] for b_ in range(nseq)]
    H2D = [[("h2d", i_, c) for c in range(NT)] for i_ in range(GRP)]
    EXPACT = [("xg", i) for i in range(4)] + [("oe", i) for i in range(4)] + ["xsT", "gT"]
    done = False
    def layer_body(b, l, bi):
        if True:
            x_src = x_in[b] if l == 0 else xs_dram[b]
            P.op("dve", lambda e: e.memset(sm[:, 32:33], 0.0), W=MIXT + ALLX + WEXPT + ALLMT + WBT + EXPACT + SYT + WDT + SUBB + [("fn", i) for i in range(8)] + ["fence"])
            if stop_after == "A00" and l == nlayers - 1:
                dbg_out("xs", xs_dram[0], [S, D], F32, R=[("xs_dram", 0)] + XSD[0])
                return True
            P.op("pool", lambda e: e.memset(kT2[64:128, :, 0, :], 0.0), W=["kT2_zero"])
            P.op("pool", lambda e: e.memset(kT2[0:64, :, 1, :], 0.0), W=["kT2_zero"])
            P.op("dve", lambda e: e.memset(vaug[:, :, :, 0:64], 1.0), W=["vaug_ones"])
            P.op("dve", lambda e: e.memset(vaug[:, :, :, 128:192], 1.0), W=["vaug_ones"])
            for s in range(3):
                P.dma("pool", wB[:, s], w_in[l][:, s * 512:(s + 1) * 512].rearrange("(k p) n -> p k n", p=128),
                      W=ALLMT + [("wB", s)] if s == 0 else [("wB", s)], R=[])
            if stop_after == "A0" and l == nlayers - 1:
                dbg_out("xs", xs_dram[0], [S, D], F32, R=[("xs_dram", 0)] + XSD[0])
                return True
            if stop_after == "A" and l == nlayers - 1:
                dbg_out("hT", hT, [128, KD, S], BF16, R=ALLHT)
                if l > 0:
                    dbg_out("xs", xs_dram[0], [S, D], F32, R=[("xs_dram", 0)] + XSD[0])
                return True
            mtail = mT.ap().rearrange("p k t -> p (k t)")[:, 12288:16384]
            qkb = [tslot(0, 3, F32)[:, 0:640], tslot(3, 3, F32)[:, 0:640]]
            qkt = [ttok(0, 3), ttok(3, 3)]
            qtok2 = tslot(6, 2)[:, 0:640]
            sq_ = mtail[:, 0:1280].bitcast(F32)
            qr_ = mtail[:, 1280:2560].bitcast(F32)
            utk = [mtail[:, 2560:3072].bitcast(F32), mtail[:, 3072:3584].bitcast(F32)]

            def pool_chunk(c):
                for g in range(4):
                    terms = []
                    if c > 0:
                        terms.append((c - 1, 0))
                    terms.append((c, 3 if c == 0 else (4 if c == NT - 1 else 2)))
                    if c < NT - 1:
                        terms.append((c + 1, 1))
                    for i, (cj, v) in enumerate(terms):
                        P.op("pe", lambda e, g=g, cj=cj, v=v, i=i, n=len(terms): e.matmul(
                            pbank[5][:, 256 + g * 64:256 + (g + 1) * 64], lhsT=band[:, g, v, :], rhs=ptok[:, cj, g * 64:(g + 1) * 64],
                            start=(i == 0), stop=(i == n - 1)), R=[("ptok", cj), "band"], W=[("pb", 5)])
                P.op("act", lambda e: e.activation(out=dtok[:, :], in_=pbank[5][:, 256:512], func=AF.Copy), R=[("pb", 5)], W=["dtok"])
                tpd = pb_bf(3)
                for j in range(2):
                    P.op("pe", lambda e, j=j: e.transpose(tpd[:, 512 + j * 128:512 + (j + 1) * 128], dtok[:, j * 128:(j + 1) * 128], ident[:]),
                         R=["dtok", "ident"], W=[("pb", 3)])
                P.op("act", lambda e: e.activation(out=dTc[:, :, :], in_=tpd[:, 512:768].rearrange("p (j t) -> p j t", j=2), func=AF.Copy),
                     R=[("pb", 3)], W=["dTc"])
                for j in range(2):
                    P.op("pe", lambda e, j=j: e.matmul(pbank[4][:, 256 + j * 128:256 + (j + 1) * 128], lhsT=wpl2[:, l, j, :], rhs=dTc[:, j, :],
                                                       start=True, stop=True), R=["dTc", "wpl2"], W=[("pb", 4)])
                P.op("dve", lambda e: e.tensor_tensor(out=yT[:, :, c * 128:(c + 1) * 128], in0=pbank[4][:, 256:512].rearrange("p (j t) -> p j t", j=2),
                                                      in1=psc[:, l, :].unsqueeze(2).to_broadcast([128, 2, 128]), op=ALU.mult),
                     R=[("pb", 4), "psc"], W=[("yT", c)])

            def projB1(c):
                cb = c % 2
                qk = qkb[cb]
                QK = qkt[cb]
                vn_ = vnb[:, cb, :]
                VN = [("vn", cb)]
                utok_ = utk[cb]
                UT = [("utok_", cb)]
                for s_ in range(3):
                    for k in range(KD):
                        P.op("pe", lambda e, s_=s_, k=k: e.matmul(pbank[s_][:, :], lhsT=hT[:, k, c * 128:(c + 1) * 128], rhs=wB[:, s_, k, :],
                                                                  start=(k == 0), stop=(k == KD - 1)),
                             R=[("hT", c), ("wB", s_)], W=[("pb", s_)])
                P.op("act", lambda e: e.activation(out=qk[:, 0:512], in_=pbank[0][:, :], func=AF.Copy), R=[("pb", 0)], W=QK)
                P.op("act", lambda e: e.activation(out=qk[:, 512:640], in_=pbank[1][:, 0:128], func=AF.Copy), R=[("pb", 1)], W=QK)
                P.op("act", lambda e: e.activation(out=sq_[:, :], in_=qk[:, :], func=AF.Square, scale=0.125), R=QK, W=["sq_"])
                P.op("act", lambda e: e.activation(out=vaug[:, c, :, 64:128], in_=pbank[1][:, 128:256].rearrange("p (g d) -> p g d", g=2), func=AF.Copy),
                     R=[("pb", 1)], W=[("vaug", c)])
                P.op("act", lambda e: e.activation(out=utok_[:, :], in_=pbank[1][:, 256:512], func=AF.Copy), R=[("pb", 1)], W=UT)
                P.op("dve", lambda e: e.tensor_reduce(out=sm[:, 0:10], in_=sq_[:, :].rearrange("p (h d) -> p h d", h=10), axis=AX.X, op=ALU.add),
                     R=["sq_"], W=["sm"])
                rsqrt_eps(sm[:, 0:10], ["sm"])
                P.op("dve", lambda e: e.tensor_tensor(out=qk[:, :].rearrange("p (h d) -> p h d", h=10),
                                                      in0=qk[:, :].rearrange("p (h d) -> p h d", h=10),
                                                      in1=sm[:, 0:10].unsqueeze(2).to_broadcast([128, 10, 64]), op=ALU.mult),
                     R=QK + ["sm"], W=QK)
                P.op("act", lambda e: e.activation(out=vn_, in_=pbank[2][:, 0:256], func=AF.Square, scale=1.0 / 16.0, accum_out=sm[:, 16:17]),
                     R=[("pb", 2)], W=VN + ["sm2"])
                rsqrt_eps(sm[:, 16:17], ["sm2"])
                P.op("dve", lambda e: e.scalar_tensor_tensor(out=vn_, in0=pbank[2][:, 0:256], scalar=sm[:, 16:17], in1=gsg[:, l, :],
                                                             op0=ALU.mult, op1=ALU.mult), R=[("pb", 2), "sm2", "gsg"], W=VN)
                P.op("act", lambda e: e.activation(out=ptok[:, c, :], in_=pbank[2][:, 256:512], func=AF.Copy), R=[("pb", 2)], W=[("ptok", c)])
            def projB2(c):
                cb = c % 2
                qk = qkb[cb]
                QK = qkt[cb]
                vn_ = vnb[:, cb, :]
                VN = [("vn", cb)]
                utok_ = utk[cb]
                UT = [("utok_", cb)]
                P.op("dve", lambda e: e.tensor_tensor(out=qk[:, 0:512].rearrange("p (h d) -> p h d", h=8),
                                                       in0=qk[:, 0:512].rearrange("p (h d) -> p h d", h=8),
                                                       in1=gq[:, l, :].unsqueeze(1).to_broadcast([128, 8, 64]), op=ALU.mult), R=QK + ["gq"], W=QK)
                P.op("dve", lambda e: e.tensor_tensor(out=qk[:, 512:640].rearrange("p (h d) -> p h d", h=2),
                                                       in0=qk[:, 512:640].rearrange("p (h d) -> p h d", h=2),
                                                       in1=gk[:, l, :].unsqueeze(1).to_broadcast([128, 2, 64]), op=ALU.mult), R=QK + ["gk"], W=QK)
                x1 = qk[:, :].rearrange("p (h a two f) -> p h a two f", h=10, a=2, two=2)
                r_ = qr_[:, :].rearrange("p (h a two f) -> p h a two f", h=10, a=2, two=2)
                d_ = qtok2.rearrange("p (h a two f) -> p h a two f", h=10, a=2, two=2)
                cosb = cs[:, c, 0:32].rearrange("p (a f) -> p a f", a=2).unsqueeze(1).to_broadcast([128, 10, 2, 16])
                sinb = cs[:, c, 32:64].rearrange("p (a f) -> p a f", a=2).unsqueeze(1).to_broadcast([128, 10, 2, 16])
                QT2 = ttok(6, 2)
                P.op("pool", lambda e: e.tensor_tensor(out=r_[:, :, :, 0, :], in0=x1[:, :, :, 0, :], in1=cosb, op=ALU.mult), R=QK + ["cs"], W=["qr_"])
                P.op("pool", lambda e: e.tensor_tensor(out=r_[:, :, :, 1, :], in0=x1[:, :, :, 1, :], in1=sinb, op=ALU.mult), R=QK + ["cs"], W=["qr_"])
                P.op("pool", lambda e: e.tensor_tensor(out=d_[:, :, :, 0, :], in0=r_[:, :, :, 0, :], in1=r_[:, :, :, 1, :], op=ALU.subtract), R=["qr_"], W=QT2)
                P.op("pool", lambda e: e.tensor_tensor(out=r_[:, :, :, 0, :], in0=x1[:, :, :, 1, :], in1=cosb, op=ALU.mult), R=QK + ["cs"], W=["qr_"])
                P.op("pool", lambda e: e.tensor_tensor(out=r_[:, :, :, 1, :], in0=x1[:, :, :, 0, :], in1=sinb, op=ALU.mult), R=QK + ["cs"], W=["qr_"])
                P.op("pool", lambda e: e.tensor_tensor(out=d_[:, :, :, 1, :], in0=r_[:, :, :, 0, :], in1=r_[:, :, :, 1, :], op=ALU.add), R=["qr_"], W=QT2)
                tp = pb_bf(3)
                for j in range(4):
                    P.op("pe", lambda e, j=j: e.transpose(tp[:, j * 128:(j + 1) * 128], qtok2[:, j * 128:(j + 1) * 128], ident[:]),
                         R=QT2 + ["ident"], W=[("pb", 3)])
                P.op("act", lambda e: e.activation(out=qT[:, :, c * 128:(c + 1) * 128],
                                                   in_=tp[:, 0:512].rearrange("p (j t) -> p j t", j=4), func=AF.Copy),
                     R=[("pb", 3)], W=[("qT", c, j_, hp_) for j_ in range(4) for hp_ in range(2)])
                tpk = pb_bf(4)
                for g in range(2):
                    for hh in range(2):
                        P.op("pe", lambda e, g=g, hh=hh: e.transpose(
                            tpk[hh * 64:(hh + 1) * 64, g * 128:(g + 1) * 128], qtok2[:, 512 + g * 64:512 + (g + 1) * 64], ident[:]),
                            R=QT2 + ["ident"], W=[("pb", 4)])
                P.op("act", lambda e: e.activation(out=kT2[0:64, :, 0, c * 128:(c + 1) * 128],
                                                   in_=tpk[0:64, 0:256].rearrange("p (g t) -> p g t", g=2), func=AF.Copy),
                     R=[("pb", 4)], W=[("kT2", c)])
                P.op("dve", lambda e: e.tensor_copy(out=kT2[64:128, :, 1, c * 128:(c + 1) * 128],
                                                    in_=tpk[64:128, 0:256].rearrange("p (g t) -> p g t", g=2)),
                     R=[("pb", 4)], W=[("kT2b", c)])
                for g in range(4):
                    P.op("pe", lambda e, g=g: e.matmul(pbank[5][:, g * 64:(g + 1) * 64], lhsT=wsT[:, l, g, :], rhs=vn_[:, g * 64:(g + 1) * 64],
                                                       start=True, stop=True), R=VN + ["wsT"], W=[("pb", 5)])
                P.op("dve", lambda e: e.tensor_tensor(out=sgtok[:, :].rearrange("p (g d) -> p g d", g=4),
                                                      in0=pbank[5][:, 0:256].rearrange("p (g d) -> p g d", g=4),
                                                      in1=bsp[:, l, :].unsqueeze(2).to_broadcast([128, 4, 64]), op=ALU.add),
                     R=[("pb", 5), "bsp"], W=["sgtok"])
                P.op("pool", lambda e: e.tensor_tensor(out=sgtok[:, :], in0=sgtok[:, :], in1=utok_[:, :], op=ALU.mult), R=["sgtok"] + UT, W=["sgtok"])
                tps = pb_bf(4)
                for j in range(2):
                    P.op("pe", lambda e, j=j: e.transpose(tps[:, 256 + j * 128:256 + (j + 1) * 128], sgtok[:, j * 128:(j + 1) * 128], ident[:]),
                         R=["sgtok", "ident"], W=[("pb", 4)])
                P.op("act", lambda e: e.activation(out=sgT[:, :, c * 128:(c + 1) * 128],
                                                   in_=tps[:, 256:512].rearrange("p (j t) -> p j t", j=2), func=AF.Copy),
                     R=[("pb", 4)], W=[("sgT", c)])

            def stepB(c):
                if c + 1 < NT:
                    projB1(c + 1)
                projB2(c)
                if c >= 1:
                    pool_chunk(c - 1)

            def ab_post(c):
                if c == 1:
                    projB1(0)
                if c >= 2:
                    stepB(c - 2)
            norm_phase(x_src, gmix_b[l], hT, xtok=b, post_chunk=ab_post)
            stepB(NT - 2)
            stepB(NT - 1)
            pool_chunk(NT - 1)
            P.op("dve", lambda e: e.memset(sm[:, 34:35], 0.0),
                 W=[("pb", 3), ("pb", 4), ("pb", 5), ("pb", 3), ("pb", 3), ("pb", 4), ("pb", 4), ("pb", 5), ("pb", 5), "sq_", "qr_", ("utok_", 0), ("utok_", 1), "fenceB"])
            if stop_after == "B" and l == nlayers - 1:
                dbg_out("qT", qT, [128, 4, S], BF16, R=ALLQT)
                dbg_out("kT2", kT2, [128, 2, 2, S], BF16, R=[("kT2", c) for c in range(NT)] + [("kT2b", c) for c in range(NT)] + ["kT2_zero"])
                dbg_out("vaug", vaug, [128, NT, 2, 192], BF16, R=[("vaug", c) for c in range(NT)] + ["vaug_ones"])
                dbg_out("sgT", sgT, [128, 2, S], BF16, R=[("sgT", c) for c in range(NT)])
                dbg_out("yT", yT, [128, 2, S], BF16, R=[("yT", c) for c in range(NT)])
                return True
            items = [(qb, h, s_) for qb in range(4) for h in range(8) for s_ in range(NT)]
            recb = tslot(3, 2, F32)
            RECT = ttok(3, 2)

            def qk_exp(i):
                qb, h, s_ = items[i]
                g = h // 4
                hp = h % 2
                ho = hp * 64
                j = h // 2
                sbk = i % 3
                P.op("pe", lambda e: e.matmul(
                    pbank[sbk][:, :], lhsT=kT2[:, g, hp, s_ * 128:(s_ + 1) * 128], rhs=qT[:, j, qb * 512:(qb + 1) * 512],
                    start=True, stop=True),
                    R=[("kT2", s_), ("kT2b", s_), "kT2_zero"] + [("qT", qb * 4 + i_, j, hp_) for i_ in range(4) for hp_ in range(2)],
                    W=[("pb", sbk)], cost=230.0)
                P.op("act", lambda e: e.activation(out=pT[sbk], in_=pbank[sbk][:, :], func=AF.Exp, scale=0.125),
                     R=[("pb", sbk)], W=ttok(sbk), cost=560.0)

            def pv(i):
                qb, h, s_ = items[i]
                g = h // 4
                hp = h % 2
                j = h // 2
                sbk = i % 3
                pob = 3 + (h % 2)
                win = slice(64, 192) if hp == 0 else slice(0, 128)
                P.op("pe", lambda e: e.matmul(pbank[pob][:, :], lhsT=vaug[:, s_, g, win], rhs=pT[sbk],
                                              start=(s_ == 0), stop=(s_ == NT - 1)),
                     R=ttok(sbk) + [("vaug", s_), "vaug_ones"], W=[("pb", pob)], cost=230.0)
                if s_ == NT - 1:
                    op_ = slice(0, 64) if hp == 0 else slice(64, 128)
                    dp_ = slice(64, 128) if hp == 0 else slice(0, 64)
                    P.op("dve", lambda e: e.reciprocal(out=recb[dp_, :], in_=pbank[pob][dp_, :]), R=[("pb", pob)], W=RECT, cost=600.0)
                    P.op("dve", lambda e: e.tensor_tensor(out=qT[op_, j, qb * 512:(qb + 1) * 512], in0=pbank[pob][op_, :], in1=recb[dp_, :], op=ALU.mult),
                         R=[("pb", pob)] + RECT, W=[("qT", qb * 4 + i_, j, hp) for i_ in range(4)], cost=600.0)
            LOOK = 2
            for i in range(min(LOOK, len(items))):
                qk_exp(i)
            for i in range(len(items)):
                if i + LOOK < len(items):
                    qk_exp(i + LOOK)
                pv(i)
            if stop_after == "C" and l == nlayers - 1:
                dbg_out("oT", qT, [128, 4, S], BF16, R=ALLQT)
                return True
            P.op("dve", lambda e: e.memset(sm[:, 32:33], 0.0), W=ALLMT + WBT + ["sq_", "qr_", ("utok_", 0), ("utok_", 1), "fence"])

            def load_D(ns):
                buf = ns % 2
                for x_ in range(3):
                    c0 = 1536 + x_ * 1024 + ns * 256
                    P.dma("pool", wDv[:, buf, :, x_, :], w_in[l][:, c0:c0 + 256].rearrange("(k p) n -> p k n", p=128),
                          W=[("wD", buf, x_)] + (VAUGT if buf == 1 else []))
                P.dma("pool", woA[:, buf], w_attn_o[l][:, ns * 256:(ns + 1) * 256].rearrange("(j p) n -> p j n", p=128), W=[("woA", buf)])
                P.dma("pool", woB[:, buf], w_sgu_o[l][:, ns * 256:(ns + 1) * 256].rearrange("(j p) n -> p j n", p=128), W=[("woB", buf)])
                P.dma("pool", woC[:, buf], w_pool_o[l][:, ns * 256:(ns + 1) * 256].rearrange("(j p) n -> p j n", p=128), W=[("woC", buf)])
            load_D(0)
            for ns in range(4):
                buf = ns % 2
                if ns + 1 < 4:
                    load_D(ns + 1)
                for tb in range(4):
                    tsl = slice(tb * 512, (tb + 1) * 512)
                    tt = [tb * 4 + i for i in range(4)]
                    for nn in range(2):
                        n = ns * 2 + nn
                        nsl = slice(nn * 128, (nn + 1) * 128)
                        for x_ in range(3):
                            for k in range(KD):
                                P.op("pe", lambda e, x_=x_, k=k, buf=buf, nsl=nsl, tsl=tsl: e.matmul(
                                    pbank[x_][:, :], lhsT=wDv[:, buf, k, x_, nsl], rhs=hT[:, k, tsl], start=(k == 0), stop=(k == KD - 1)),
                                    R=[("wD", buf, x_)] + [("hT", t) for t in tt], W=[("pb", x_)])
                        for j in range(4):
                            P.op("pe", lambda e, j=j, buf=buf, nsl=nsl, tsl=tsl: e.matmul(
                                pbank[3][:, :], lhsT=woA[:, buf, j, nsl], rhs=qT[:, j, tsl], start=(j == 0), stop=(j == 3)),
                                R=[("woA", buf)] + [("qT", t, j, hp_) for t in tt for hp_ in range(2)], W=[("pb", 3)])
                        for j in range(2):
                            P.op("pe", lambda e, j=j, buf=buf, nsl=nsl, tsl=tsl: e.matmul(
                                pbank[4][:, :], lhsT=woB[:, buf, j, nsl], rhs=sgT[:, j, tsl], start=(j == 0), stop=(j == 1)),
                                R=[("woB", buf)] + [("sgT", t) for t in tt], W=[("pb", 4)])
                        for j in range(2):
                            P.op("pe", lambda e, j=j, buf=buf, nsl=nsl, tsl=tsl: e.matmul(
                                pbank[5][:, :], lhsT=woC[:, buf, j, nsl], rhs=yT[:, j, tsl], start=(j == 0), stop=(j == 1)),
                                R=[("woC", buf)] + [("yT", t) for t in tt], W=[("pb", 5)])
                        for x_ in range(3):
                            P.op("act", lambda e, x_=x_: e.activation(out=sga[x_], in_=pbank[x_][:, :], func=AF.Sigmoid),
                                 R=[("pb", x_)], W=ttok(x_))
                        P.op("dve", lambda e: e.tensor_tensor(out=mprod[0], in0=pbank[3][:, :], in1=sga[0], op=ALU.mult), R=[("pb", 3)] + ttok(0), W=ttok(3, 2))
                        P.op("dve", lambda e: e.tensor_tensor(out=mprod[1], in0=pbank[4][:, :], in1=sga[1], op=ALU.mult), R=[("pb", 4)] + ttok(1), W=ttok(5, 2))
                        P.op("pool", lambda e: e.tensor_tensor(out=mprod[0], in0=mprod[0], in1=mprod[1], op=ALU.add), R=ttok(3, 4), W=ttok(3, 2))
                        P.op("dve", lambda e: e.tensor_tensor(out=mprod[1], in0=pbank[5][:, :], in1=sga[2], op=ALU.mult), R=[("pb", 5)] + ttok(2), W=ttok(5, 2))
                        P.op("pool", lambda e, n=n, tsl=tsl: e.tensor_tensor(out=mT[:, n, tsl], in0=mprod[0], in1=mprod[1], op=ALU.add),
                             R=ttok(3, 4), W=[("mT", t) for t in tt])
            if stop_after == "D" and l == nlayers - 1:
                dbg_out("mT", mT, [128, KD, S], BF16, R=ALLMT)
                return True
            w_out_v = w_out[l].rearrange("(k p) n -> p k n", p=128)
            P.dma("pool", wO[:, 0:6, :], w_out_v[:, 0:6, :], W=[("wD", 0, x_) for x_ in range(3)] + ["wO_a"])
            P.dma("pool", wO[:, 6:8, :], w_out_v[:, 6:8, :], W=[("wD", 1, x_) for x_ in range(3)] + VAUGT + ["wO_b"])
            P.op("dve", lambda e: e.memset(sm[:, 32:33], 0.0), W=MIXT + ALLX + ["fence"])
            def emitE(c):
                b_ = c % 2
                P.dma("sp", xin[:, b_], x_src[c * 128:(c + 1) * 128, :], R=[("xs_dram", b), ("xsd", b, c)], W=[("xin", b_)])
                for half in range(2):
                    bk = (c * 2 + half) % 4
                    for n in range(KD):
                        P.op("pe", lambda e, n=n, c=c, half=half, bk=bk: e.matmul(
                            pbank[bk][:, :], lhsT=mT[:, n, c * 128:(c + 1) * 128], rhs=wO[:, n, half * 512:(half + 1) * 512],
                            start=(n == 0), stop=(n == KD - 1)), R=[("mT", c), "wO_a" if n < 6 else "wO_b"], W=[("pb", bk)], cost=230.0)
                    P.op("dve", lambda e, c=c, half=half, bk=bk, b_=b_: e.tensor_tensor(
                        out=xres[:, c, half * 512:(half + 1) * 512], in0=pbank[bk][:, :], in1=xin[:, b_, half * 512:(half + 1) * 512], op=ALU.add),
                        R=[("pb", bk), ("xin", b_)], W=[("xres", c)])
                P.dma("sp", xs_dram[b][c * 128:(c + 1) * 128, :], xres[:, c, :], R=[("xres", c), ("xs_dram", b)], W=[("xsd", b, c)])

            def e_lead(c):
                if c == 0:
                    emitE(0)
                if c + 1 < NT:
                    emitE(c + 1)
            norm_phase(None, gffn_b[l], mT.ap(), from_xres=True, h2_dst=h2_dram[bi], hT_tok="mT", h2i=bi, pre_chunk=e_lead)
            if stop_after == "F" and l == nlayers - 1:
                dbg_out("h2T", mT, [128, KD, S], BF16, R=ALLMT)
                return True
            lg = pbank[0].ap()[:, 0:256].rearrange("p (c e) -> p c e", c=NT)
            for c in range(NT):
                for k in range(KD):
                    P.op("pe", lambda e, c=c, k=k: e.matmul(pbank[0][:, c * 16:(c + 1) * 16], lhsT=mT[:, k, c * 128:(c + 1) * 128], rhs=wr[:, l, k, :],
                                                            start=(k == 0), stop=(k == KD - 1)), R=[("mT", c), "wr"], W=[("pb", 0)])
            SS = [("ss", c) for c in range(NT)]
            AFT = [("aff", bi)]
            av = aff[:, bi // 2, :, (bi % 2) * NE:(bi % 2 + 1) * NE]
            P.op("dve", lambda e: e.tensor_reduce(out=ss[:, :], in_=lg, axis=AX.X, op=ALU.max), R=[("pb", 0)], W=SS)
            P.op("dve", lambda e: e.tensor_tensor(out=av, in0=lg, in1=ss[:, :].unsqueeze(2).to_broadcast([128, NT, NE]), op=ALU.subtract),
                 R=[("pb", 0)] + SS, W=AFT)
            P.op("act", lambda e: e.activation(out=av, in_=av, func=AF.Exp), R=AFT, W=AFT)
            P.op("dve", lambda e: e.tensor_reduce(out=ss[:, :], in_=av, axis=AX.X, op=ALU.add), R=AFT, W=SS)
            P.op("dve", lambda e: e.reciprocal(out=ss[:, :], in_=ss[:, :]), R=SS, W=SS)
            P.op("dve", lambda e: e.tensor_tensor(out=av, in0=av, in1=ss[:, :].unsqueeze(2).to_broadcast([128, NT, NE]), op=ALU.mult),
                 R=AFT + SS, W=AFT)
            if stop_after == "G0" and l == nlayers - 1:
                dbg_out("aff", av, [128, NT, NE], F32, R=AFT)
                return True
        return False

    def moe_body(pair, l):
        NP = len(pair)
        TP = 16 * NP if NP > 1 else 16
        NC_ = NP * 256
        if True:
            P.op("dve", lambda e: e.memset(sm[:, 32:33], 0.0), W=ALLX + MIXT + WEXPT + SYT + WDT + ["fence"])
            wsrc = [w_gate_e, w_up_e, w_down_e]
            nload = [0]

            def load_w(i):
                e_, m_ = divmod(i, 3)
                P.dma("pool", wexp[i % NRING], wsrc[m_][l, e_].rearrange("(k p) n -> p k n", p=128), W=[("wexp", i % NRING)], cost=13000.0)
            while nload[0] < NRING:
                load_w(nload[0])
                nload[0] += 1
            affT = tmp.ap().bitcast(F32)[0:TP, :]
            AFALL = [("aff", bi) for bi in range(NP)]
            for pr in range((NP + 1) // 2):
                nb_ = min(2, NP - 2 * pr) * 16
                for c in range(NT):
                    bk = 1 + c // 4
                    P.op("pe", lambda e, c=c, bk=bk, pr=pr, nb_=nb_: e.transpose(pbank[bk][0:nb_, (c % 4) * 128:(c % 4 + 1) * 128], aff[:, pr, c, 0:nb_], identf[:]),
                         R=[("aff", 2 * pr), ("aff", 2 * pr + 1), "identf"], W=[("pb", bk)])
                for q4 in range(4):
                    P.op("dve", lambda e, q4=q4, pr=pr, nb_=nb_: e.tensor_copy(out=affT[pr * 32:pr * 32 + nb_, q4 * 512:(q4 + 1) * 512], in_=pbank[1 + q4][0:nb_, :]),
                         R=[("pb", 1 + q4)], W=ttok(0, 8))
            for r in range(CAP // 8):
                rs = slice(r * 8, (r + 1) * 8)
                P.op("dve", lambda e, rs=rs: e.max(out=tvals[0:TP, rs], in_=affT), R=ttok(0, 8), W=["tvals"], cost=2300.0)
                P.op("dve", lambda e, rs=rs: e.max_index(out=tidx[0:TP, rs], in_max=tvals[0:TP, rs], in_values=affT), R=ttok(0, 8) + ["tvals"], W=["tidx"], cost=2300.0)
                P.op("dve", lambda e, rs=rs: e.match_replace(out=affT, in_to_replace=tvals[0:TP, rs], in_values=affT, imm_value=-1.0),
                     R=ttok(0, 8) + ["tvals"], W=ttok(0, 8), cost=2300.0)
            P.op("dve", lambda e: e.tensor_copy(out=tidxf[0:TP, :], in_=tidx[0:TP, :]), R=["tidx"], W=["tidxf"])
            for half in range(2):
                P.op("pe", lambda e, half=half: e.transpose(pbank[5][:, half * 112:half * 112 + TP], tidxf[0:TP, half * 128:(half + 1) * 128], identf[0:TP, 0:TP]),
                     R=["tidxf", "identf"], W=[("pb", 5)])
                P.op("pe", lambda e, half=half: e.transpose(pbank[5][:, 224 + half * 112:224 + half * 112 + TP], tvals[0:TP, half * 128:(half + 1) * 128], identf[0:TP, 0:TP]),
                     R=["tvals", "identf"], W=[("pb", 5)])
            P.op("dve", lambda e: e.tensor_copy(out=idxT[:, :, 0:TP], in_=pbank[5][:, 0:224].rearrange("p (h e) -> p h e", h=2)[:, :, 0:TP]), R=[("pb", 5)], W=["idxT"])
            P.op("dve", lambda e: e.tensor_copy(out=valsT[:, :, 0:TP], in_=pbank[5][:, 224:448].rearrange("p (h e) -> p h e", h=2)[:, :, 0:TP]), R=[("pb", 5)], W=["valsT"])
            if stop_after == "G1" and l == nlayers - 1:
                dbg_out("idxT", idxT[:, :, 0:NE], [128, 2, NE], I32, R=["idxT"])
                dbg_out("valsT", valsT[:, :, 0:NE], [128, 2, NE], F32, R=["valsT"])
                return True
            mflat = mT.ap().rearrange("p k t -> p (k t)")
            oe = mflat[:, 0:8192].bitcast(F32).rearrange("p (s d) -> p s d", s=4)
            xg = mflat[:, 8192:12288].rearrange("p (s d) -> p s d", s=4)
            xsT = mflat[:, 12288:16384].rearrange("p (k c) -> p k c", k=KD)
            gT = ptok.ap().rearrange("p c n -> p (c n)").rearrange("p (k c) -> p k c", k=KD)
            PTK = [("ptok", c) for c in range(NT)]
            XG = [("xg", i) for i in range(4)]
            OE = [("oe", i) for i in range(4)]
            P.op("dve", lambda e: e.memset(sm[:, 33:34], 0.0), W=ALLMT + XG + OE + PTK + ["xsT", "gT", "fence2"])

            NSP = (NP + 1) // 2
            steps = [(e_, sp) for e_ in range(NE) for sp in range(NSP)]

            def gather(step):
                e_, sp = steps[step]
                for bl in range(min(2, NP - 2 * sp)):
                    bi = 2 * sp + bl
                    for half in range(2):
                        sc = bl * 2 + half
                        P.op("pool", lambda g_, half=half, bi=bi, sc=sc: g_.indirect_dma_start(
                            out=xg[:, sc, :], out_offset=None, in_=h2_dram[bi][:, :],
                            in_offset=bass.IndirectOffsetOnAxis(ap=idxT[:, half, bi * 16 + e_:bi * 16 + e_ + 1], axis=0)),
                            R=["idxT"] + H2D[bi], W=[("xg", sc)], dma=True, cost=4000.0)
            gather(0)

            def ex_trans(step):
                e_, sp = steps[step]
                NL = min(2, NP - 2 * sp)
                NCL = NL * 256
                for sc in range(2 * NL):
                    tpx = pb_bf(6 + sc % 2)
                    for k in range(KD):
                        P.op("pe", lambda e, k=k, sc=sc, tpx=tpx: e.transpose(tpx[:, k * 128:(k + 1) * 128], xg[:, sc, k * 128:(k + 1) * 128], ident[:]),
                             R=[("xg", sc), "ident"], W=[("pb", 6 + sc % 2)], cost=100.0)
                    P.op("act", lambda e, sc=sc, tpx=tpx: e.activation(out=xsT[:, :, sc * 128:(sc + 1) * 128],
                                                                       in_=tpx[:, 0:1024].rearrange("p (k t) -> p k t", k=KD), func=AF.Copy),
                         R=[("pb", 6 + sc % 2)], W=["xsT"], cost=800.0)
                if step + 1 < len(steps):
                    gather(step + 1)

            def ex_gateup(step):
                e_, sp = steps[step]
                NL = min(2, NP - 2 * sp)
                NCL = NL * 256
                if sp == 0:
                    while nload[0] <= min(3 * e_ + 5, 3 * NE - 1):
                        load_w(nload[0])
                        nload[0] += 1
                sg_, su_, sd_ = (3 * e_) % NRING, (3 * e_ + 1) % NRING, (3 * e_ + 2) % NRING
                mmc = 125.0 * NL
                for f in range(KD):
                    bk = f % 2
                    fs = slice(f * 128, (f + 1) * 128)
                    for k in range(KD):
                        P.op("pe", lambda e, k=k, fs=fs, bk=bk: e.matmul(pbank[bk][:, 0:NCL], lhsT=wexp[sg_][:, k, fs], rhs=xsT[:, k, 0:NCL],
                                                                         start=(k == 0), stop=(k == KD - 1)), R=[("wexp", sg_), "xsT"], W=[("pb", bk)], cost=mmc)
                    for k in range(KD):
                        P.op("pe", lambda e, k=k, fs=fs, bk=bk: e.matmul(pbank[2 + bk][:, 0:NCL], lhsT=wexp[su_][:, k, fs], rhs=xsT[:, k, 0:NCL],
                                                                         start=(k == 0), stop=(k == KD - 1)), R=[("wexp", su_), "xsT"], W=[("pb", 2 + bk)], cost=mmc)
                    P.op("act", lambda e, bk=bk: e.activation(out=sab[:, bk, 0:NCL], in_=pbank[bk][:, 0:NCL], func=AF.Silu), R=[("pb", bk)], W=[("sab", bk)], cost=300.0 * NL)
                    P.op("dve", lambda e, bk=bk, f=f: e.tensor_tensor(out=gT[:, f, 0:NCL], in0=pbank[2 + bk][:, 0:NCL], in1=sab[:, bk, 0:NCL], op=ALU.mult),
                         R=[("pb", 2 + bk), ("sab", bk)], W=["gT"], cost=300.0 * NL)

            def ex_down(step):
                e_, sp = steps[step]
                NL = min(2, NP - 2 * sp)
                NCL = NL * 256
                sg_, su_, sd_ = (3 * e_) % NRING, (3 * e_ + 1) % NRING, (3 * e_ + 2) % NRING
                for sc in range(2 * NL):
                    bl, half = divmod(sc, 2)
                    bi = 2 * sp + bl
                    for dh in range(2):
                        bk = 4 + dh
                        for f in range(KD):
                            P.op("pe", lambda e, f=f, sc=sc, dh=dh, bk=bk: e.matmul(
                                pbank[bk][:, :], lhsT=gT[:, f, sc * 128:(sc + 1) * 128], rhs=wexp[sd_][:, f, dh * 512:(dh + 1) * 512],
                                start=(f == 0), stop=(f == KD - 1)), R=[("wexp", sd_), "gT"], W=[("pb", bk)], cost=230.0)
                        P.op("act", lambda e, sc=sc, dh=dh, bk=bk, bi=bi, half=half: e.activation(
                            out=oe[:, sc, dh * 512:(dh + 1) * 512], in_=pbank[bk][:, :], func=AF.Copy, scale=valsT[:, half, bi * 16 + e_:bi * 16 + e_ + 1]),
                            R=[("pb", bk), "valsT"], W=[("oe", sc)], cost=600.0)
                    P.op("pool", lambda g_, sc=sc, bi=bi, half=half: g_.indirect_dma_start(
                        out=xs_dram[pair[bi]][:, :], out_offset=bass.IndirectOffsetOnAxis(ap=idxT[:, half, bi * 16 + e_:bi * 16 + e_ + 1], axis=0),
                        in_=oe[:, sc, :], in_offset=None, compute_op=ALU.add),
                        R=[("oe", sc), "idxT"] + XSD[pair[bi]], W=[("xs_dram", pair[bi])], dma=True, cost=5000.0)
            ex_trans(0)
            for step in range(len(steps)):
                ex_gateup(step)
                if step + 1 < len(steps):
                    ex_trans(step + 1)
                ex_down(step)
            if stop_after == "G" and l == nlayers - 1:
                dbg_out("xs", xs_dram[0], [S, D], F32, R=[("xs_dram", 0)] + XSD[0])
                return True
        return False

    for p0 in range(0, nseq, GRP):
        pair = list(range(p0, min(p0 + GRP, nseq)))
        for l in range(nlayers):
            for bi, b in enumerate(pair):
                if layer_body(b, l, bi):
                    done = True
                    break
            if done:
                break
            if moe_body(pair, l):
                done = True
                break
        if done:
            break
        P.op("dve", lambda e: e.memset(sm[:, 35:36], 0.0), W=ALLMT + EXPACT + [("fn", i) for i in range(8)] + ["fence3"])
        for b in pair:
            norm_phase(xs_dram[b], gfin_b, None, y_dst=y_out[b], xtok=b)

    if SCHEDULE:
        P.schedule()
    P.emit(nc)
    st.close()
    return nc, dbg


def _host_consts(inp):
    f = lambda a: np.ascontiguousarray(np.asarray(a, dtype=np.float32))
    rep = lambda a, n=128: np.ascontiguousarray(np.broadcast_to(np.asarray(a, np.float32)[:, None, :], (a.shape[0], n, a.shape[1])))
    c = {}
    for k in ("w_in", "w_spatial", "w_pool", "w_attn_o", "w_sgu_o", "w_pool_o", "w_out", "w_router",
              "w_gate_e", "w_up_e", "w_down_e"):
        c[k] = f(inp[k])
    c["gmix_b"] = rep(inp["g_mix"])
    c["gffn_b"] = rep(inp["g_ffn"])
    c["gfin_b"] = np.ascontiguousarray(np.broadcast_to(np.asarray(inp["g_final"], np.float32)[None, :], (128, D)))
    c["gq_b"] = rep(inp["g_q"])
    c["gk_b"] = rep(inp["g_k"])
    c["gsgu_b"] = rep(inp["g_sgu"])
    c["bsp_t"] = np.ascontiguousarray(np.asarray(inp["b_spatial"], np.float32).transpose(0, 2, 1))
    c["psc_t"] = np.ascontiguousarray(np.asarray(inp["pool_scale"], np.float32).reshape(L, 2, 128).transpose(0, 2, 1))
    c["ident"] = np.eye(128, dtype=np.float32)
    c["cs"] = _rope_table()
    c["band"] = _pool_bands()
    return c


def kernel(**inputs):
    x = np.asarray(inputs["x"], dtype=np.float32)
    consts = _host_consts(inputs)
    nc, _ = build_program()
    in_maps = []
    for i in range(N_CORES):
        m = dict(consts)
        m["x"] = np.ascontiguousarray(x[i * NSEQ:(i + 1) * NSEQ])
        in_maps.append(m)
    res = run_bass_kernel_spmd(nc, in_maps, core_ids=list(range(N_CORES)))
    out = np.concatenate([np.asarray(r["y"]) for r in res.results], axis=0)
    return out.astype(np.float32)
```

```python
import numpy as np
import concourse.bass as bass
import concourse.mybir as mybir
from concourse.bass_utils import run_bass_kernel_spmd

F32 = mybir.dt.float32
BF16 = mybir.dt.bfloat16
I32 = mybir.dt.int32
U32 = mybir.dt.uint32
ALU = mybir.AluOpType
AF = mybir.ActivationFunctionType
AX = mybir.AxisListType

D = 1024
S = 2048
NT = 16
KD = 8
L = 2
NSEQ = 4
IN_W = 4608
NE = 16
CAP = 256
EPS = 1e-6
POOL_WINDOWS = (2, 4, 8, 16)
N_CORES = 8
SCHEDULE = True
GRP = 4
STRICT = True


class Op:
    __slots__ = ("eng", "fn", "deps", "signal", "val", "is_dma", "sem_slot", "idx", "prev_same_slot", "odeps", "cost")

    def __init__(self, eng, fn, is_dma):
        self.eng = eng
        self.fn = fn
        self.deps = []
        self.odeps = []
        self.cost = None
        self.signal = False
        self.val = 0
        self.is_dma = is_dma
        self.sem_slot = None
        self.prev_same_slot = None


class Prog:
    ENGS = ("pe", "act", "dve", "pool", "sp")
    NDMA_SEMS = {"sp": 12, "pool": 12, "act": 4}

    def __init__(self):
        self.ops = []
        self.last_w = {}
        self.readers = {}
        self.dma_count = {"sp": 0, "pool": 0, "act": 0}
        self.dma_last = {}

    def _add_dep(self, op, dep, kind):
        if dep is None or dep is op:
            return
        op.odeps.append(dep)
        if (not dep.is_dma) and dep.eng == op.eng and not op.is_dma:
            if op.eng == "pe" or (kind != "raw" and not STRICT):
                return
        op.deps.append(dep)
        dep.signal = True

    def op(self, eng, fn, R=(), W=(), dma=False, cost=None):
        o = Op(eng, fn, dma)
        o.cost = cost
        for t in R:
            self._add_dep(o, self.last_w.get(t), "raw")
        for t in W:
            self._add_dep(o, self.last_w.get(t), "waw")
            for r in self.readers.get(t, ()):
                self._add_dep(o, r, "war")
        for t in R:
            lst = self.readers.setdefault(t, [])
            if not dma:
                lst[:] = [r for r in lst if r.is_dma or r.eng != eng]
            lst.append(o)
        for t in W:
            self.last_w[t] = o
            self.readers[t] = []
        if dma:
            o.signal = True
            self.dma_count[eng] += 1
        self.ops.append(o)
        return o

    def dma(self, queue, out, in_, R=(), W=(), cost=None, **kw):
        return self.op(queue, lambda e: e.dma_start(out=out, in_=in_, **kw), R, W, dma=True, cost=cost)

    DEF_COST = {"pe": 160.0, "act": 450.0, "dve": 450.0, "pool": 800.0}

    def schedule(self, window=24):
        ops = self.ops
        for i, o in enumerate(ops):
            o.idx = i
        pend = {e: [o for o in ops if o.eng == e] for e in self.ENGS}
        head = {e: 0 for e in self.ENGS}
        fin = [None] * len(ops)
        etime = {e: 0.0 for e in self.ENGS}
        order = {e: [] for e in self.ENGS}
        done = [False] * len(ops)
        remaining = len(ops)
        SEM = 120.0

        def cand(e):
            lst = pend[e]
            h = head[e]
            while h < len(lst) and done[lst[h].idx]:
                h += 1
            head[e] = h
            best = None
            seen = 0
            i = h
            while i < len(lst) and seen < window:
                o = lst[i]
                i += 1
                if done[o.idx]:
                    continue
                seen += 1
                st = etime[e]
                ok = True
                for d in o.odeps:
                    f = fin[d.idx]
                    if f is None:
                        ok = False
                        break
                    if d.eng != e or d.is_dma:
                        f = f + SEM
                    elif not o.is_dma and not d.is_dma:
                        f = f if (e == "pe") else f + 60.0
                    if f > st:
                        st = f
                if not ok:
                    continue
                if best is None or st < best[0]:
                    best = (st, o)
                    if st <= etime[e]:
                        break
            return best
        cands = {e: cand(e) for e in self.ENGS}
        while remaining:
            be = None
            for e in self.ENGS:
                c = cands[e]
                if c is not None and (be is None or c[0] < cands[be][0] or (c[0] == cands[be][0] and c[1].idx < cands[be][1].idx)):
                    be = e
            assert be is not None, "scheduler deadlock"
            st, o = cands[be]
            if o.is_dma:
                etime[be] = st + 90.0
                fin[o.idx] = st + (o.cost if o.cost is not None else 3000.0)
            else:
                c = o.cost if o.cost is not None else self.DEF_COST[be]
                etime[be] = st + c
                fin[o.idx] = st + c + (150.0 if be == "pe" else 0.0)
            done[o.idx] = True
            order[be].append(o)
            remaining -= 1
            for e in self.ENGS:
                cands[e] = cand(e)
        self.order = order
        self.sim_time = max(etime.values())

    def emit(self, nc):
        if not hasattr(self, "order"):
            self.order = {e: [o for o in self.ops if o.eng == e] for e in self.ENGS}
        per_eng = self.order
        cnt = {e: 0 for e in self.ENGS}
        for e in self.ENGS:
            ndma = 0
            last_slot = {}
            for o in per_eng[e]:
                if o.is_dma:
                    k = self.NDMA_SEMS[e]
                    o.sem_slot = (e, ndma % k)
                    o.val = 16 * (ndma // k + 1)
                    o.prev_same_slot = last_slot.get(o.sem_slot)
                    last_slot[o.sem_slot] = o
                    ndma += 1
                elif o.signal:
                    cnt[e] += 1
                    o.val = cnt[e]
        self.sig_counts = dict(cnt)
        import contextlib
        with contextlib.ExitStack() as st:
            esem = {e: st.enter_context(nc.semaphore("s_" + e)) for e in self.ENGS}
            dsem = {}
            for q, k in self.NDMA_SEMS.items():
                for i in range(k):
                    dsem[(q, i)] = st.enter_context(nc.semaphore("d_%s%d" % (q, i)))
            block = st.enter_context(nc.Block())

            def sem_of(o):
                return dsem[o.sem_slot] if o.is_dma else esem[o.eng]

            def run(engname, eng):
                waited = {}
                tail = {}
                for o in per_eng[engname]:
                    deps = list(o.deps)
                    if o.is_dma and o.prev_same_slot is not None:
                        deps.append(o.prev_same_slot)
                    for d in deps:
                        s = sem_of(d)
                        key = id(s)
                        if waited.get(key, 0) < d.val:
                            eng.wait_ge(s, d.val)
                            waited[key] = d.val
                    ins = o.fn(eng)
                    if o.signal:
                        ins.then_inc(sem_of(o), 16 if o.is_dma else 1)
                    if o.is_dma:
                        tail[id(sem_of(o))] = (sem_of(o), o.val)
                for s, v in tail.values():
                    if waited.get(id(s), 0) < v:
                        eng.wait_ge(s, v)

            @block.tensor
            def _(e):
                run("pe", e)

            @block.scalar
            def _(e):
                run("act", e)

            @block.vector
            def _(e):
                run("dve", e)

            @block.gpsimd
            def _(e):
                run("pool", e)

            @block.sync
            def _(e):
                run("sp", e)


def _rope_table():
    rows = S // 64
    row = np.broadcast_to(np.arange(rows, dtype=np.float32)[:, None], (rows, 64)).reshape(-1)
    col = np.broadcast_to(np.arange(64, dtype=np.float32)[None, :], (rows, 64)).reshape(-1)
    freqs = (10000.0 ** (-np.arange(0, 32, 2, dtype=np.float32) / 32)).astype(np.float32)
    ang = np.stack([row[:, None] * freqs, col[:, None] * freqs], axis=1)
    cs = np.concatenate([np.cos(ang).reshape(S, 32), np.sin(ang).reshape(S, 32)], axis=1)
    return cs.astype(np.float32)


def _pool_bands():
    out = np.zeros((4, 5, 128, 128), np.float32)
    t = np.arange(S)
    for g, w in enumerate(POOL_WINDOWS):
        lo = np.clip(t - w // 2, 0, S - 1)
        hi = np.clip(t + (w - 1 - w // 2), 0, S - 1)
        cnt = (hi - lo + 1).astype(np.float32)

        def blk(ci, cj):
            m = np.zeros((128, 128), np.float32)
            for tt in range(ci * 128, ci * 128 + 128):
                for tp in range(max(lo[tt], cj * 128), min(hi[tt], cj * 128 + 127) + 1):
                    m[tp - cj * 128, tt - ci * 128] += 1.0 / cnt[tt]
                if ci == cj:
                    m[tt - cj * 128, tt - ci * 128] -= 1.0
            return m
        out[g, 0] = blk(5, 4)
        out[g, 1] = blk(5, 6)
        out[g, 2] = blk(5, 5)
        out[g, 3] = blk(0, 0)
        out[g, 4] = blk(15, 15)
    return out


def build_program(nseq=NSEQ, nlayers=L, debug=None, stop_after=None):
    nc = bass.Bass("TRN2", target_bir_lowering=False)
    P = Prog()
    dbg = {}

    def din(name, shape, dt=F32):
        return nc.dram_tensor(name, list(shape), dt, kind="ExternalInput").ap()

    x_in = din("x", [nseq, S, D])
    w_in = din("w_in", [L, D, IN_W])
    w_spatial = din("w_spatial", [L, 4, 128, 128])
    w_pool = din("w_pool", [L, 4, 64, 64])
    w_attn_o = din("w_attn_o", [L, 512, D])
    w_sgu_o = din("w_sgu_o", [L, 256, D])
    w_pool_o = din("w_pool_o", [L, 256, D])
    w_out = din("w_out", [L, D, D])
    w_router = din("w_router", [L, D, NE])
    w_gate_e = din("w_gate_e", [L, NE, D, D])
    w_up_e = din("w_up_e", [L, NE, D, D])
    w_down_e = din("w_down_e", [L, NE, D, D])
    gmix_b = din("gmix_b", [L, 128, D])
    gffn_b = din("gffn_b", [L, 128, D])
    gfin_b = din("gfin_b", [128, D])
    gq_b = din("gq_b", [L, 128, 64])
    gk_b = din("gk_b", [L, 128, 64])
    gsgu_b = din("gsgu_b", [L, 128, 256])
    bsp_t = din("bsp_t", [L, 128, 4])
    psc_t = din("psc_t", [L, 128, 2])
    ident_in = din("ident", [128, 128])
    cs_in = din("cs", [S, 64])
    band_in = din("band", [4, 5, 128, 128])

    y_out = nc.dram_tensor("y", [nseq, S, D], F32, kind="ExternalOutput").ap()
    xs_dram = [nc.dram_tensor("xs_scr%d" % i, [S, D], F32, kind="Internal").ap() for i in range(nseq)]
    h2_dram = [nc.dram_tensor("h2_scr%d" % i, [S, D], BF16, kind="Internal").ap() for i in range(GRP)]

    import contextlib
    st = contextlib.ExitStack()

    def sb(name, shape, dt):
        return st.enter_context(nc.sbuf_tensor(name, list(shape), dt))

    def ps(name, shape, dt):
        return st.enter_context(nc.psum_tensor(name, list(shape), dt))

    arena = sb("arena", [128, 32768], BF16)
    hT = arena[:, 0:16384].rearrange("p (k t) -> p k t", k=KD)
    qT = arena[:, 16384:24576].rearrange("p (j t) -> p j t", j=4)
    kT2 = arena[:, 24576:32768].rearrange("p (g v t) -> p g v t", g=2, v=2)
    xres = arena.bitcast(F32).rearrange("p (c d) -> p c d", c=NT)
    mT = sb("mT", [128, KD, S], BF16)
    wB = mT.ap().rearrange("p k t -> p (k t)")[:, 0:12288].rearrange("p (s k n) -> p s k n", s=3, k=KD)
    sy = sb("sy", [128, 8192], BF16)
    sgT = sy.ap()[:, 0:4096].rearrange("p (j t) -> p j t", j=2)
    yT = sy.ap()[:, 4096:8192].rearrange("p (j t) -> p j t", j=2)
    wD = sb("wD", [128, 2 * KD * 3 * 256], BF16)
    wDv = wD.ap().rearrange("p (b k x n) -> p b k x n", b=2, k=KD, x=3)
    wO = wD.ap()[:, 0:KD * D].rearrange("p (k n) -> p k n", k=KD)
    vaug = wD.ap()[:, 6144:12288].rearrange("p (c g e) -> p c g e", c=NT, g=2)
    NRING = 6
    wexp = [arena[:, i * 8192:(i + 1) * 8192].rearrange("p (k n) -> p k n", k=KD) for i in range(4)]
    wexp.append(sy.ap().rearrange("p (k n) -> p k n", k=KD))
    wexp.append(wD.ap()[:, 0:8192].rearrange("p (k n) -> p k n", k=KD))
    woA = sb("woA", [128, 2, 4, 256], BF16)
    woB = sb("woB", [128, 2, 2, 256], BF16)
    woC = sb("woC", [128, 2, 2, 256], BF16)
    xin = sb("xin", [128, 2, D], F32)
    hb = sb("hb", [128, 2, D], BF16)
    gnb = sb("gnb", [128, D], F32)
    ss = sb("ss", [128, NT], F32)
    sm = sb("sm", [128, 40], F32)
    epsb = sb("epsb", [128, 1], F32)
    ident = sb("identb", [128, 128], BF16)
    identf = sb("identf", [128, 128], F32)
    cs = sb("cs_sb", [128, NT, 64], F32)
    band = sb("band_sb", [128, 4, 5, 128], BF16)
    wsT = sb("wsT", [128, L, 4, 128], BF16)
    wpl2 = sb("wpl2", [128, L, 2, 128], BF16)
    wr = sb("wr", [128, L, KD, NE], BF16)
    gq = sb("gq", [128, L, 64], F32)
    gk = sb("gk", [128, L, 64], F32)
    gsg = sb("gsg", [128, L, 256], F32)
    bsp = sb("bsp", [128, L, 4], F32)
    psc = sb("psc", [128, L, 2], F32)
    vnb = sb("vnb", [128, 2, 256], BF16)
    sgtok = sb("sgtok", [128, 256], BF16)
    dtok = sb("dtok", [128, 256], BF16)
    ptok = sb("ptok", [128, NT, 256], BF16)
    dTc = sb("dTc", [128, 2, 128], BF16)
    aff = sb("aff", [128, (GRP + 1) // 2, NT, 2 * NE], F32)
    tvals = sb("tvals", [112, CAP], F32)
    tidx = sb("tidx", [112, CAP], U32)
    tidxf = sb("tidxf", [112, CAP], F32)
    idxT = sb("idxT", [128, 2, 112], I32)
    valsT = sb("valsT", [128, 2, 112], F32)
    sab = sb("sab", [128, 2, 512], F32)
    tmp = sb("tmp", [128, 4096], BF16)

    def tslot(i, n=1, dt=BF16):
        v = tmp.ap()[:, i * 512:(i + n) * 512]
        return v if dt == BF16 else v.bitcast(dt)

    def ttok(i, n=1):
        return [("tmp", j) for j in range(i, i + n)]
    wsp_raw = tslot(0, 1).rearrange("p (g q) -> p g q", g=4)
    qn = tslot(0, 2, F32); qr = tslot(2, 2, F32); sqf = tslot(4, 2, F32); qtok = tslot(6); utok = tslot(7, 1, F32)
    pT = [tslot(i) for i in range(3)]
    otok = tslot(3, 4).rearrange("p (c n) -> p c n", c=4)
    sga = [tslot(i) for i in range(3)]
    mprod = [tslot(3, 2, F32), tslot(5, 2, F32)]

    pbank = [ps("pb%d" % i, [128, 512], F32) for i in range(8)]

    def pb_bf(i):
        return pbank[i].ap().bitcast(BF16)

    P.dma("pool", ident[:], ident_in, W=["ident"])
    P.dma("sp", identf[:], ident_in, W=["identf"])
    P.dma("sp", cs[:], cs_in.rearrange("(c p) f -> p c f", p=128), W=["cs"])
    P.dma("pool", band[:], band_in.rearrange("g v a b -> a g v b"), W=["band"])
    P.op("dve", lambda e: e.memset(wpl2[:], 0.0), W=["wpl2"])
    P.op("dve", lambda e: e.memset(epsb[:], EPS), W=["epsb"])
    for l in range(L):
        for g in range(4):
            gg = g % 2
            P.dma("pool", wpl2[gg * 64:(gg + 1) * 64, l, g // 2, gg * 64:(gg + 1) * 64], w_pool[l, g], W=["wpl2"], R=[])
        P.dma("pool", wr[:, l], w_router[l].rearrange("(k p) e -> p k e", p=128), W=["wr"])
        P.dma("sp", gq[:, l], gq_b[l], W=["gq"])
        P.dma("sp", gk[:, l], gk_b[l], W=["gk"])
        P.dma("sp", gsg[:, l], gsgu_b[l], W=["gsg"])
        P.dma("sp", bsp[:, l], bsp_t[l], W=["bsp"])
        P.dma("sp", psc[:, l], psc_t[l], W=["psc"])
        P.dma("pool", wsp_raw, w_spatial[l].rearrange("g p q -> p g q"), W=["wsp_raw"] + ttok(0))
        tp = pb_bf(0)
        for g in range(4):
            P.op("pe", lambda e, g=g, tp=tp: e.transpose(tp[:, g * 128:(g + 1) * 128], wsp_raw[:, g, :], ident[:]),
                 R=["wsp_raw", "ident"] + ttok(0), W=[("pb", 0)])
        P.op("dve", lambda e, l=l, tp=tp: e.tensor_copy(out=wsT[:, l], in_=tp[:, 0:512].rearrange("p (g q) -> p g q", g=4)),
             R=[("pb", 0)], W=["wsT"])

    def rsqrt_eps(ap, toks):
        P.op("act", lambda e: e.activation(out=ap, in_=ap, func=AF.Sqrt, bias=epsb[0:ap.shape[0], 0:1], scale=1.0), R=toks + ["epsb"], W=toks)
        P.op("dve", lambda e: e.reciprocal(out=ap, in_=ap), R=toks, W=toks)

    def norm_phase(x_src_dram, g_dram, to_hT, from_xres=False, h2_dst=None, y_dst=None, hT_tok="hT", pbase=6, xtok=0, h2i=0,
                   pre_chunk=None, post_chunk=None):
        P.dma("sp", gnb[:], g_dram, W=["gnb"])
        if y_dst is not None:
            fnb = mT.ap().rearrange("p k t -> p (k t)").bitcast(F32).rearrange("p (i d) -> p i d", i=8)
        for c in range(NT):
            b = c % 2
            if pre_chunk is not None:
                pre_chunk(c)
            if y_dst is not None:
                bi_, bo_ = c % 4, 4 + c % 4
                P.dma("sp", fnb[:, bi_], x_src_dram[c * 128:(c + 1) * 128, :], R=[("xs_dram", xtok), ("xsd", xtok, c)], W=[("fn", bi_)])
                P.op("act", lambda e, c=c, bi_=bi_, bo_=bo_: e.activation(out=fnb[:, bo_], in_=fnb[:, bi_], func=AF.Square, scale=1.0 / 32.0, accum_out=ss[:, c:c + 1]),
                     R=[("fn", bi_)], W=[("fn", bo_), ("ss", c)])
                rsqrt_eps(ss[:, c:c + 1], [("ss", c)])
                P.op("dve", lambda e, c=c, bi_=bi_, bo_=bo_: e.scalar_tensor_tensor(out=fnb[:, bo_], in0=fnb[:, bi_], scalar=ss[:, c:c + 1],
                                                                                     in1=gnb[:], op0=ALU.mult, op1=ALU.mult),
                     R=[("fn", bi_), ("fn", bo_), ("ss", c), "gnb"], W=[("fn", bo_)])
                P.dma("sp", y_dst[c * 128:(c + 1) * 128, :], fnb[:, bo_], R=[("fn", bo_)], W=[("y_out", xtok, c)])
                continue
            if from_xres:
                src = xres[:, c, :]
                srcR = [("xres", c)]
            else:
                P.dma("sp", xin[:, b], x_src_dram[c * 128:(c + 1) * 128, :], R=[("xs_dram", xtok), ("xsd", xtok, c)], W=[("xin", b)])
                src = xin[:, b]
                srcR = [("xin", b)]
            P.op("act", lambda e, src=src, c=c, b=b: e.activation(out=hb[:, b], in_=src, func=AF.Square, scale=1.0 / 32.0, accum_out=ss[:, c:c + 1]),
                 R=srcR, W=[("hb", b), ("ss", c)])
            rsqrt_eps(ss[:, c:c + 1], [("ss", c)])
            P.op("dve", lambda e, src=src, c=c, b=b: e.scalar_tensor_tensor(out=hb[:, b], in0=src, scalar=ss[:, c:c + 1],
                                                                             in1=gnb[:], op0=ALU.mult, op1=ALU.mult),
                 R=srcR + [("ss", c), "gnb"], W=[("hb", b)])
            if h2_dst is not None:
                P.dma("sp", h2_dst[c * 128:(c + 1) * 128, :], hb[:, b], R=[("hb", b)], W=[("h2d", h2i, c)])
            bk = pbase + (c % 2)
            tp = pb_bf(bk)
            for k in range(KD):
                P.op("pe", lambda e, k=k, b=b, tp=tp: e.transpose(tp[:, k * 128:(k + 1) * 128], hb[:, b, k * 128:(k + 1) * 128], ident[:]),
                     R=[("hb", b), "ident"], W=[("pb", bk)])
            P.op("act", lambda e, c=c, tp=tp: e.activation(out=to_hT[:, :, c * 128:(c + 1) * 128],
                                                           in_=tp[:, 0:1024].rearrange("p (k t) -> p k t", k=KD), func=AF.Copy),
                 R=[("pb", bk)], W=[(hT_tok, c)])
            if post_chunk is not None:
                post_chunk(c)

    def headnorm_rope(src_ps, nh, gain, c, dst_bf, Rsrc, Wdst):
        w = nh * 64
        P.op("act", lambda e: e.activation(out=sqf[:, 0:w], in_=src_ps, func=AF.Square, scale=0.125), R=Rsrc, W=ttok(4, 2))
        P.op("dve", lambda e: e.tensor_reduce(out=sm[:, 0:nh], in_=sqf[:, 0:w].rearrange("p (h d) -> p h d", h=nh), axis=AX.X, op=ALU.add),
             R=ttok(4, 2), W=["sm"])
        rsqrt_eps(sm[:, 0:nh], ["sm"])
        P.op("dve", lambda e: e.tensor_tensor(out=qn[:, 0:w].rearrange("p (h d) -> p h d", h=nh),
                                              in0=src_ps.rearrange("p (h d) -> p h d", h=nh),
                                              in1=sm[:, 0:nh].unsqueeze(2).to_broadcast([128, nh, 64]), op=ALU.mult),
             R=Rsrc + ["sm"], W=ttok(0, 2))
        P.op("dve", lambda e: e.tensor_tensor(out=qn[:, 0:w].rearrange("p (h d) -> p h d", h=nh),
                                              in0=qn[:, 0:w].rearrange("p (h d) -> p h d", h=nh),
                                              in1=gain.unsqueeze(1).to_broadcast([128, nh, 64]), op=ALU.mult),
             R=ttok(0, 2) + ["gq", "gk"], W=ttok(0, 2))
        m = nh * 2
        qv = qn[:, 0:w].rearrange("p (m two f) -> p m two f", m=m, two=2)
        rv = qr[:, 0:w].rearrange("p (m two f) -> p m two f", m=m, two=2)
        dv = dst_bf.rearrange("p (m two f) -> p m two f", m=m, two=2)
        cosb = cs[:, c, 0:32].rearrange("p (a f) -> p a f", a=2)
        sinb = cs[:, c, 32:64].rearrange("p (a f) -> p a f", a=2)

        def bc(t):
            return t.unsqueeze(1).to_broadcast([128, nh, 2, 16])
        x1 = qn[:, 0:w].rearrange("p (h a two f) -> p h a two f", h=nh, a=2, two=2)
        r_ = qr[:, 0:w].rearrange("p (h a two f) -> p h a two f", h=nh, a=2, two=2)
        d_ = dst_bf.rearrange("p (h a two f) -> p h a two f", h=nh, a=2, two=2)
        P.op("dve", lambda e: e.tensor_tensor(out=r_[:, :, :, 0, :], in0=x1[:, :, :, 0, :], in1=bc(cosb), op=ALU.mult), R=ttok(0, 2) + ["cs"], W=ttok(2, 2))
        P.op("dve", lambda e: e.tensor_tensor(out=r_[:, :, :, 1, :], in0=x1[:, :, :, 1, :], in1=bc(sinb), op=ALU.mult), R=ttok(0, 2) + ["cs"], W=ttok(2, 2))
        P.op("dve", lambda e: e.tensor_tensor(out=d_[:, :, :, 0, :], in0=r_[:, :, :, 0, :], in1=r_[:, :, :, 1, :], op=ALU.subtract), R=ttok(2, 2), W=Wdst)
        P.op("dve", lambda e: e.tensor_tensor(out=r_[:, :, :, 0, :], in0=x1[:, :, :, 1, :], in1=bc(cosb), op=ALU.mult), R=ttok(0, 2) + ["cs"], W=ttok(2, 2))
        P.op("dve", lambda e: e.tensor_tensor(out=r_[:, :, :, 1, :], in0=x1[:, :, :, 0, :], in1=bc(sinb), op=ALU.mult), R=ttok(0, 2) + ["cs"], W=ttok(2, 2))
        P.op("dve", lambda e: e.tensor_tensor(out=d_[:, :, :, 1, :], in0=r_[:, :, :, 0, :], in1=r_[:, :, :, 1, :], op=ALU.add), R=ttok(2, 2), W=Wdst)

    def dbg_out(name, ap_sb, shape, dt=F32, R=()):
        t = nc.dram_tensor("dbg_" + name, list(shape), dt, kind="ExternalOutput").ap()
        if hasattr(ap_sb, "ap") and callable(getattr(ap_sb, "ap")):
            ap_sb = ap_sb.ap()
        P.dma("sp", t, ap_sb, R=list(R), W=["dbg_" + name])
        dbg[name] = t

    ALLHT = [("hT", c) for c in range(NT)]
    ALLQT = [("qT", c) for c in range(NT)]
    ALLMT = [("mT", c) for c in range(NT)]
    ALLX = [("xres", c) for c in range(NT)]
    ALLQT = [("qT", c, j, hp) for c in range(NT) for j in range(4) for hp in range(2)]
    VAUGT = [("vaug", c) for c in range(NT)] + ["vaug_ones"]
    MIXT = ALLHT + ALLQT + [("kT2", c) for c in range(NT)] + [("kT2b", c) for c in range(NT)] + ["kT2_zero"] + VAUGT
    WEXPT = [("wexp", i) for i in range(6)]
    SYT = [("sgT", c) for c in range(NT)] + [("yT", c) for c in range(NT)]
    WDT = [("wD", bb, x_) for bb in range(2) for x_ in range(3)] + ["wO_a", "wO_b"]
    WBT = [("wB", s) for s in range(3)]
    SUBB = [("pb", 3), ("pb", 4), ("pb", 5), ("pb", 3), ("pb", 3), ("pb", 4), ("pb", 4), ("pb", 5), ("pb", 5), "sq_", "qr_", ("utok_", 0), ("utok_", 1)]
    XSD = [[("xsd", b_, c) for c in range(NT)] for b_ in range(nseq)]
    H2D = [[("h2d", i_, c) for c in range(NT)] for i_ in range(GRP)]
    EXPACT = [("xg", i) for i in range(4)] + [("oe", i) for i in range(4)] + ["xsT", "gT"]
    done = False
    def layer_body(b, l, bi):
        if True:
            x_src = x_in[b] if l == 0 else xs_dram[b]
            P.op("dve", lambda e: e.memset(sm[:, 32:33], 0.0), W=MIXT + ALLX + WEXPT + ALLMT + WBT + EXPACT + SYT + WDT + SUBB + [("fn", i) for i in range(8)] + ["fence"])
            if stop_after == "A00" and l == nlayers - 1:
                dbg_out("xs", xs_dram[0], [S, D], F32, R=[("xs_dram", 0)] + XSD[0])
                return True
            P.op("pool", lambda e: e.memset(kT2[64:128, :, 0, :], 0.0), W=["kT2_zero"])
            P.op("pool", lambda e: e.memset(kT2[0:64, :, 1, :], 0.0), W=["kT2_zero"])
            P.op("dve", lambda e: e.memset(vaug[:, :, :, 0:64], 1.0), W=["vaug_ones"])
            P.op("dve", lambda e: e.memset(vaug[:, :, :, 128:192], 1.0), W=["vaug_ones"])
            for s in range(3):
                P.dma("pool", wB[:, s], w_in[l][:, s * 512:(s + 1) * 512].rearrange("(k p) n -> p k n", p=128),
                      W=ALLMT + [("wB", s)] if s == 0 else [("wB", s)], R=[])
            if stop_after == "A0" and l == nlayers - 1:
                dbg_out("xs", xs_dram[0], [S, D], F32, R=[("xs_dram", 0)] + XSD[0])
                return True
            if stop_after == "A" and l == nlayers - 1:
                dbg_out("hT", hT, [128, KD, S], BF16, R=ALLHT)
                if l > 0:
                    dbg_out("xs", xs_dram[0], [S, D], F32, R=[("xs_dram", 0)] + XSD[0])
                return True
            mtail = mT.ap().rearrange("p k t -> p (k t)")[:, 12288:16384]
            qkb = [tslot(0, 3, F32)[:, 0:640], tslot(3, 3, F32)[:, 0:640]]
            qkt = [ttok(0, 3), ttok(3, 3)]
            qtok2 = tslot(6, 2)[:, 0:640]
            sq_ = mtail[:, 0:1280].bitcast(F32)
            qr_ = mtail[:, 1280:2560].bitcast(F32)
            utk = [mtail[:, 2560:3072].bitcast(F32), mtail[:, 3072:3584].bitcast(F32)]

            def pool_chunk(c):
                for g in range(4):
                    terms = []
                    if c > 0:
                        terms.append((c - 1, 0))
                    terms.append((c, 3 if c == 0 else (4 if c == NT - 1 else 2)))
                    if c < NT - 1:
                        terms.append((c + 1, 1))
                    for i, (cj, v) in enumerate(terms):
                        P.op("pe", lambda e, g=g, cj=cj, v=v, i=i, n=len(terms): e.matmul(
                            pbank[5][:, 256 + g * 64:256 + (g + 1) * 64], lhsT=band[:, g, v, :], rhs=ptok[:, cj, g * 64:(g + 1) * 64],
                            start=(i == 0), stop=(i == n - 1)), R=[("ptok", cj), "band"], W=[("pb", 5)])
                P.op("act", lambda e: e.activation(out=dtok[:, :], in_=pbank[5][:, 256:512], func=AF.Copy), R=[("pb", 5)], W=["dtok"])
                tpd = pb_bf(3)
                for j in range(2):
                    P.op("pe", lambda e, j=j: e.transpose(tpd[:, 512 + j * 128:512 + (j + 1) * 128], dtok[:, j * 128:(j + 1) * 128], ident[:]),
                         R=["dtok", "ident"], W=[("pb", 3)])
                P.op("act", lambda e: e.activation(out=dTc[:, :, :], in_=tpd[:, 512:768].rearrange("p (j t) -> p j t", j=2), func=AF.Copy),
                     R=[("pb", 3)], W=["dTc"])
                for j in range(2):
                    P.op("pe", lambda e, j=j: e.matmul(pbank[4][:, 256 + j * 128:256 + (j + 1) * 128], lhsT=wpl2[:, l, j, :], rhs=dTc[:, j, :],
                                                       start=True, stop=True), R=["dTc", "wpl2"], W=[("pb", 4)])
                P.op("dve", lambda e: e.tensor_tensor(out=yT[:, :, c * 128:(c + 1) * 128], in0=pbank[4][:, 256:512].rearrange("p (j t) -> p j t", j=2),
                                                      in1=psc[:, l, :].unsqueeze(2).to_broadcast([128, 2, 128]), op=ALU.mult),
                     R=[("pb", 4), "psc"], W=[("yT", c)])

            def projB1(c):
                cb = c % 2
                qk = qkb[cb]
                QK = qkt[cb]
                vn_ = vnb[:, cb, :]
                VN = [("vn", cb)]
                utok_ = utk[cb]
                UT = [("utok_", cb)]
                for s_ in range(3):
                    for k in range(KD):
                        P.op("pe", lambda e, s_=s_, k=k: e.matmul(pbank[s_][:, :], lhsT=hT[:, k, c * 128:(c + 1) * 128], rhs=wB[:, s_, k, :],
                                                                  start=(k == 0), stop=(k == KD - 1)),
                             R=[("hT", c), ("wB", s_)], W=[("pb", s_)])
                P.op("act", lambda e: e.activation(out=qk[:, 0:512], in_=pbank[0][:, :], func=AF.Copy), R=[("pb", 0)], W=QK)
                P.op("act", lambda e: e.activation(out=qk[:, 512:640], in_=pbank[1][:, 0:128], func=AF.Copy), R=[("pb", 1)], W=QK)
                P.op("act", lambda e: e.activation(out=sq_[:, :], in_=qk[:, :], func=AF.Square, scale=0.125), R=QK, W=["sq_"])
                P.op("act", lambda e: e.activation(out=vaug[:, c, :, 64:128], in_=pbank[1][:, 128:256].rearrange("p (g d) -> p g d", g=2), func=AF.Copy),
                     R=[("pb", 1)], W=[("vaug", c)])
                P.op("act", lambda e: e.activation(out=utok_[:, :], in_=pbank[1][:, 256:512], func=AF.Copy), R=[("pb", 1)], W=UT)
                P.op("dve", lambda e: e.tensor_reduce(out=sm[:, 0:10], in_=sq_[:, :].rearrange("p (h d) -> p h d", h=10), axis=AX.X, op=ALU.add),
                     R=["sq_"], W=["sm"])
                rsqrt_eps(sm[:, 0:10], ["sm"])
                P.op("dve", lambda e: e.tensor_tensor(out=qk[:, :].rearrange("p (h d) -> p h d", h=10),
                                                      in0=qk[:, :].rearrange("p (h d) -> p h d", h=10),
                                                      in1=sm[:, 0:10].unsqueeze(2).to_broadcast([128, 10, 64]), op=ALU.mult),
                     R=QK + ["sm"], W=QK)
                P.op("act", lambda e: e.activation(out=vn_, in_=pbank[2][:, 0:256], func=AF.Square, scale=1.0 / 16.0, accum_out=sm[:, 16:17]),
                     R=[("pb", 2)], W=VN + ["sm2"])
                rsqrt_eps(sm[:, 16:17], ["sm2"])
                P.op("dve", lambda e: e.scalar_tensor_tensor(out=vn_, in0=pbank[2][:, 0:256], scalar=sm[:, 16:17], in1=gsg[:, l, :],
                                                             op0=ALU.mult, op1=ALU.mult), R=[("pb", 2), "sm2", "gsg"], W=VN)
                P.op("act", lambda e: e.activation(out=ptok[:, c, :], in_=pbank[2][:, 256:512], func=AF.Copy), R=[("pb", 2)], W=[("ptok", c)])
            def projB2(c):
                cb = c % 2
                qk = qkb[cb]
                QK = qkt[cb]
                vn_ = vnb[:, cb, :]
                VN = [("vn", cb)]
                utok_ = utk[cb]
                UT = [("utok_", cb)]
                P.op("dve", lambda e: e.tensor_tensor(out=qk[:, 0:512].rearrange("p (h d) -> p h d", h=8),
                                                       in0=qk[:, 0:512].rearrange("p (h d) -> p h d", h=8),
                                                       in1=gq[:, l, :].unsqueeze(1).to_broadcast([128, 8, 64]), op=ALU.mult), R=QK + ["gq"], W=QK)
                P.op("dve", lambda e: e.tensor_tensor(out=qk[:, 512:640].rearrange("p (h d) -> p h d", h=2),
                                                       in0=qk[:, 512:640].rearrange("p (h d) -> p h d", h=2),
                                                       in1=gk[:, l, :].unsqueeze(1).to_broadcast([128, 2, 64]), op=ALU.mult), R=QK + ["gk"], W=QK)
                x1 = qk[:, :].rearrange("p (h a two f) -> p h a two f", h=10, a=2, two=2)
                r_ = qr_[:, :].rearrange("p (h a two f) -> p h a two f", h=10, a=2, two=2)
                d_ = qtok2.rearrange("p (h a two f) -> p h a two f", h=10, a=2, two=2)
                cosb = cs[:, c, 0:32].rearrange("p (a f) -> p a f", a=2).unsqueeze(1).to_broadcast([128, 10, 2, 16])
                sinb = cs[:, c, 32:64].rearrange("p (a f) -> p a f", a=2).unsqueeze(1).to_broadcast([128, 10, 2, 16])
                QT2 = ttok(6, 2)
                P.op("pool", lambda e: e.tensor_tensor(out=r_[:, :, :, 0, :], in0=x1[:, :, :, 0, :], in1=cosb, op=ALU.mult), R=QK + ["cs"], W=["qr_"])
                P.op("pool", lambda e: e.tensor_tensor(out=r_[:, :, :, 1, :], in0=x1[:, :, :, 1, :], in1=sinb, op=ALU.mult), R=QK + ["cs"], W=["qr_"])
                P.op("pool", lambda e: e.tensor_tensor(out=d_[:, :, :, 0, :], in0=r_[:, :, :, 0, :], in1=r_[:, :, :, 1, :], op=ALU.subtract), R=["qr_"], W=QT2)
                P.op("pool", lambda e: e.tensor_tensor(out=r_[:, :, :, 0, :], in0=x1[:, :, :, 1, :], in1=cosb, op=ALU.mult), R=QK + ["cs"], W=["qr_"])
                P.op("pool", lambda e: e.tensor_tensor(out=r_[:, :, :, 1, :], in0=x1[:, :, :, 0, :], in1=sinb, op=ALU.mult), R=QK + ["cs"], W=["qr_"])
                P.op("pool", lambda e: e.tensor_tensor(out=d_[:, :, :, 1, :], in0=r_[:, :, :, 0, :], in1=r_[:, :, :, 1, :], op=ALU.add), R=["qr_"], W=QT2)
                tp = pb_bf(3)
                for j in range(4):
                    P.op("pe", lambda e, j=j: e.transpose(tp[:, j * 128:(j + 1) * 128], qtok2[:, j * 128:(j + 1) * 128], ident[:]),
                         R=QT2 + ["ident"], W=[("pb", 3)])
                P.op("act", lambda e: e.activation(out=qT[:, :, c * 128:(c + 1) * 128],
                                                   in_=tp[:, 0:512].rearrange("p (j t) -> p j t", j=4), func=AF.Copy),
                     R=[("pb", 3)], W=[("qT", c, j_, hp_) for j_ in range(4) for hp_ in range(2)])
                tpk = pb_bf(4)
                for g in range(2):
                    for hh in range(2):
                        P.op("pe", lambda e, g=g, hh=hh: e.transpose(
                            tpk[hh * 64:(hh + 1) * 64, g * 128:(g + 1) * 128], qtok2[:, 512 + g * 64:512 + (g + 1) * 64], ident[:]),
                            R=QT2 + ["ident"], W=[("pb", 4)])
                P.op("act", lambda e: e.activation(out=kT2[0:64, :, 0, c * 128:(c + 1) * 128],
                                                   in_=tpk[0:64, 0:256].rearrange("p (g t) -> p g t", g=2), func=AF.Copy),
                     R=[("pb", 4)], W=[("kT2", c)])
                P.op("dve", lambda e: e.tensor_copy(out=kT2[64:128, :, 1, c * 128:(c + 1) * 128],
                                                    in_=tpk[64:128, 0:256].rearrange("p (g t) -> p g t", g=2)),
                     R=[("pb", 4)], W=[("kT2b", c)])
                for g in range(4):
                    P.op("pe", lambda e, g=g: e.matmul(pbank[5][:, g * 64:(g + 1) * 64], lhsT=wsT[:, l, g, :], rhs=vn_[:, g * 64:(g + 1) * 64],
                                                       start=True, stop=True), R=VN + ["wsT"], W=[("pb", 5)])
                P.op("dve", lambda e: e.tensor_tensor(out=sgtok[:, :].rearrange("p (g d) -> p g d", g=4),
                                                      in0=pbank[5][:, 0:256].rearrange("p (g d) -> p g d", g=4),
                                                      in1=bsp[:, l, :].unsqueeze(2).to_broadcast([128, 4, 64]), op=ALU.add),
                     R=[("pb", 5), "bsp"], W=["sgtok"])
                P.op("pool", lambda e: e.tensor_tensor(out=sgtok[:, :], in0=sgtok[:, :], in1=utok_[:, :], op=ALU.mult), R=["sgtok"] + UT, W=["sgtok"])
                tps = pb_bf(4)
                for j in range(2):
                    P.op("pe", lambda e, j=j: e.transpose(tps[:, 256 + j * 128:256 + (j + 1) * 128], sgtok[:, j * 128:(j + 1) * 128], ident[:]),
                         R=["sgtok", "ident"], W=[("pb", 4)])
                P.op("act", lambda e: e.activation(out=sgT[:, :, c * 128:(c + 1) * 128],
                                                   in_=tps[:, 256:512].rearrange("p (j t) -> p j t", j=2), func=AF.Copy),
                     R=[("pb", 4)], W=[("sgT", c)])

            def stepB(c):
                if c + 1 < NT:
                    projB1(c + 1)
                projB2(c)
                if c >= 1:
                    pool_chunk(c - 1)

            def ab_post(c):
                if c == 1:
                    projB1(0)
                if c >= 2:
                    stepB(c - 2)
            norm_phase(x_src, gmix_b[l], hT, xtok=b, post_chunk=ab_post)
            stepB(NT - 2)
            stepB(NT - 1)
            pool_chunk(NT - 1)
            P.op("dve", lambda e: e.memset(sm[:, 34:35], 0.0),
                 W=[("pb", 3), ("pb", 4), ("pb", 5), ("pb", 3), ("pb", 3), ("pb", 4), ("pb", 4), ("pb", 5), ("pb", 5), "sq_", "qr_", ("utok_", 0), ("utok_", 1), "fenceB"])
            if stop_after == "B" and l == nlayers - 1:
                dbg_out("qT", qT, [128, 4, S], BF16, R=ALLQT)
                dbg_out("kT2", kT2, [128, 2, 2, S], BF16, R=[("kT2", c) for c in range(NT)] + [("kT2b", c) for c in range(NT)] + ["kT2_zero"])
                dbg_out("vaug", vaug, [128, NT, 2, 192], BF16, R=[("vaug", c) for c in range(NT)] + ["vaug_ones"])
                dbg_out("sgT", sgT, [128, 2, S], BF16, R=[("sgT", c) for c in range(NT)])
                dbg_out("yT", yT, [128, 2, S], BF16, R=[("yT", c) for c in range(NT)])
                return True
            items = [(qb, h, s_) for qb in range(4) for h in range(8) for s_ in range(NT)]
            recb = tslot(3, 2, F32)
            RECT = ttok(3, 2)
            NSB = 4
            SB_ = [0, 1, 2, 5]
            PS_ = [0, 1, 2, 5]

            def qk_exp(i):
                qb, h, s_ = items[i]
                g = h // 4
                hp = h % 2
                ho = hp * 64
                j = h // 2
                sbk = SB_[i % NSB]
                pts = PS_[i % NSB]
                P.op("pe", lambda e: e.matmul(
                    pbank[sbk][:, :], lhsT=kT2[:, g, hp, s_ * 128:(s_ + 1) * 128], rhs=qT[:, j, qb * 512:(qb + 1) * 512],
                    start=True, stop=True),
                    R=[("kT2", s_), ("kT2b", s_), "kT2_zero"] + [("qT", qb * 4 + i_, j, hp_) for i_ in range(4) for hp_ in range(2)],
                    W=[("pb", sbk)], cost=230.0)
                P.op("act", lambda e: e.activation(out=tslot(pts), in_=pbank[sbk][:, :], func=AF.Exp, scale=0.125),
                     R=[("pb", sbk)], W=ttok(pts), cost=560.0)

            def pv(i):
                qb, h, s_ = items[i]
                g = h // 4
                hp = h % 2
                j = h // 2
                pts = PS_[i % NSB]
                pob = 3 + (h % 2)
                win = slice(64, 192) if hp == 0 else slice(0, 128)
                P.op("pe", lambda e: e.matmul(pbank[pob][:, :], lhsT=vaug[:, s_, g, win], rhs=tslot(pts),
                                              start=(s_ == 0), stop=(s_ == NT - 1)),
                     R=ttok(pts) + [("vaug", s_), "vaug_ones"], W=[("pb", pob)], cost=230.0)
                if s_ == NT - 1:
                    op_ = slice(0, 64) if hp == 0 else slice(64, 128)
                    dp_ = slice(64, 128) if hp == 0 else slice(0, 64)
                    P.op("dve", lambda e: e.reciprocal(out=recb[dp_, :], in_=pbank[pob][dp_, :]), R=[("pb", pob)], W=RECT, cost=600.0)
                    P.op("dve", lambda e: e.tensor_tensor(out=qT[op_, j, qb * 512:(qb + 1) * 512], in0=pbank[pob][op_, :], in1=recb[dp_, :], op=ALU.mult),
                         R=[("pb", pob)] + RECT, W=[("qT", qb * 4 + i_, j, hp) for i_ in range(4)], cost=600.0)
            LOOK = 3
            for i in range(min(LOOK, len(items))):
                qk_exp(i)
            for i in range(len(items)):
                if i + LOOK < len(items):
                    qk_exp(i + LOOK)
                pv(i)
            if stop_after == "C" and l == nlayers - 1:
                dbg_out("oT", qT, [128, 4, S], BF16, R=ALLQT)
                return True
            P.op("dve", lambda e: e.memset(sm[:, 32:33], 0.0), W=ALLMT + WBT + ["sq_", "qr_", ("utok_", 0), ("utok_", 1), "fence"])

            def load_D(ns):
                buf = ns % 2
                for x_ in range(3):
                    c0 = 1536 + x_ * 1024 + ns * 256
                    P.dma("pool", wDv[:, buf, :, x_, :], w_in[l][:, c0:c0 + 256].rearrange("(k p) n -> p k n", p=128),
                          W=[("wD", buf, x_)] + (VAUGT if buf == 1 else []))
                P.dma("pool", woA[:, buf], w_attn_o[l][:, ns * 256:(ns + 1) * 256].rearrange("(j p) n -> p j n", p=128), W=[("woA", buf)])
                P.dma("pool", woB[:, buf], w_sgu_o[l][:, ns * 256:(ns + 1) * 256].rearrange("(j p) n -> p j n", p=128), W=[("woB", buf)])
                P.dma("pool", woC[:, buf], w_pool_o[l][:, ns * 256:(ns + 1) * 256].rearrange("(j p) n -> p j n", p=128), W=[("woC", buf)])
            load_D(0)
            for ns in range(4):
                buf = ns % 2
                if ns + 1 < 4:
                    load_D(ns + 1)
                for tb in range(4):
                    tsl = slice(tb * 512, (tb + 1) * 512)
                    tt = [tb * 4 + i for i in range(4)]
                    for nn in range(2):
                        n = ns * 2 + nn
                        nsl = slice(nn * 128, (nn + 1) * 128)
                        for x_ in range(3):
                            for k in range(KD):
                                P.op("pe", lambda e, x_=x_, k=k, buf=buf, nsl=nsl, tsl=tsl: e.matmul(
                                    pbank[x_][:, :], lhsT=wDv[:, buf, k, x_, nsl], rhs=hT[:, k, tsl], start=(k == 0), stop=(k == KD - 1)),
                                    R=[("wD", buf, x_)] + [("hT", t) for t in tt], W=[("pb", x_)])
                        for j in range(4):
                            P.op("pe", lambda e, j=j, buf=buf, nsl=nsl, tsl=tsl: e.matmul(
                                pbank[3][:, :], lhsT=woA[:, buf, j, nsl], rhs=qT[:, j, tsl], start=(j == 0), stop=(j == 3)),
                                R=[("woA", buf)] + [("qT", t, j, hp_) for t in tt for hp_ in range(2)], W=[("pb", 3)])
                        for j in range(2):
                            P.op("pe", lambda e, j=j, buf=buf, nsl=nsl, tsl=tsl: e.matmul(
                                pbank[4][:, :], lhsT=woB[:, buf, j, nsl], rhs=sgT[:, j, tsl], start=(j == 0), stop=(j == 1)),
                                R=[("woB", buf)] + [("sgT", t) for t in tt], W=[("pb", 4)])
                        for j in range(2):
                            P.op("pe", lambda e, j=j, buf=buf, nsl=nsl, tsl=tsl: e.matmul(
                                pbank[5][:, :], lhsT=woC[:, buf, j, nsl], rhs=yT[:, j, tsl], start=(j == 0), stop=(j == 1)),
                                R=[("woC", buf)] + [("yT", t) for t in tt], W=[("pb", 5)])
                        for x_ in range(3):
                            P.op("act", lambda e, x_=x_: e.activation(out=sga[x_], in_=pbank[x_][:, :], func=AF.Sigmoid),
                                 R=[("pb", x_)], W=ttok(x_))
                        P.op("dve", lambda e: e.tensor_tensor(out=mprod[0], in0=pbank[3][:, :], in1=sga[0], op=ALU.mult), R=[("pb", 3)] + ttok(0), W=ttok(3, 2))
                        P.op("dve", lambda e: e.tensor_tensor(out=mprod[1], in0=pbank[4][:, :], in1=sga[1], op=ALU.mult), R=[("pb", 4)] + ttok(1), W=ttok(5, 2))
                        P.op("pool", lambda e: e.tensor_tensor(out=mprod[0], in0=mprod[0], in1=mprod[1], op=ALU.add), R=ttok(3, 4), W=ttok(3, 2))
                        P.op("dve", lambda e: e.tensor_tensor(out=mprod[1], in0=pbank[5][:, :], in1=sga[2], op=ALU.mult), R=[("pb", 5)] + ttok(2), W=ttok(5, 2))
                        P.op("pool", lambda e, n=n, tsl=tsl: e.tensor_tensor(out=mT[:, n, tsl], in0=mprod[0], in1=mprod[1], op=ALU.add),
                             R=ttok(3, 4), W=[("mT", t) for t in tt])
            if stop_after == "D" and l == nlayers - 1:
                dbg_out("mT", mT, [128, KD, S], BF16, R=ALLMT)
                return True
            w_out_v = w_out[l].rearrange("(k p) n -> p k n", p=128)
            P.dma("pool", wO[:, 0:6, :], w_out_v[:, 0:6, :], W=[("wD", 0, x_) for x_ in range(3)] + ["wO_a"])
            P.dma("pool", wO[:, 6:8, :], w_out_v[:, 6:8, :], W=[("wD", 1, x_) for x_ in range(3)] + VAUGT + ["wO_b"])
            P.op("dve", lambda e: e.memset(sm[:, 32:33], 0.0), W=MIXT + ALLX + ["fence"])
            def emitE(c):
                b_ = c % 2
                P.dma("sp", xin[:, b_], x_src[c * 128:(c + 1) * 128, :], R=[("xs_dram", b), ("xsd", b, c)], W=[("xin", b_)])
                for half in range(2):
                    bk = (c * 2 + half) % 4
                    for n in range(KD):
                        P.op("pe", lambda e, n=n, c=c, half=half, bk=bk: e.matmul(
                            pbank[bk][:, :], lhsT=mT[:, n, c * 128:(c + 1) * 128], rhs=wO[:, n, half * 512:(half + 1) * 512],
                            start=(n == 0), stop=(n == KD - 1)), R=[("mT", c), "wO_a" if n < 6 else "wO_b"], W=[("pb", bk)], cost=230.0)
                    P.op("dve", lambda e, c=c, half=half, bk=bk, b_=b_: e.tensor_tensor(
                        out=xres[:, c, half * 512:(half + 1) * 512], in0=pbank[bk][:, :], in1=xin[:, b_, half * 512:(half + 1) * 512], op=ALU.add),
                        R=[("pb", bk), ("xin", b_)], W=[("xres", c)])
                P.dma("sp", xs_dram[b][c * 128:(c + 1) * 128, :], xres[:, c, :], R=[("xres", c), ("xs_dram", b)], W=[("xsd", b, c)])

            def e_lead(c):
                if c == 0:
                    emitE(0)
                if c + 1 < NT:
                    emitE(c + 1)
            norm_phase(None, gffn_b[l], mT.ap(), from_xres=True, h2_dst=h2_dram[bi], hT_tok="mT", h2i=bi, pre_chunk=e_lead)
            if stop_after == "F" and l == nlayers - 1:
                dbg_out("h2T", mT, [128, KD, S], BF16, R=ALLMT)
                return True
            lg = pbank[0].ap()[:, 0:256].rearrange("p (c e) -> p c e", c=NT)
            for c in range(NT):
                for k in range(KD):
                    P.op("pe", lambda e, c=c, k=k: e.matmul(pbank[0][:, c * 16:(c + 1) * 16], lhsT=mT[:, k, c * 128:(c + 1) * 128], rhs=wr[:, l, k, :],
                                                            start=(k == 0), stop=(k == KD - 1)), R=[("mT", c), "wr"], W=[("pb", 0)])
            SS = [("ss", c) for c in range(NT)]
            AFT = [("aff", bi)]
            av = aff[:, bi // 2, :, (bi % 2) * NE:(bi % 2 + 1) * NE]
            P.op("dve", lambda e: e.tensor_reduce(out=ss[:, :], in_=lg, axis=AX.X, op=ALU.max), R=[("pb", 0)], W=SS)
            P.op("dve", lambda e: e.tensor_tensor(out=av, in0=lg, in1=ss[:, :].unsqueeze(2).to_broadcast([128, NT, NE]), op=ALU.subtract),
                 R=[("pb", 0)] + SS, W=AFT)
            P.op("act", lambda e: e.activation(out=av, in_=av, func=AF.Exp), R=AFT, W=AFT)
            P.op("dve", lambda e: e.tensor_reduce(out=ss[:, :], in_=av, axis=AX.X, op=ALU.add), R=AFT, W=SS)
            P.op("dve", lambda e: e.reciprocal(out=ss[:, :], in_=ss[:, :]), R=SS, W=SS)
            P.op("dve", lambda e: e.tensor_tensor(out=av, in0=av, in1=ss[:, :].unsqueeze(2).to_broadcast([128, NT, NE]), op=ALU.mult),
                 R=AFT + SS, W=AFT)
            if stop_after == "G0" and l == nlayers - 1:
                dbg_out("aff", av, [128, NT, NE], F32, R=AFT)
                return True
        return False

    def moe_body(pair, l):
        NP = len(pair)
        TP = 16 * NP if NP > 1 else 16
        NC_ = NP * 256
        if True:
            P.op("dve", lambda e: e.memset(sm[:, 32:33], 0.0), W=ALLX + MIXT + WEXPT + SYT + WDT + ["fence"])
            wsrc = [w_gate_e, w_up_e, w_down_e]
            nload = [0]

            def load_w(i):
                e_, m_ = divmod(i, 3)
                P.dma("pool", wexp[i % NRING], wsrc[m_][l, e_].rearrange("(k p) n -> p k n", p=128), W=[("wexp", i % NRING)], cost=13000.0)
            while nload[0] < NRING:
                load_w(nload[0])
                nload[0] += 1
            affT = tmp.ap().bitcast(F32)[0:TP, :]
            AFALL = [("aff", bi) for bi in range(NP)]
            for pr in range((NP + 1) // 2):
                nb_ = min(2, NP - 2 * pr) * 16
                for c in range(NT):
                    bk = 1 + c // 4
                    P.op("pe", lambda e, c=c, bk=bk, pr=pr, nb_=nb_: e.transpose(pbank[bk][0:nb_, (c % 4) * 128:(c % 4 + 1) * 128], aff[:, pr, c, 0:nb_], identf[:]),
                         R=[("aff", 2 * pr), ("aff", 2 * pr + 1), "identf"], W=[("pb", bk)])
                for q4 in range(4):
                    P.op("dve", lambda e, q4=q4, pr=pr, nb_=nb_: e.tensor_copy(out=affT[pr * 32:pr * 32 + nb_, q4 * 512:(q4 + 1) * 512], in_=pbank[1 + q4][0:nb_, :]),
                         R=[("pb", 1 + q4)], W=ttok(0, 8))
            for r in range(CAP // 8):
                rs = slice(r * 8, (r + 1) * 8)
                P.op("dve", lambda e, rs=rs: e.max(out=tvals[0:TP, rs], in_=affT), R=ttok(0, 8), W=["tvals"], cost=2300.0)
                P.op("dve", lambda e, rs=rs: e.max_index(out=tidx[0:TP, rs], in_max=tvals[0:TP, rs], in_values=affT), R=ttok(0, 8) + ["tvals"], W=["tidx"], cost=2300.0)
                P.op("dve", lambda e, rs=rs: e.match_replace(out=affT, in_to_replace=tvals[0:TP, rs], in_values=affT, imm_value=-1.0),
                     R=ttok(0, 8) + ["tvals"], W=ttok(0, 8), cost=2300.0)
            P.op("dve", lambda e: e.tensor_copy(out=tidxf[0:TP, :], in_=tidx[0:TP, :]), R=["tidx"], W=["tidxf"])
            for half in range(2):
                P.op("pe", lambda e, half=half: e.transpose(pbank[5][:, half * 112:half * 112 + TP], tidxf[0:TP, half * 128:(half + 1) * 128], identf[0:TP, 0:TP]),
                     R=["tidxf", "identf"], W=[("pb", 5)])
                P.op("pe", lambda e, half=half: e.transpose(pbank[5][:, 224 + half * 112:224 + half * 112 + TP], tvals[0:TP, half * 128:(half + 1) * 128], identf[0:TP, 0:TP]),
                     R=["tvals", "identf"], W=[("pb", 5)])
            P.op("dve", lambda e: e.tensor_copy(out=idxT[:, :, 0:TP], in_=pbank[5][:, 0:224].rearrange("p (h e) -> p h e", h=2)[:, :, 0:TP]), R=[("pb", 5)], W=["idxT"])
            P.op("dve", lambda e: e.tensor_copy(out=valsT[:, :, 0:TP], in_=pbank[5][:, 224:448].rearrange("p (h e) -> p h e", h=2)[:, :, 0:TP]), R=[("pb", 5)], W=["valsT"])
            if stop_after == "G1" and l == nlayers - 1:
                dbg_out("idxT", idxT[:, :, 0:NE], [128, 2, NE], I32, R=["idxT"])
                dbg_out("valsT", valsT[:, :, 0:NE], [128, 2, NE], F32, R=["valsT"])
                return True
            mflat = mT.ap().rearrange("p k t -> p (k t)")
            oe = mflat[:, 0:8192].bitcast(F32).rearrange("p (s d) -> p s d", s=4)
            xg = mflat[:, 8192:12288].rearrange("p (s d) -> p s d", s=4)
            xsT = mflat[:, 12288:16384].rearrange("p (k c) -> p k c", k=KD)
            gT = ptok.ap().rearrange("p c n -> p (c n)").rearrange("p (k c) -> p k c", k=KD)
            PTK = [("ptok", c) for c in range(NT)]
            XG = [("xg", i) for i in range(4)]
            OE = [("oe", i) for i in range(4)]
            P.op("dve", lambda e: e.memset(sm[:, 33:34], 0.0), W=ALLMT + XG + OE + PTK + ["xsT", "gT", "fence2"])

            NSP = (NP + 1) // 2
            steps = [(e_, sp) for e_ in range(NE) for sp in range(NSP)]

            def gather(step):
                e_, sp = steps[step]
                for bl in range(min(2, NP - 2 * sp)):
                    bi = 2 * sp + bl
                    for half in range(2):
                        sc = bl * 2 + half
                        P.op("pool", lambda g_, half=half, bi=bi, sc=sc: g_.indirect_dma_start(
                            out=xg[:, sc, :], out_offset=None, in_=h2_dram[bi][:, :],
                            in_offset=bass.IndirectOffsetOnAxis(ap=idxT[:, half, bi * 16 + e_:bi * 16 + e_ + 1], axis=0)),
                            R=["idxT"] + H2D[bi], W=[("xg", sc)], dma=True, cost=4000.0)
            gather(0)

            def ex_trans(step):
                e_, sp = steps[step]
                NL = min(2, NP - 2 * sp)
                NCL = NL * 256
                for sc in range(2 * NL):
                    tpx = pb_bf(6 + sc % 2)
                    for k in range(KD):
                        P.op("pe", lambda e, k=k, sc=sc, tpx=tpx: e.transpose(tpx[:, k * 128:(k + 1) * 128], xg[:, sc, k * 128:(k + 1) * 128], ident[:]),
                             R=[("xg", sc), "ident"], W=[("pb", 6 + sc % 2)], cost=100.0)
                    P.op("act", lambda e, sc=sc, tpx=tpx: e.activation(out=xsT[:, :, sc * 128:(sc + 1) * 128],
                                                                       in_=tpx[:, 0:1024].rearrange("p (k t) -> p k t", k=KD), func=AF.Copy),
                         R=[("pb", 6 + sc % 2)], W=["xsT"], cost=800.0)
                if step + 1 < len(steps):
                    gather(step + 1)

            def ex_gateup(step):
                e_, sp = steps[step]
                NL = min(2, NP - 2 * sp)
                NCL = NL * 256
                if sp == 0:
                    while nload[0] <= min(3 * e_ + 5, 3 * NE - 1):
                        load_w(nload[0])
                        nload[0] += 1
                sg_, su_, sd_ = (3 * e_) % NRING, (3 * e_ + 1) % NRING, (3 * e_ + 2) % NRING
                mmc = 125.0 * NL
                for f in range(KD):
                    bk = f % 2
                    fs = slice(f * 128, (f + 1) * 128)
                    for k in range(KD):
                        P.op("pe", lambda e, k=k, fs=fs, bk=bk: e.matmul(pbank[bk][:, 0:NCL], lhsT=wexp[sg_][:, k, fs], rhs=xsT[:, k, 0:NCL],
                                                                         start=(k == 0), stop=(k == KD - 1)), R=[("wexp", sg_), "xsT"], W=[("pb", bk)], cost=mmc)
                    for k in range(KD):
                        P.op("pe", lambda e, k=k, fs=fs, bk=bk: e.matmul(pbank[2 + bk][:, 0:NCL], lhsT=wexp[su_][:, k, fs], rhs=xsT[:, k, 0:NCL],
                                                                         start=(k == 0), stop=(k == KD - 1)), R=[("wexp", su_), "xsT"], W=[("pb", 2 + bk)], cost=mmc)
                    P.op("act", lambda e, bk=bk: e.activation(out=sab[:, bk, 0:NCL], in_=pbank[bk][:, 0:NCL], func=AF.Silu), R=[("pb", bk)], W=[("sab", bk)], cost=300.0 * NL)
                    P.op("dve", lambda e, bk=bk, f=f: e.tensor_tensor(out=gT[:, f, 0:NCL], in0=pbank[2 + bk][:, 0:NCL], in1=sab[:, bk, 0:NCL], op=ALU.mult),
                         R=[("pb", 2 + bk), ("sab", bk)], W=["gT"], cost=300.0 * NL)

            def ex_down(step):
                e_, sp = steps[step]
                NL = min(2, NP - 2 * sp)
                NCL = NL * 256
                sg_, su_, sd_ = (3 * e_) % NRING, (3 * e_ + 1) % NRING, (3 * e_ + 2) % NRING
                for sc in range(2 * NL):
                    bl, half = divmod(sc, 2)
                    bi = 2 * sp + bl
                    for dh in range(2):
                        bk = 4 + dh
                        for f in range(KD):
                            P.op("pe", lambda e, f=f, sc=sc, dh=dh, bk=bk: e.matmul(
                                pbank[bk][:, :], lhsT=gT[:, f, sc * 128:(sc + 1) * 128], rhs=wexp[sd_][:, f, dh * 512:(dh + 1) * 512],
                                start=(f == 0), stop=(f == KD - 1)), R=[("wexp", sd_), "gT"], W=[("pb", bk)], cost=230.0)
                        P.op("act", lambda e, sc=sc, dh=dh, bk=bk, bi=bi, half=half: e.activation(
                            out=oe[:, sc, dh * 512:(dh + 1) * 512], in_=pbank[bk][:, :], func=AF.Copy, scale=valsT[:, half, bi * 16 + e_:bi * 16 + e_ + 1]),
                            R=[("pb", bk), "valsT"], W=[("oe", sc)], cost=600.0)
                    P.op("pool", lambda g_, sc=sc, bi=bi, half=half: g_.indirect_dma_start(
                        out=xs_dram[pair[bi]][:, :], out_offset=bass.IndirectOffsetOnAxis(ap=idxT[:, half, bi * 16 + e_:bi * 16 + e_ + 1], axis=0),
                        in_=oe[:, sc, :], in_offset=None, compute_op=ALU.add),
                        R=[("oe", sc), "idxT"] + XSD[pair[bi]], W=[("xs_dram", pair[bi])], dma=True, cost=5000.0)
            ex_trans(0)
            for step in range(len(steps)):
                ex_gateup(step)
                if step + 1 < len(steps):
                    ex_trans(step + 1)
                ex_down(step)
            if stop_after == "G" and l == nlayers - 1:
                dbg_out("xs", xs_dram[0], [S, D], F32, R=[("xs_dram", 0)] + XSD[0])
                return True
        return False

    for p0 in range(0, nseq, GRP):
        pair = list(range(p0, min(p0 + GRP, nseq)))
        for l in range(nlayers):
            for bi, b in enumerate(pair):
                if layer_body(b, l, bi):
                    done = True
                    break
            if done:
                break
            if moe_body(pair, l):
                done = True
                break
        if done:
            break
        P.op("dve", lambda e: e.memset(sm[:, 35:36], 0.0), W=ALLMT + EXPACT + [("fn", i) for i in range(8)] + ["fence3"])
        for b in pair:
            norm_phase(xs_dram[b], gfin_b, None, y_dst=y_out[b], xtok=b)

    if SCHEDULE:
        P.schedule()
    P.emit(nc)
    st.close()
    return nc, dbg


def _host_consts(inp):
    f = lambda a: np.ascontiguousarray(np.asarray(a, dtype=np.float32))
    rep = lambda a, n=128: np.ascontiguousarray(np.broadcast_to(np.asarray(a, np.float32)[:, None, :], (a.shape[0], n, a.shape[1])))
    c = {}
    for k in ("w_in", "w_spatial", "w_pool", "w_attn_o", "w_sgu_o", "w_pool_o", "w_out", "w_router",
              "w_gate_e", "w_up_e", "w_down_e"):
        c[k] = f(inp[k])
    c["gmix_b"] = rep(inp["g_mix"])
    c["gffn_b"] = rep(inp["g_ffn"])
    c["gfin_b"] = np.ascontiguousarray(np.broadcast_to(np.asarray(inp["g_final"], np.float32)[None, :], (128, D)))
    c["gq_b"] = rep(inp["g_q"])
    c["gk_b"] = rep(inp["g_k"])
    c["gsgu_b"] = rep(inp["g_sgu"])
    c["bsp_t"] = np.ascontiguousarray(np.asarray(inp["b_spatial"], np.float32).transpose(0, 2, 1))
    c["psc_t"] = np.ascontiguousarray(np.asarray(inp["pool_scale"], np.float32).reshape(L, 2, 128).transpose(0, 2, 1))
    c["ident"] = np.eye(128, dtype=np.float32)
    c["cs"] = _rope_table()
    c["band"] = _pool_bands()
    return c


def kernel(**inputs):
    x = np.asarray(inputs["x"], dtype=np.float32)
    consts = _host_consts(inputs)
    nc, _ = build_program()
    in_maps = []
    for i in range(N_CORES):
        m = dict(consts)
        m["x"] = np.ascontiguousarray(x[i * NSEQ:(i + 1) * NSEQ])
        in_maps.append(m)
    res = run_bass_kernel_spmd(nc, in_maps, core_ids=list(range(N_CORES)))
    out = np.concatenate([np.asarray(r["y"]) for r in res.results], axis=0)
    return out.astype(np.float32)
```
